# Optimizing a Trainium2 kernel written in Bass

```python
import jax, jax.numpy as jnp
from jax import lax
import numpy as np

D_MODEL = 4096
BATCH = 2
SEQ = 8192
DEPTH = 1

EPS = 1e-6
ROPE_THETA = 10000.0

MLA_HEADS = 16
MLA_Q_LORA = 1024
MLA_KV_LORA = 512
MLA_NOPE_DIM = 128
MLA_ROPE_DIM = 64
MLA_QK_DIM = MLA_NOPE_DIM + MLA_ROPE_DIM
MLA_V_DIM = 128
MLA_WIDTH = MLA_HEADS * MLA_V_DIM
Q_BLK = 128

GLA_HEADS = 4
GLA_V_WIDTH = D_MODEL - MLA_WIDTH
GLA_K_WIDTH = GLA_V_WIDTH // 2
GLA_HEAD_K = GLA_K_WIDTH // GLA_HEADS
GLA_HEAD_V = GLA_V_WIDTH // GLA_HEADS
GLA_GATE_RANK = 16
GLA_GATE_TAU = 16.0
GLA_CHUNK = 64

MIX_WIDTH = MLA_WIDTH + GLA_V_WIDTH
IN_SIZES = (MLA_Q_LORA, MLA_KV_LORA, MLA_ROPE_DIM, GLA_K_WIDTH, GLA_K_WIDTH, GLA_V_WIDTH, GLA_GATE_RANK, GLA_V_WIDTH)
IN_WIDTH = int(sum(IN_SIZES))
IN_SPLIT_POINTS = tuple(int(c) for c in np.cumsum(IN_SIZES)[:-1])

MEM_TOKENS = 256
XATTN_HEADS = 4
XATTN_HEAD_DIM = 256
XATTN_WIDTH = XATTN_HEADS * XATTN_HEAD_DIM

PEER_HEADS = 8
PEER_N_KEYS = 128
PEER_N_EXPERTS = PEER_N_KEYS * PEER_N_KEYS
PEER_QUERY_DIM = 256
PEER_HALF = PEER_QUERY_DIM // 2
PEER_TOPK = 16
PEER_TOK_BLK = 64

kernel_name = 'hybrid_mla_gla_peer_block'


def rms_norm(x, g):
    xf = x.astype(jnp.float32)
    y = xf * lax.rsqrt(jnp.mean(xf * xf, axis=-1, keepdims=True) + EPS)
    return (y * g.astype(jnp.float32)).astype(x.dtype)


def apply_rope(x, positions):
    d = x.shape[-1]
    inv_freq = ROPE_THETA ** (-jnp.arange(0, d, 2, dtype=jnp.float32) / d)
    ang = positions.astype(jnp.float32)[:, :, None] * inv_freq
    cos = jnp.cos(ang)[:, :, None, :]
    sin = jnp.sin(ang)[:, :, None, :]
    x1, x2 = jnp.split(x.astype(jnp.float32), 2, axis=-1)
    return jnp.concatenate([x1 * cos - x2 * sin, x2 * cos + x1 * sin], axis=-1).astype(x.dtype)


def causal_block_attention(q, k, v, scale):
    seq = q.shape[1]
    outs = []
    for blk in range(seq // Q_BLK):
        lo, hi = blk * Q_BLK, (blk + 1) * Q_BLK
        s = jnp.einsum('bqhd,bkhd->bhqk', q[:, lo:hi], k[:, :hi]).astype(jnp.float32) * scale
        mask = jnp.arange(hi)[None, :] <= jnp.arange(lo, hi)[:, None]
        s = jnp.where(mask, s, -jnp.inf)
        p = jax.nn.softmax(s, axis=-1).astype(v.dtype)
        outs.append(jnp.einsum('bhqk,bkhd->bqhd', p, v[:, :hi]))
    return jnp.concatenate(outs, axis=1)


def gla_chunked(q, k, v, g):
    B, S, H, dk = q.shape
    dv = v.shape[-1]
    C = GLA_CHUNK
    N = S // C

    def to_chunks(t):
        return t.reshape(B, N, C, H, t.shape[-1]).transpose(0, 3, 1, 2, 4)

    q, k, v, g = to_chunks(q), to_chunks(k), to_chunks(v), to_chunks(g)
    b = jnp.cumsum(g, axis=3)
    b_last = b[:, :, :, -1:, :]
    qe = q * (dk ** -0.5) * jnp.exp(b)
    ke = k * jnp.exp(-b)
    kd = k * jnp.exp(b_last - b)
    causal = jnp.tril(jnp.ones((C, C), dtype=bool))
    att = jnp.where(causal, jnp.einsum('bhncd,bhnjd->bhncj', qe, ke), 0.0)
    o_intra = jnp.einsum('bhncj,bhnjv->bhncv', att, v)

    def step(state, inp):
        qe_n, kd_n, v_n, dec_n = inp
        o_n = jnp.einsum('bhcd,bhdv->bhcv', qe_n, state)
        state = state * dec_n[..., None] + jnp.einsum('bhcd,bhcv->bhdv', kd_n, v_n)
        return state, o_n

    xs = (jnp.moveaxis(qe, 2, 0), jnp.moveaxis(kd, 2, 0), jnp.moveaxis(v, 2, 0),
          jnp.moveaxis(jnp.exp(b_last[:, :, :, 0, :]), 2, 0))
    state0 = jnp.zeros((B, H, dk, dv), jnp.float32)
    _, o_inter = lax.scan(step, state0, xs)
    o = o_intra + jnp.moveaxis(o_inter, 0, 2)
    return o.transpose(0, 2, 3, 1, 4).reshape(B, S, H, dv)


def hybrid_mixer(xn, positions, w_in, mla_q_norm, w_uq, mla_kv_norm, w_ukv, mla_out_norm,
                 w_gate_up, b_gate, gla_out_norm, w_out):
    B, S, _ = xn.shape
    h = xn @ w_in
    c_q, c_kv, k_r, g_q, g_k, g_v, g_lr, g_og = jnp.split(h, IN_SPLIT_POINTS, axis=-1)

    q = (rms_norm(c_q, mla_q_norm) @ w_uq).reshape(B, S, MLA_HEADS, MLA_QK_DIM)
    q_full = jnp.concatenate([q[..., :MLA_NOPE_DIM], apply_rope(q[..., MLA_NOPE_DIM:], positions)], axis=-1)
    kv = (rms_norm(c_kv, mla_kv_norm) @ w_ukv).reshape(B, S, MLA_HEADS, MLA_NOPE_DIM + MLA_V_DIM)
    k_nope, v_mla = kv[..., :MLA_NOPE_DIM], kv[..., MLA_NOPE_DIM:]
    k_rope = apply_rope(k_r[:, :, None, :], positions)
    k_full = jnp.concatenate([k_nope, jnp.broadcast_to(k_rope, (B, S, MLA_HEADS, MLA_ROPE_DIM))], axis=-1)
    o_mla = causal_block_attention(q_full, k_full, v_mla, MLA_QK_DIM ** -0.5).reshape(B, S, MLA_WIDTH)
    o_mla = rms_norm(o_mla, mla_out_norm)

    log_a = jax.nn.log_sigmoid((g_lr @ w_gate_up + b_gate).astype(jnp.float32)) / GLA_GATE_TAU
    f32 = jnp.float32
    o_gla = gla_chunked(g_q.astype(f32).reshape(B, S, GLA_HEADS, GLA_HEAD_K),
                        g_k.astype(f32).reshape(B, S, GLA_HEADS, GLA_HEAD_K),
                        g_v.astype(f32).reshape(B, S, GLA_HEADS, GLA_HEAD_V),
                        log_a.reshape(B, S, GLA_HEADS, GLA_HEAD_K))
    o_gla = rms_norm(o_gla, gla_out_norm).reshape(B, S, GLA_V_WIDTH) * jax.nn.silu(g_og.astype(f32))

    mixed = jnp.concatenate([o_mla, o_gla.astype(xn.dtype)], axis=-1)
    return mixed @ w_out


def memory_cross_attention(xn, mn, w_cq, w_ck, w_cv, w_co):
    B, S, _ = xn.shape
    M = mn.shape[1]
    q = (xn @ w_cq).reshape(B, S, XATTN_HEADS, XATTN_HEAD_DIM)
    k = (mn @ w_ck).reshape(B, M, XATTN_HEADS, XATTN_HEAD_DIM)
    v = (mn @ w_cv).reshape(B, M, XATTN_HEADS, XATTN_HEAD_DIM)
    s = jnp.einsum('bshd,bmhd->bhsm', q, k).astype(jnp.float32) * (XATTN_HEAD_DIM ** -0.5)
    p = jax.nn.softmax(s, axis=-1).astype(v.dtype)
    o = jnp.einsum('bhsm,bmhd->bshd', p, v).reshape(B, S, XATTN_WIDTH)
    return o @ w_co


def peer_ffn(xn, w_peer_q, peer_sub_keys, peer_u, peer_v):
    B, S, D = xn.shape
    q = (xn @ w_peer_q).reshape(B, S, PEER_HEADS, 2, PEER_HALF)
    s = jnp.einsum('bshpd,hpnd->bshpn', q, peer_sub_keys).astype(jnp.float32)
    top_s, top_i = lax.top_k(s, PEER_TOPK)
    cand = top_s[..., 0, :, None] + top_s[..., 1, None, :]
    cand_s, cand_i = lax.top_k(cand.reshape(B, S, PEER_HEADS, PEER_TOPK * PEER_TOPK), PEER_TOPK)
    i1 = jnp.take_along_axis(top_i[..., 0, :], cand_i // PEER_TOPK, axis=-1)
    i2 = jnp.take_along_axis(top_i[..., 1, :], cand_i % PEER_TOPK, axis=-1)
    expert = i1 * PEER_N_KEYS + i2
    gate = jax.nn.softmax(cand_s, axis=-1).astype(xn.dtype)

    n_blk = (B * S) // PEER_TOK_BLK
    xb = xn.reshape(n_blk, PEER_TOK_BLK, D)
    eb = expert.reshape(n_blk, PEER_TOK_BLK, PEER_HEADS * PEER_TOPK)
    gb = gate.reshape(n_blk, PEER_TOK_BLK, PEER_HEADS * PEER_TOPK)

    def token_block(args):
        xt, et, gt = args
        a = jax.nn.gelu(jnp.einsum('td,ted->te', xt, peer_u[et]), approximate=False) * gt
        return jnp.einsum('te,ted->td', a, peer_v[et])

    return lax.map(token_block, (xb, eb, gb)).reshape(B, S, D)


def setup_inputs(seed: int = 0) -> dict:
    key = jax.random.key(seed)
    ks = jax.random.split(key, 32)
    f32 = jnp.float32
    L = DEPTH

    def normal(k, shape, scale):
        return jax.random.normal(k, shape, f32) * scale

    def gain(k, shape):
        return 1.0 + 0.01 * jax.random.normal(k, shape, f32)

    return {
        'x': normal(ks[0], (BATCH, SEQ, D_MODEL), 1.0),
        'mem': normal(ks[1], (BATCH, MEM_TOKENS, D_MODEL), 1.0),
        'positions': jnp.arange(SEQ, dtype=jnp.int32)[None, :]
                     + jax.random.randint(ks[2], (BATCH, 1), 0, 4096, dtype=jnp.int32),
        'norm_mem': gain(ks[3], (D_MODEL,)),
        'norm_mix': gain(ks[4], (L, D_MODEL)),
        'w_in': normal(ks[5], (L, D_MODEL, IN_WIDTH), D_MODEL ** -0.5),
        'mla_q_norm': gain(ks[6], (L, MLA_Q_LORA)),
        'w_uq': normal(ks[7], (L, MLA_Q_LORA, MLA_HEADS * MLA_QK_DIM), MLA_Q_LORA ** -0.5),
        'mla_kv_norm': gain(ks[8], (L, MLA_KV_LORA)),
        'w_ukv': normal(ks[9], (L, MLA_KV_LORA, MLA_HEADS * (MLA_NOPE_DIM + MLA_V_DIM)), MLA_KV_LORA ** -0.5),
        'mla_out_norm': gain(ks[10], (L, MLA_WIDTH)),
        'w_gate_up': normal(ks[11], (L, GLA_GATE_RANK, GLA_K_WIDTH), GLA_GATE_RANK ** -0.5),
        'b_gate': normal(ks[12], (L, GLA_K_WIDTH), 0.1),
        'gla_out_norm': gain(ks[13], (L, GLA_HEAD_V)),
        'w_out': normal(ks[14], (L, MIX_WIDTH, D_MODEL), MIX_WIDTH ** -0.5),
        'norm_cross': gain(ks[15], (L, D_MODEL)),
        'w_cq': normal(ks[16], (L, D_MODEL, XATTN_WIDTH), D_MODEL ** -0.5),
        'w_ck': normal(ks[17], (L, D_MODEL, XATTN_WIDTH), D_MODEL ** -0.5),
        'w_cv': normal(ks[18], (L, D_MODEL, XATTN_WIDTH), D_MODEL ** -0.5),
        'w_co': normal(ks[19], (L, XATTN_WIDTH, D_MODEL), XATTN_WIDTH ** -0.5),
        'norm_ffn': gain(ks[20], (L, D_MODEL)),
        'w_peer_q': normal(ks[21], (L, D_MODEL, PEER_HEADS * PEER_QUERY_DIM), D_MODEL ** -0.5),
        'peer_sub_keys': normal(ks[22], (L, PEER_HEADS, 2, PEER_N_KEYS, PEER_HALF), PEER_HALF ** -0.5),
        'peer_u': normal(ks[23], (L, PEER_N_EXPERTS, D_MODEL), D_MODEL ** -0.5),
        'peer_v': normal(ks[24], (L, PEER_N_EXPERTS, D_MODEL), PEER_HEADS ** -0.5),
        'norm_final': gain(ks[25], (D_MODEL,)),
    }


def reference(x, mem, positions, norm_mem, norm_mix, w_in, mla_q_norm, w_uq, mla_kv_norm, w_ukv,
              mla_out_norm, w_gate_up, b_gate, gla_out_norm, w_out, norm_cross, w_cq, w_ck, w_cv, w_co,
              norm_ffn, w_peer_q, peer_sub_keys, peer_u, peer_v, norm_final):
    mn = rms_norm(mem, norm_mem)
    h = x
    for l in range(DEPTH):
        h = h + hybrid_mixer(rms_norm(h, norm_mix[l]), positions, w_in[l], mla_q_norm[l], w_uq[l],
                             mla_kv_norm[l], w_ukv[l], mla_out_norm[l], w_gate_up[l], b_gate[l],
                             gla_out_norm[l], w_out[l])
        h = h + memory_cross_attention(rms_norm(h, norm_cross[l]), mn, w_cq[l], w_ck[l], w_cv[l], w_co[l])
        h = h + peer_ffn(rms_norm(h, norm_ffn[l]), w_peer_q[l], peer_sub_keys[l], peer_u[l], peer_v[l])
    return rms_norm(h, norm_final)
```

```python
import contextlib
import numpy as np
import concourse.bass as bass
import concourse.mybir as mybir
from concourse.bass_utils import run_bass_kernel_spmd

F32 = mybir.dt.float32
BF16 = mybir.dt.bfloat16
I32 = mybir.dt.int32
AF = mybir.ActivationFunctionType
ALU = mybir.AluOpType

D = 4096
NT = 2
TG = NT * 128
EPS = 1e-6
NEG = -30000.0


class Buf:
    __slots__ = ("name", "w", "rd")

    def __init__(self, name):
        self.name = name
        self.w = None
        self.rd = []


class Op:
    __slots__ = ("eng", "fn", "deps", "is_dma", "semkey", "count", "needs_inc", "idx")


class Prog:
    ENGS = ("pe", "act", "dve", "pool", "sp")

    def __init__(self, nc, es):
        self.nc = nc
        self.es = es
        self.ops = []
        self.start = 0
        self.sems = {}
        self.counts = {}
        self.waited = {e: {} for e in self.ENGS}

    def op(self, eng, fn, reads=(), writes=(), dma=False, nowaw=False, semname=None):
        o = Op()
        o.eng, o.fn, o.is_dma, o.idx = eng, fn, dma, len(self.ops)
        o.needs_inc, o.count = False, None
        deps = {}
        for b in reads:
            if b.w is not None and b.w >= self.start:
                deps[b.w] = "raw"
        for b in writes:
            if b.w is not None and b.w >= self.start and b.w not in deps:
                if not (nowaw and dma and self.ops[b.w].is_dma and not b.rd):
                    deps[b.w] = "waw"
            last = {}
            for r in b.rd:
                if r < self.start:
                    continue
                ro = self.ops[r]
                if ro.is_dma:
                    if r not in deps:
                        deps[r] = "war"
                else:
                    last[ro.eng] = max(last.get(ro.eng, -1), r)
            for r in last.values():
                if r not in deps:
                    deps[r] = "war"
        o.deps = []
        for d, kind in deps.items():
            po = self.ops[d]
            if (not dma) and (not po.is_dma) and po.eng == eng and (kind != "raw" or eng == "pe"):
                continue
            o.deps.append(d)
        if dma:
            o.semkey = ("dma", semname or writes[0].name)
        else:
            o.semkey = ("eng", eng)
        for b in writes:
            b.w = o.idx
            b.rd = []
        for b in reads:
            b.rd.append(o.idx)
        self.ops.append(o)
        return o.idx

    def _sem(self, k):
        if k not in self.sems:
            self.sems[k] = self.es.enter_context(self.nc.semaphore("s%d" % len(self.sems)))
        return self.sems[k]

    def emit(self, final_waits=()):
        nc = self.nc
        ops = self.ops
        cur = ops[self.start:]
        for o in cur:
            for d in o.deps:
                ops[d].needs_inc = True
        for d in final_waits:
            ops[d].needs_inc = True
        for o in cur:
            if o.is_dma:
                self.counts[o.semkey] = self.counts.get(o.semkey, 0) + 16
                o.count = self.counts[o.semkey]
                self._sem(o.semkey)
            elif o.needs_inc:
                self.counts[o.semkey] = self.counts.get(o.semkey, 0) + 1
                o.count = self.counts[o.semkey]
                self._sem(o.semkey)
        per = {e: [o for o in cur if o.eng == e] for e in self.ENGS}
        dma_last = {}
        for o in cur:
            if o.is_dma:
                dma_last[o.semkey] = o.count
        sems = self.sems

        def run(engname, eng):
            waited = self.waited[engname]
            for o in per[engname]:
                need = {}
                for d in o.deps:
                    po = ops[d]
                    if need.get(po.semkey, 0) < po.count:
                        need[po.semkey] = po.count
                for k, v in need.items():
                    if waited.get(k, 0) >= v:
                        continue
                    eng.wait_ge(sems[k], v)
                    waited[k] = v
                ins = o.fn(eng)
                if o.is_dma:
                    ins.then_inc(sems[o.semkey], 16)
                elif o.needs_inc:
                    ins.then_inc(sems[o.semkey], 1)
            if engname == "sp":
                for k, v in dma_last.items():
                    if waited.get(k, 0) < v:
                        eng.wait_ge(sems[k], v)
                        waited[k] = v

        with nc.Block(no_gpsimd_drain=True) as block:
            @block.tensor
            def _(e):
                run("pe", e)

            @block.scalar
            def _(e):
                run("act", e)

            @block.vector
            def _(e):
                run("dve", e)

            @block.gpsimd
            def _(e):
                run("pool", e)

            @block.sync
            def _(e):
                run("sp", e)
        self.start = len(ops)


class TB:
    def __init__(self, t, name):
        self.t = t
        self.b = Buf(name)

    def __getitem__(self, k):
        return self.t[k]


def build(S, NOWN, dbg=False):
    NGRP = S // TG
    assert NGRP == 4 * NOWN
    nc = bass.Bass("TRN2", target_bir_lowering=False)

    def din(name, shape, dt=F32):
        return nc.dram_tensor(name, list(shape), dt, kind="ExternalInput").ap()

    x_all = din("x_all", [S, D])
    x_own = din("x_own", [NOWN * TG, D])
    pos_all = din("pos_all", [S, 1], I32)
    pos_own = din("pos_own", [NOWN * TG, 1], I32)
    mem = din("mem", [256, D])
    w_in = din("w_in", [D, 7760])
    w_uq = din("w_uq", [1024, 3072])
    w_ukv = din("w_ukv", [512, 4096])
    wg_in = din("wg", [17, 1024])
    w_out = din("w_out", [D, D])
    w_cq = din("w_cq", [D, 1024])
    w_ck = din("w_ck", [D, 1024])
    w_cv = din("w_cv", [D, 1024])
    w_co = din("w_co", [1024, D])
    w_pq = din("w_pq", [D, 2048])
    subk = din("subk", [128, 16, 128])
    peer_uT = din("peer_uT", [D, 16384])
    peer_v = din("peer_v", [16384, D])
    vecs_in = din("vecs", [128, 160])
    gfin_in = din("gfin", [1, D])
    cst_in = din("cst", [128, 1024])
    core_in = din("corec", [128, 1028])
    out = nc.dram_tensor("out", [NOWN * TG, D], F32, kind="ExternalOutput").ap()
    KnS = nc.dram_tensor("KnS", [16, 128, S], BF16, kind="Internal").ap()
    KrS = nc.dram_tensor("KrS", [128, S], BF16, kind="Internal").ap()
    VS = nc.dram_tensor("VS", [16, 128, S // 128, 130], BF16, kind="Internal").ap()
    w_in_b = nc.dram_tensor("w_in_b", [D, 7760], BF16, kind="Internal").ap()
    w_uq_b = nc.dram_tensor("w_uq_b", [1024, 3072], BF16, kind="Internal").ap()
    w_ukv_b = nc.dram_tensor("w_ukv_b", [512, 4096], BF16, kind="Internal").ap()
    w_out_b = nc.dram_tensor("w_out_b", [D, D], BF16, kind="Internal").ap()
    w_cq_b = nc.dram_tensor("w_cq_b", [D, 1024], BF16, kind="Internal").ap()
    w_co_b = nc.dram_tensor("w_co_b", [1024, D], BF16, kind="Internal").ap()
    w_pq_b = nc.dram_tensor("w_pq_b", [D, 2048], BF16, kind="Internal").ap()
    Ubf = nc.dram_tensor("Ubf", [64, 128, 32 * 256], BF16, kind="Internal").ap()
    Vbf = nc.dram_tensor("Vbf", [128, 128, 16 * 256], BF16, kind="Internal").ap()
    hA = nc.dram_tensor("hA", [TG, D], F32, kind="Internal").ap()
    hB = nc.dram_tensor("hB", [TG, D], F32, kind="Internal").ap()
    dbg_out = None
    if dbg:
        dbg_out = nc.dram_tensor("dbg", [NOWN * TG, D], F32, kind="ExternalOutput").ap()

    with contextlib.ExitStack() as ges:
        P = Prog(nc, ges)

        uid = [0]

        def sb(es, name, shape, dt):
            uid[0] += 1
            return TB(es.enter_context(nc.sbuf_tensor("s%d_%s" % (uid[0], name), list(shape), dt)), name)

        def ps(es, name, shape, dt):
            return TB(es.enter_context(nc.psum_tensor("p_" + name, list(shape), dt)), name)

        vecs = sb(ges, "vecs", [128, 160], F32)
        cst = sb(ges, "cst", [128, 1024], F32)
        corec = sb(ges, "corec", [128, 4], F32)
        identb = sb(ges, "identb", [128, 128], BF16)
        sidb = sb(ges, "sidb", [128, 8, 128], BF16)
        negfull = sb(ges, "negfull", [128, TG], BF16)
        maskdiag = sb(ges, "maskdiag", [128, NT, TG], BF16)
        state = sb(ges, "state", [128, 8, 512], F32)
        snap = sb(ges, "snap", [128, 8, 512], F32)
        KmT = sb(ges, "KmT", [128, 8, 256], BF16)
        Vm = sb(ges, "Vm", [128, 2, 4, 258], BF16)
        subkT = sb(ges, "subkT", [128, 16, 128], BF16)
        wg = sb(ges, "wg", [32, 1024], BF16)
        glrT = sb(ges, "glrT", [32, TG], BF16)
        pb = [ps(ges, "pb%d" % i, [128, 512], F32) for i in range(6)]
        pt = [ps(ges, "pt%d" % i, [128, 1024], BF16) for i in range(2)]
        rot = {"a": 0, "t": 0}

        bank_lo = [0]

        def bank():
            n = 6 - bank_lo[0]
            rot["a"] = (rot["a"] + 1) % n
            return pb[bank_lo[0] + rot["a"]]

        def tbank():
            rot["t"] = (rot["t"] + 1) % 2
            return pt[rot["t"]]

        V_MIX, V_CROSS, V_FFN, V_MEM, V_Q, V_KV, V_MO, V_GO = 0, 32, 64, 96, 128, 136, 140, 156
        C_ID, C_M1, C_TRI, C_CM, C_CIND, C_ROPE = 0, 128, 256, 384, 512, 514

        def act_copy(o, i, reads, writes, scale=None):
            if scale is None:
                P.op("act", lambda e: e.copy(out=o, in_=i), reads, writes)
            else:
                P.op("act", lambda e: e.activation(out=o, in_=i, func=AF.Copy, scale=scale), reads, writes)

        def rstd_from_ssq(es, ssq, n, tag):
            k = ssq.t.shape[1]
            r = sb(es, "rstd_" + tag, [128, k], F32)
            P.op("dve", lambda e: e.tensor_scalar(out=r[:], in0=ssq[:], scalar1=1.0 / n, scalar2=EPS,
                                                  op0=ALU.mult, op1=ALU.add), [ssq.b], [r.b])
            P.op("act", lambda e: e.activation(out=r[:], in_=r[:], func=AF.Sqrt), [r.b], [r.b])
            P.op("dve", lambda e: e.reciprocal(out=r[:], in_=r[:]), [r.b], [r.b])
            return r

        def transposes(src, nk, dstT, tt, gcol=None, kc0=0):
            for kc in range(nk):
                tb = tbank()
                P.op("pe", lambda e, kc=kc, tb=tb: e.transpose(out=tb[:, 0:128], in_=src(kc), identity=identb[:]),
                     [src.tb.b, identb.b], [tb.b])
                o = dstT[:, kc0 + kc, tt * 128:(tt + 1) * 128]
                if gcol is None:
                    P.op("dve", lambda e, o=o, tb=tb: e.tensor_copy(out=o, in_=tb[:, 0:128]), [tb.b], [dstT.b])
                else:
                    P.op("dve", lambda e, o=o, tb=tb, c=gcol + kc: e.tensor_scalar(
                        out=o, in0=tb[:, 0:128], scalar1=vecs[:, c:c + 1], scalar2=None, op0=ALU.mult),
                        [tb.b, vecs.b], [dstT.b])

        class Src:
            def __init__(self, tb, f):
                self.tb, self.f = tb, f

            def __call__(self, kc):
                return self.f(kc)

        def norm_T(es, rows, ntile, gcol, dstT, tag):
            xt = [sb(es, "xt%d_%s" % (i, tag), [128, D], F32) for i in range(2)]
            xs = sb(es, "xs_" + tag, [128, D], BF16)
            junk = xs
            ssq = sb(es, "ssq_" + tag, [128, ntile], F32)
            for tt in range(ntile):
                x = xt[tt % 2]
                P.op("sp", lambda e, x=x, tt=tt: e.dma_start(out=x[:], in_=rows(tt)), [], [x.b], dma=True)
                P.op("act", lambda e, x=x, tt=tt: e.activation(out=junk[:], in_=x[:], func=AF.Square,
                                                              accum_out=ssq[:, tt:tt + 1]), [x.b], [junk.b, ssq.b])
            rstd = rstd_from_ssq(es, ssq, D, tag)
            for tt in range(ntile):
                x = xt[tt % 2]
                if tt >= 2:
                    P.op("sp", lambda e, x=x, tt=tt: e.dma_start(out=x[:], in_=rows(tt)), [], [x.b], dma=True)
                P.op("dve", lambda e, x=x, tt=tt: e.tensor_scalar(out=xs[:], in0=x[:], scalar1=rstd[:, tt:tt + 1],
                                                                 scalar2=None, op0=ALU.mult), [x.b, rstd.b], [xs.b])
                transposes(Src(xs, lambda kc: xs[:, kc * 128:(kc + 1) * 128]), 32, dstT, tt, gcol)

        wrot = {"i": 0}

        def linear(wb, actT, KC, wap, blocks, ntile, evac):
            for ci, csz in enumerate(blocks):
                wrot["i"] ^= 1
                w = wb[wrot["i"]]
                P.op("pool", lambda e, w=w, ci=ci, csz=csz: e.dma_start(out=w[:, 0:KC, 0:csz], in_=wap(ci)),
                     [], [w.b], dma=True)
                for tt in range(ntile):
                    pbk = bank()
                    for kc in range(KC):
                        P.op("pe", lambda e, kc=kc, tt=tt, w=w, pbk=pbk, csz=csz: e.matmul(
                            pbk[:, 0:csz], lhsT=actT[:, kc, tt * 128:(tt + 1) * 128], rhs=w[:, kc, 0:csz],
                            start=(kc == 0), stop=(kc == KC - 1)), [actT.b, w.b], [pbk.b])
                    evac(ci, tt, pbk, csz)

        def linearT(wb, actT, KC, wap, nblk, ncols, evac):
            for ci in range(nblk):
                wrot["i"] ^= 1
                w = wb[wrot["i"]]
                P.op("pool", lambda e, w=w, ci=ci: e.dma_start(out=w[:, 0:KC, 0:512], in_=wap(ci)), [], [w.b], dma=True)
                for cb in range(4):
                    pbk = bank()
                    for kc in range(KC):
                        P.op("pe", lambda e, kc=kc, cb=cb, w=w, pbk=pbk: e.matmul(
                            pbk[:, 0:ncols], lhsT=w[:, kc, cb * 128:(cb + 1) * 128], rhs=actT[:, kc, 0:ncols],
                            start=(kc == 0), stop=(kc == KC - 1)), [actT.b, w.b], [pbk.b])
                    evac(ci * 4 + cb, pbk)

        def wcols(wdram, c0):
            v = wdram.rearrange("(c p) n -> p c n", p=128)
            return lambda ci, c0=c0: v[:, :, c0 + ci * 512: c0 + ci * 512 + 512]

        with contextlib.ExitStack() as es:
            P.op("sp", lambda e: e.dma_start(out=vecs[:], in_=vecs_in), [], [vecs.b], dma=True)
            P.op("sp", lambda e: e.dma_start(out=cst[:], in_=cst_in), [], [cst.b], dma=True)
            P.op("sp", lambda e: e.dma_start(out=corec[:], in_=core_in[:, 0:4]), [], [corec.b], dma=True)
            sidf = sb(es, "sidf", [128, 1024], F32)
            P.op("sp", lambda e: e.dma_start(out=sidf[:], in_=core_in[:, 4:1028]), [], [sidf.b], dma=True)
            P.op("pool", lambda e: e.dma_start(out=subkT[:], in_=subk), [], [subkT.b], dma=True)
            P.op("dve", lambda e: e.memset(wg[:], 0.0), [], [wg.b])
            P.op("pool", lambda e: e.dma_start(out=wg[0:17, :], in_=wg_in), [], [wg.b], dma=True)
            P.op("dve", lambda e: e.memset(glrT[:], 1.0), [], [glrT.b])
            P.op("dve", lambda e: e.tensor_copy(out=identb[:], in_=cst[:, C_ID:C_ID + 128]), [cst.b], [identb.b])
            P.op("dve", lambda e: e.tensor_copy(out=sidb[:].rearrange("p a b -> p (a b)"), in_=sidf[:]),
                 [sidf.b], [sidb.b])
            P.op("dve", lambda e: e.memset(negfull[:], NEG), [], [negfull.b])
            P.op("dve", lambda e: e.memset(maskdiag[:], 0.0), [], [maskdiag.b])
            for kcin in range(NT):
                for qb in range(NT):
                    if qb < kcin:
                        P.op("dve", lambda e, kcin=kcin, qb=qb: e.memset(maskdiag[:, kcin, qb * 128:(qb + 1) * 128], NEG),
                             [], [maskdiag.b])
                    elif qb == kcin:
                        P.op("dve", lambda e, kcin=kcin, qb=qb: e.tensor_scalar(
                            out=maskdiag[:, kcin, qb * 128:(qb + 1) * 128], in0=cst[:, 640:768],
                            scalar1=-NEG, scalar2=NEG, op0=ALU.mult, op1=ALU.add), [cst.b], [maskdiag.b])
            P.op("dve", lambda e: e.memset(state[:], 0.0), [], [state.b])
            P.op("dve", lambda e: e.memset(snap[:], 0.0), [], [snap.b])
            P.op("dve", lambda e: e.memset(Vm[:], 1.0), [], [Vm.b])
            wb = [sb(es, "wb%d" % i, [128, 32, 512], BF16) for i in range(2)]
            mnT = sb(es, "mnT", [128, 32, 256], BF16)
            norm_T(es, lambda tt: mem[tt * 128:(tt + 1) * 128, :], 2, V_MEM, mnT, "mem")

            def ev_k(cb, pbk):
                P.op("act", lambda e: e.copy(out=KmT[:, cb, :], in_=pbk[:, 0:256]), [pbk.b], [KmT.b])
            linearT(wb, mnT, 32, wcols(w_ck, 0), 2, 256, ev_k)

            def ev_v(ci, tt, pbk, csz):
                P.op("act", lambda e: e.copy(out=Vm[:, tt, ci * 2:(ci + 1) * 2, 0:256],
                                             in_=pbk[:, 0:512].rearrange("p (h d) -> p h d", d=256)), [pbk.b], [Vm.b])
            linear(wb, mnT, 32, wcols(w_cv, 0), [512, 512], 2, ev_v)
            P.emit()

        with contextlib.ExitStack() as es:
            stg = [sb(es, "stg%d" % i, [128, 32, 512], BF16) for i in range(2)]
            cn = [0]

            def convert(src, dst, K, N):
                KC = K // 128
                sv = src.rearrange("(c p) n -> p c n", p=128)
                dv = dst.rearrange("(c p) n -> p c n", p=128)
                db_ = Buf("cv_" + str(cn[0]))
                for c0 in range(0, N, 512):
                    csz = min(512, N - c0)
                    st = stg[cn[0] % 2]
                    cn[0] += 1
                    P.op("pool", lambda e, st=st, c0=c0, csz=csz: e.dma_start(out=st[:, 0:KC, 0:csz], in_=sv[:, :, c0:c0 + csz]),
                         [], [st.b], dma=True)
                    P.op("sp", lambda e, st=st, c0=c0, csz=csz: e.dma_start(out=dv[:, :, c0:c0 + csz], in_=st[:, 0:KC, 0:csz]),
                         [st.b], [db_], dma=True, nowaw=True, semname="cvout")
            convert(w_in, w_in_b, D, 7760)
            convert(w_uq, w_uq_b, 1024, 3072)
            convert(w_ukv, w_ukv_b, 512, 4096)
            convert(w_out, w_out_b, D, D)
            convert(w_cq, w_cq_b, D, 1024)
            convert(w_co, w_co_b, 1024, D)
            convert(w_pq, w_pq_b, D, 2048)
            P.emit()

        def rope_tables(es, posrows, ntile, tag):
            cs = sb(es, "cs_" + tag, [128, ntile, 2, 32], F32)
            pi_ = sb(es, "posi_" + tag, [128, ntile], I32)
            pf = sb(es, "posf_" + tag, [128, ntile], F32)
            y = sb(es, "ry_" + tag, [128, 32], F32)
            ki = sb(es, "rk_" + tag, [128, 32], I32)
            kf = sb(es, "rkf_" + tag, [128, 32], F32)
            m = sb(es, "rm_" + tag, [128, 32], F32)
            for tt in range(ntile):
                P.op("sp", lambda e, tt=tt: e.dma_start(out=pi_[:, tt:tt + 1], in_=posrows(tt)), [], [pi_.b], dma=True)
            P.op("dve", lambda e: e.tensor_copy(out=pf[:], in_=pi_[:]), [pi_.b], [pf.b])
            for tt in range(ntile):
                for which, off in ((0, 0.25), (1, 0.0)):
                    P.op("dve", lambda e, tt=tt, off=off: e.tensor_scalar(
                        out=y[:], in0=cst[:, C_ROPE:C_ROPE + 32], scalar1=pf[:, tt:tt + 1], scalar2=off,
                        op0=ALU.mult, op1=ALU.add), [cst.b, pf.b], [y.b])
                    P.op("dve", lambda e: e.tensor_copy(out=ki[:], in_=y[:]), [y.b], [ki.b])
                    P.op("dve", lambda e: e.tensor_copy(out=kf[:], in_=ki[:]), [ki.b], [kf.b])
                    P.op("dve", lambda e: e.tensor_tensor(out=y[:], in0=y[:], in1=kf[:], op=ALU.subtract), [y.b, kf.b], [y.b])
                    P.op("dve", lambda e: e.tensor_scalar(out=m[:], in0=y[:], scalar1=0.5, scalar2=None, op0=ALU.is_gt),
                         [y.b], [m.b])
                    P.op("dve", lambda e: e.tensor_tensor(out=y[:], in0=y[:], in1=m[:], op=ALU.subtract), [y.b, m.b], [y.b])
                    P.op("dve", lambda e: e.tensor_scalar(out=m[:], in0=y[:], scalar1=-0.5, scalar2=None, op0=ALU.is_lt),
                         [y.b], [m.b])
                    P.op("dve", lambda e: e.tensor_tensor(out=y[:], in0=y[:], in1=m[:], op=ALU.add), [y.b, m.b], [y.b])
                    P.op("act", lambda e, tt=tt, which=which: e.activation(
                        out=cs[:, tt, which, :], in_=y[:], func=AF.Sin, scale=2.0 * np.pi), [y.b], [cs.b])
            return cs

        def apply_rope(es, src, dst, cs, tt, nh, tag):
            t1 = sb(es, "rt1_" + tag, [128, nh, 32], F32)
            t2 = sb(es, "rt2_" + tag, [128, nh, 32], F32)
            cosb = cs[:, tt, 0, :].unsqueeze(1).to_broadcast([128, nh, 32])
            sinb = cs[:, tt, 1, :].unsqueeze(1).to_broadcast([128, nh, 32])
            x1 = src[0][:, :, 0:32]
            x2 = src[0][:, :, 32:64]
            sbuf_src = src[1]
            P.op("dve", lambda e: e.tensor_tensor(out=t1[:], in0=x1, in1=cosb, op=ALU.mult), [sbuf_src, cs.b], [t1.b])
            P.op("dve", lambda e: e.tensor_tensor(out=t2[:], in0=x2, in1=sinb, op=ALU.mult), [sbuf_src, cs.b], [t2.b])
            P.op("dve", lambda e: e.tensor_tensor(out=dst[0][:, :, 0:32], in0=t1[:], in1=t2[:], op=ALU.subtract),
                 [t1.b, t2.b], [dst[1]])
            P.op("dve", lambda e: e.tensor_tensor(out=t1[:], in0=x2, in1=cosb, op=ALU.mult), [sbuf_src, cs.b], [t1.b])
            P.op("dve", lambda e: e.tensor_tensor(out=t2[:], in0=x1, in1=sinb, op=ALU.mult), [sbuf_src, cs.b], [t2.b])
            P.op("dve", lambda e: e.tensor_tensor(out=dst[0][:, :, 32:64], in0=t1[:], in1=t2[:], op=ALU.add),
                 [t1.b, t2.b], [dst[1]])


        w_in_v = w_in_b.rearrange("(c p) n -> p c n", p=128)

        def stage_norm(rows, ntile, gcol, dstT, tag):
            with contextlib.ExitStack() as es:
                norm_T(es, rows, ntile, gcol, dstT, tag)
                P.emit()

        def sweep_mla(g, nT, es, wb):
            if True:
                ckv = sb(es, "ckv", [128, NT, 512], F32)
                kr = sb(es, "kr", [128, NT, 1, 64], F32)

                def ev_ckv(ci, tt, pbk, csz):
                    if ci == 0:
                        P.op("act", lambda e: e.copy(out=ckv[:, tt, :], in_=pbk[:, 0:512]), [pbk.b], [ckv.b])
                    else:
                        P.op("act", lambda e: e.copy(out=kr[:, tt, 0, :], in_=pbk[:, 0:64]), [pbk.b], [kr.b])
                linear(wb, nT, 32, lambda ci: (w_in_v[:, :, 1024:1536] if ci == 0 else w_in_v[:, :, 1536:1600]),
                       [512, 64], NT, ev_ckv)
                junk = sb(es, "junk2", [128, 512], BF16)
                ssq = sb(es, "ssq2", [128, NT], F32)
                for tt in range(NT):
                    P.op("act", lambda e, tt=tt: e.activation(out=junk[:], in_=ckv[:, tt, :], func=AF.Square,
                                                              accum_out=ssq[:, tt:tt + 1]), [ckv.b], [junk.b, ssq.b])
                rstd = rstd_from_ssq(es, ssq, 512, "kv")
                ckvs = sb(es, "ckvs", [128, 512], BF16)
                ckvnT = sb(es, "ckvnT", [128, 4, TG], BF16)
                for tt in range(NT):
                    P.op("dve", lambda e, tt=tt: e.tensor_scalar(out=ckvs[:], in0=ckv[:, tt, :], scalar1=rstd[:, tt:tt + 1],
                                                                 scalar2=None, op0=ALU.mult), [ckv.b, rstd.b], [ckvs.b])
                    transposes(Src(ckvs, lambda kc: ckvs[:, kc * 128:(kc + 1) * 128]), 4, ckvnT, tt, V_KV)
                cs = rope_tables(es, lambda tt: pos_all[g * TG + tt * 128: g * TG + (tt + 1) * 128, :], NT, "sw")
                krr = sb(es, "krr", [128, 2, 1, 64], BF16)
                krT = sb(es, "krT", [128, 1, TG], BF16)
                for tt in range(NT):
                    apply_rope(es, (kr[:, tt], kr.b), (krr[:, 0], krr.b), cs, tt, 1, "k%d" % tt)
                    P.op("dve", lambda e: e.tensor_copy(out=krr[:, 1], in_=krr[:, 0]), [krr.b], [krr.b])
                    transposes(Src(krr, lambda kc: krr[:].rearrange("p a b c -> p (a b c)")), 1, krT, tt)
                P.op("sp", lambda e: e.dma_start(out=KrS[:, g * TG:(g + 1) * TG], in_=krT[:, 0, :]), [krT.b], [Buf("KrS")], dma=True)
                wkv = wb[1]
                wkv_f = wkv[:].rearrange("p a b -> p (a b)").rearrange("p (c n) -> p c n", c=4)
                P.op("pool", lambda e: e.dma_start(out=wkv_f, in_=w_ukv_b.rearrange("(c p) n -> p c n", p=128)), [], [wkv.b], dma=True)
                wkv_h = wkv_f.rearrange("p c (h t d) -> p c h t d", t=2, d=128)
                kout = sb(es, "kout", [128, 16, TG], BF16)
                for h in range(16):
                    pbk = bank()
                    for c in range(4):
                        P.op("pe", lambda e, c=c, h=h, pbk=pbk: e.matmul(pbk[:, 0:TG], lhsT=wkv_h[:, c, h, 0, :], rhs=ckvnT[:, c, :],
                                                                        start=(c == 0), stop=(c == 3)), [wkv.b, ckvnT.b], [pbk.b])
                    P.op("act", lambda e, h=h, pbk=pbk: e.copy(out=kout[:, h, :], in_=pbk[:, 0:TG]), [pbk.b], [kout.b])
                P.op("sp", lambda e: e.dma_start(out=KnS[:, :, g * TG:(g + 1) * TG].rearrange("h p s -> p h s"), in_=kout[:]),
                     [kout.b], [Buf("KnS")], dma=True)
                vout = sb(es, "vout", [128, NT, 16, 130], BF16)
                P.op("dve", lambda e: e.memset(vout[:], 1.0), [], [vout.b])
                for tt in range(NT):
                    for hb in range(4):
                        pbk = bank()
                        for c in range(4):
                            P.op("pe", lambda e, c=c, hb=hb, tt=tt, pbk=pbk: e.matmul(
                                pbk[:, 0:512].rearrange("p (h d) -> p h d", d=128), lhsT=ckvnT[:, c, tt * 128:(tt + 1) * 128],
                                rhs=wkv_h[:, c, hb * 4:(hb + 1) * 4, 1, :], start=(c == 0), stop=(c == 3)), [wkv.b, ckvnT.b], [pbk.b])
                        P.op("act", lambda e, hb=hb, tt=tt, pbk=pbk: e.copy(
                            out=vout[:, tt, hb * 4:(hb + 1) * 4, 0:128], in_=pbk[:, 0:512].rearrange("p (h d) -> p h d", d=128)),
                            [pbk.b], [vout.b])
                vsb = Buf("VS")
                for tt in range(NT):
                    P.op("sp", lambda e, tt=tt: e.dma_start(out=VS[:, :, g * NT + tt, :].rearrange("h p e -> p h e"), in_=vout[:, tt, :, :]),
                         [vout.b], [vsb], dma=True, nowaw=True)

        def gla_alloc(es, own):
            r = {"gk": sb(es, "gk", [128, NT, 1024], F32), "gv": sb(es, "gv", [128, NT, 2048], BF16)}
            if own:
                r["gq"] = sb(es, "gq", [128, NT, 1024], F32)
                r["sog"] = sb(es, "sog", [128, NT, 2048], BF16)
            return r

        def gla_proj(r, wb, nT, own):
            gk, gv = r["gk"], r["gv"]

            def ev(ci, tt, pbk, csz):
                if own and ci < 2:
                    P.op("act", lambda e: e.copy(out=r["gq"][:, tt, ci * 512:(ci + 1) * 512], in_=pbk[:, 0:512]), [pbk.b], [r["gq"].b])
                    return
                c2 = ci - (2 if own else 0)
                if c2 < 2:
                    P.op("act", lambda e: e.copy(out=gk[:, tt, c2 * 512:(c2 + 1) * 512], in_=pbk[:, 0:512]), [pbk.b], [gk.b])
                elif c2 < 6:
                    P.op("act", lambda e: e.copy(out=gv[:, tt, (c2 - 2) * 512:(c2 - 1) * 512], in_=pbk[:, 0:512]), [pbk.b], [gv.b])
                else:
                    P.op("act", lambda e: e.activation(out=r["sog"][:, tt, (c2 - 6) * 512:(c2 - 5) * 512], in_=pbk[:, 0:512],
                                                       func=AF.Silu), [pbk.b], [r["sog"].b])
            c0 = 1600 if own else 2624
            nblk = (8 if own else 6)
            blocks = [512] * nblk
            if own:
                def wap(ci):
                    if ci < 8:
                        return w_in_v[:, :, 1600 + ci * 512:1600 + (ci + 1) * 512]
                    return w_in_v[:, :, 5712 + (ci - 8) * 512:5712 + (ci - 7) * 512]
                linear(wb, nT, 32, wap, [512] * 12, NT, ev)
            else:
                linear(wb, nT, 32, lambda ci: w_in_v[:, :, 2624 + ci * 512:2624 + (ci + 1) * 512], [512] * 6, NT, ev)
            w = wb[0]
            P.op("pool", lambda e: e.dma_start(out=w[:, 0:32, 0:16], in_=w_in_v[:, :, 5696:5712]), [], [w.b], dma=True)
            pbk = bank()
            for kc in range(32):
                P.op("pe", lambda e, kc=kc: e.matmul(pbk[0:16, 0:TG], lhsT=w[:, kc, 0:16], rhs=nT[:, kc, :],
                                                     start=(kc == 0), stop=(kc == 31)), [w.b, nT.b], [pbk.b])
            P.op("act", lambda e: e.copy(out=glrT[0:16, :], in_=pbk[0:16, 0:TG]), [pbk.b], [glrT.b])
            return r

        def gla_tile(es, tt, r, st, own, tag):
            gk, gv = r["gk"], r["gv"]
            lp = sb(es, "lp" + tag, [128, 1024], F32)
            for hf in range(2):
                pbk = bank()
                P.op("pe", lambda e, hf=hf, pbk=pbk: e.matmul(pbk[:, 0:512], lhsT=glrT[0:32, tt * 128:(tt + 1) * 128],
                                                             rhs=wg[0:32, hf * 512:(hf + 1) * 512], start=True, stop=True),
                     [glrT.b, wg.b], [pbk.b])
                P.op("act", lambda e, hf=hf, pbk=pbk: e.activation(out=lp[:, hf * 512:(hf + 1) * 512], in_=pbk[:, 0:512],
                                                                  func=AF.Exp, scale=-1.0), [pbk.b], [lp.b])
            P.op("act", lambda e: e.activation(out=lp[:], in_=lp[:], func=AF.Ln, bias=1.0), [lp.b], [lp.b])
            e1 = sb(es, "e1" + tag, [128, 1024], F32)
            kd = sb(es, "kd" + tag, [128, 1024], BF16)
            for hf in range(2):
                pbk = bank()
                P.op("pe", lambda e, hf=hf, pbk=pbk: e.matmul(pbk[:, 0:512], lhsT=cst[:, C_M1:C_M1 + 128],
                                                             rhs=lp[:, hf * 512:(hf + 1) * 512], start=True, stop=True),
                     [cst.b, lp.b], [pbk.b])
                P.op("act", lambda e, hf=hf, pbk=pbk: e.activation(out=e1[:, hf * 512:(hf + 1) * 512], in_=pbk[:, 0:512],
                                                                  func=AF.Exp), [pbk.b], [e1.b])
            P.op("dve", lambda e: e.tensor_tensor(out=kd[:], in0=gk[:, tt, :], in1=e1[:], op=ALU.mult), [gk.b, e1.b], [kd.b])
            decT = sb(es, "dec" + tag, [128, 16], F32)
            pbd = bank()
            for dc in range(8):
                P.op("pe", lambda e, dc=dc: e.matmul(pbd[:, dc * 2:dc * 2 + 2], lhsT=lp[:, dc * 128:(dc + 1) * 128],
                                                     rhs=cst[:, C_CIND:C_CIND + 2], start=True, stop=True), [lp.b, cst.b], [pbd.b])
            P.op("act", lambda e: e.activation(out=decT[:], in_=pbd[:, 0:16], func=AF.Exp), [pbd.b], [decT.b])
            if own is not None:
                ostbf, mixedT, sog, gq = own["ostbf"], own["mixedT"], r["sog"], r["gq"]
                eb = sb(es, "eb" + tag, [128, 1024], F32)
                enb = sb(es, "enb" + tag, [128, 1024], F32)
                for hf in range(2):
                    pbk = bank()
                    P.op("pe", lambda e, hf=hf, pbk=pbk: e.matmul(pbk[:, 0:512], lhsT=cst[:, C_TRI:C_TRI + 128],
                                                                 rhs=lp[:, hf * 512:(hf + 1) * 512], start=True, stop=True),
                         [cst.b, lp.b], [pbk.b])
                    P.op("act", lambda e, hf=hf, pbk=pbk: e.activation(out=eb[:, hf * 512:(hf + 1) * 512], in_=pbk[:, 0:512],
                                                                      func=AF.Exp), [pbk.b], [eb.b])
                    P.op("act", lambda e, hf=hf, pbk=pbk: e.activation(out=enb[:, hf * 512:(hf + 1) * 512], in_=pbk[:, 0:512],
                                                                      func=AF.Exp, scale=-1.0), [pbk.b], [enb.b])
                qe = sb(es, "qe" + tag, [128, 1024], BF16)
                ke = sb(es, "ke" + tag, [128, 1024], BF16)
                P.op("dve", lambda e: e.scalar_tensor_tensor(out=qe[:], in0=gq[:, tt, :], scalar=1.0 / 16, in1=eb[:],
                                                             op0=ALU.mult, op1=ALU.mult), [gq.b, eb.b], [qe.b])
                P.op("dve", lambda e: e.tensor_tensor(out=ke[:], in0=gk[:, tt, :], in1=enb[:], op=ALU.mult), [gk.b, enb.b], [ke.b])
                qeT = sb(es, "qeT" + tag, [128, 8, 128], BF16)
                keT = sb(es, "keT" + tag, [128, 8, 128], BF16)
                transposes(Src(qe, lambda kc: qe[:, kc * 128:(kc + 1) * 128]), 8, qeT, 0)
                transposes(Src(ke, lambda kc: ke[:, kc * 128:(kc + 1) * 128]), 8, keT, 0)
                qz = [sb(es, "qz%d" % n + tag, [128, 8, 128], BF16) for n in range(2)]
                for n in range(2):
                    P.op("dve", lambda e, n=n: e.memset(qz[n][:], 0.0), [], [qz[n].b])
                    P.op("dve", lambda e, n=n: e.tensor_copy(out=qz[n][:, :, n * 64:(n + 1) * 64], in_=qeT[:, :, n * 64:(n + 1) * 64]),
                         [qeT.b], [qz[n].b])
                attT = sb(es, "attT" + tag, [128, 4, 128], BF16)
                for h in range(4):
                    pa = bank()
                    for dc in range(2):
                        P.op("pe", lambda e, h=h, dc=dc, pa=pa: e.matmul(pa[:, 0:128], lhsT=keT[:, h * 2 + dc, :], rhs=qeT[:, h * 2 + dc, :],
                                                                        start=(dc == 0), stop=(dc == 1)), [keT.b, qeT.b], [pa.b])
                    P.op("dve", lambda e, h=h, pa=pa: e.tensor_tensor(out=attT[:, h, :], in0=pa[:, 0:128], in1=cst[:, C_CM:C_CM + 128],
                                                                     op=ALU.mult), [pa.b, cst.b], [attT.b])
                po = [pb[h] for h in range(4)]
                for h in range(4):
                    P.op("pe", lambda e, h=h: e.matmul(po[h][:, 0:512], lhsT=attT[:, h, :], rhs=gv[:, tt, h * 512:(h + 1) * 512],
                                                       start=True, stop=False), [attT.b, gv.b], [po[h].b])
            for n in range(2):
                if own is not None:
                    for h in range(4):
                        for dc in range(2):
                            P.op("pe", lambda e, h=h, dc=dc, n=n: e.matmul(
                                po[h][:, 0:512], lhsT=qz[n][:, h * 2 + dc, :], rhs=ostbf[:, h * 2 + dc, :],
                                start=False, stop=(n == 1 and dc == 1)), [qz[n].b, ostbf.b], [po[h].b])
                for hd in range(8):
                    h, dc = divmod(hd, 2)
                    pbk = bank()
                    P.op("pe", lambda e, h=h, dc=dc, n=n, pbk=pbk: e.matmul(
                        pbk[:, 0:512], lhsT=kd[n * 64:(n + 1) * 64, h * 256 + dc * 128:h * 256 + (dc + 1) * 128],
                        rhs=gv[n * 64:(n + 1) * 64, tt, h * 512:(h + 1) * 512], start=True, stop=True), [kd.b, gv.b], [pbk.b])
                    P.op("dve", lambda e, hd=hd, n=n, pbk=pbk: e.scalar_tensor_tensor(
                        out=st[:, hd, :], in0=st[:, hd, :], scalar=decT[:, hd * 2 + n:hd * 2 + n + 1], in1=pbk[:, 0:512],
                        op0=ALU.mult, op1=ALU.add), [st.b, decT.b, pbk.b], [st.b])
                if own is not None:
                    P.op("act", lambda e: e.copy(out=ostbf[:].rearrange("p a b -> p (a b)"), in_=st[:].rearrange("p a b -> p (a b)")),
                         [st.b], [ostbf.b])
            if own is not None:
                junk = sb(es, "gj" + tag, [128, 512], BF16)
                ssq = sb(es, "gss" + tag, [128, 4], F32)
                for h in range(4):
                    P.op("act", lambda e, h=h: e.activation(out=junk[:], in_=po[h][:, 0:512], func=AF.Square,
                                                            accum_out=ssq[:, h:h + 1]), [po[h].b], [junk.b, ssq.b])
                rstd = rstd_from_ssq(es, ssq, 512, "g" + tag)
                omix = sb(es, "omix" + tag, [128, 2048], BF16)
                for h in range(4):
                    P.op("dve", lambda e, h=h: e.scalar_tensor_tensor(
                        out=omix[:, h * 512:(h + 1) * 512], in0=po[h][:, 0:512], scalar=rstd[:, h:h + 1],
                        in1=sog[:, tt, h * 512:(h + 1) * 512], op0=ALU.mult, op1=ALU.mult), [po[h].b, rstd.b, sog.b], [omix.b])
                for h in range(4):
                    transposes(Src(omix, lambda kc, h=h: omix[:, h * 512 + kc * 128:h * 512 + (kc + 1) * 128]), 4, mixedT, tt,
                               V_GO, kc0=16 + h * 4)

        def sweep_gla(g, nT):
            with contextlib.ExitStack() as es:
                wb = [sb(es, "wb%d" % i, [128, 32, 512], BF16) for i in range(2)]
                sweep_mla(g, nT, es, wb)
                rr = g % 4
                if rr == 0:
                    P.op("dve", lambda e: e.tensor_scalar(out=snap[:], in0=state[:], scalar1=corec[:, 0:1], scalar2=None,
                                                          op0=ALU.mult), [state.b, corec.b], [snap.b])
                else:
                    P.op("dve", lambda e: e.scalar_tensor_tensor(out=snap[:].rearrange("p a b -> p (a b)"),
                                                                 in0=state[:].rearrange("p a b -> p (a b)"),
                                                                 scalar=corec[:, rr:rr + 1], in1=snap[:].rearrange("p a b -> p (a b)"),
                                                                 op0=ALU.mult, op1=ALU.add), [state.b, corec.b, snap.b], [snap.b])
                r = gla_alloc(es, False)
                gla_proj(r, wb, nT, False)
                for tt in range(NT):
                    gla_tile(es, tt, r, state, None, "s%d" % tt)
                P.emit()

        def own_mla(i, nT, mixedT):
            with contextlib.ExitStack() as oes:
                qnT = sb(oes, "qnT", [128, 16, TG], BF16)
                qrT = sb(oes, "qrT", [128, 8, TG], BF16)
                cqnT = sb(oes, "cqnT", [128, 8, TG], BF16)
                with contextlib.ExitStack() as es:
                    wb = [sb(es, "wb%d" % k, [128, 32, 512], BF16) for k in range(2)]
                    cq = sb(es, "cq", [128, NT, 1024], F32)

                    def ev(ci, tt, pbk, csz):
                        P.op("act", lambda e: e.copy(out=cq[:, tt, ci * 512:(ci + 1) * 512], in_=pbk[:, 0:512]), [pbk.b], [cq.b])
                    linear(wb, nT, 32, lambda ci: w_in_v[:, :, ci * 512:(ci + 1) * 512], [512, 512], NT, ev)
                    junk = sb(es, "junk3", [128, 1024], BF16)
                    ssq = sb(es, "ssq3", [128, NT], F32)
                    for tt in range(NT):
                        P.op("act", lambda e, tt=tt: e.activation(out=junk[:], in_=cq[:, tt, :], func=AF.Square,
                                                                  accum_out=ssq[:, tt:tt + 1]), [cq.b], [junk.b, ssq.b])
                    rstd = rstd_from_ssq(es, ssq, 1024, "q")
                    for tt in range(NT):
                        P.op("dve", lambda e, tt=tt: e.tensor_scalar(out=junk[:], in0=cq[:, tt, :], scalar1=rstd[:, tt:tt + 1],
                                                                     scalar2=None, op0=ALU.mult), [cq.b, rstd.b], [junk.b])
                        transposes(Src(junk, lambda kc: junk[:, kc * 128:(kc + 1) * 128]), 8, cqnT, tt, V_Q)
                    wq_v = w_uq_b.rearrange("(c p) (h e) -> p c h e", p=128, e=192)

                    def wap_n(ci):
                        return wq_v[:, :, ci * 4:(ci + 1) * 4, 0:128]

                    def ev_n(cb, pbk):
                        P.op("act", lambda e: e.copy(out=qnT[:, cb, :], in_=pbk[:, 0:TG]), [pbk.b], [qnT.b])
                    for ci in range(4):
                        wrot["i"] ^= 1
                        w = wb[wrot["i"]]
                        for hh in range(4):
                            P.op("pool", lambda e, w=w, ci=ci, hh=hh: e.dma_start(
                                out=w[:, 0:8, hh * 128:(hh + 1) * 128], in_=wq_v[:, :, ci * 4 + hh, 0:128]), [], [w.b],
                                dma=True, nowaw=True)
                        for cb in range(4):
                            pbk = bank()
                            for kc in range(8):
                                P.op("pe", lambda e, kc=kc, cb=cb, w=w, pbk=pbk: e.matmul(
                                    pbk[:, 0:TG], lhsT=w[:, kc, cb * 128:(cb + 1) * 128], rhs=cqnT[:, kc, :],
                                    start=(kc == 0), stop=(kc == 7)), [cqnT.b, w.b], [pbk.b])
                            ev_n(ci * 4 + cb, pbk)
                    qr = sb(es, "qr", [128, NT, 16, 64], F32)
                    for ci in range(2):
                        wrot["i"] ^= 1
                        w = wb[wrot["i"]]
                        for hh in range(8):
                            P.op("pool", lambda e, w=w, ci=ci, hh=hh: e.dma_start(
                                out=w[:, 0:8, hh * 64:(hh + 1) * 64], in_=wq_v[:, :, ci * 8 + hh, 128:192]), [], [w.b],
                                dma=True, nowaw=True)
                        for tt in range(NT):
                            pbk = bank()
                            for kc in range(8):
                                P.op("pe", lambda e, kc=kc, tt=tt, w=w, pbk=pbk: e.matmul(
                                    pbk[:, 0:512], lhsT=cqnT[:, kc, tt * 128:(tt + 1) * 128], rhs=w[:, kc, 0:512],
                                    start=(kc == 0), stop=(kc == 7)), [cqnT.b, w.b], [pbk.b])
                            P.op("act", lambda e, tt=tt, ci=ci, pbk=pbk: e.copy(
                                out=qr[:, tt, ci * 8:(ci + 1) * 8, :], in_=pbk[:, 0:512].rearrange("p (h e) -> p h e", e=64)),
                                [pbk.b], [qr.b])
                    cs = rope_tables(es, lambda tt: pos_own[i * TG + tt * 128: i * TG + (tt + 1) * 128, :], NT, "ow")
                    qrr = sb(es, "qrr", [128, 16, 64], BF16)
                    for tt in range(NT):
                        apply_rope(es, (qr[:, tt], qr.b), (qrr[:], qrr.b), cs, tt, 16, "q%d" % tt)
                        transposes(Src(qrr, lambda kc: qrr[:, kc * 2:(kc + 1) * 2, :].rearrange("p a b -> p (a b)")), 8, qrT, tt)
                    P.emit()
                with contextlib.ExitStack() as es:
                    nk = (4 * i + 4) * TG
                    nkc = nk // 128
                    krT2 = sb(es, "krT2", [128, nk], BF16)
                    P.op("sp", lambda e: e.dma_start(out=krT2[:], in_=KrS[:, 0:nk]), [], [krT2.b], dma=True)
                    SEG = nk if nk <= 4096 else nk // 2
                    nseg = nk // SEG
                    skc = SEG // 128
                    knT = [sb(es, "knT%d" % k, [128, SEG], BF16) for k in range(2)]
                    vaug = [sb(es, "vaug%d" % k, [128, skc, 130], BF16) for k in range(2)]
                    pT = [sb(es, "pT%d" % k, [128, 2 * TG], BF16) for k in range(2)]
                    omla = sb(es, "omla", [128, NT, 2048], F32)
                    rs = sb(es, "rsum", [128, NT], F32)
                    bank_lo[0] = 1
                    po = pb[0]
                    pov = po[:, 0:NT * 130].rearrange("p (a b) -> p a b", b=130)
                    sc = 192.0 ** -0.5
                    bcnt = 0
                    pcnt = 0
                    for h in range(16):
                        half = (h % 2) * 64
                        for sg in range(nseg):
                            kt, va = knT[bcnt % 2], vaug[bcnt % 2]
                            bcnt += 1
                            k0 = sg * SEG
                            P.op("sp", lambda e, kt=kt, h=h, k0=k0: e.dma_start(out=kt[:], in_=KnS[h, :, k0:k0 + SEG]), [], [kt.b], dma=True)
                            P.op("sp", lambda e, va=va, h=h, sg=sg: e.dma_start(out=va[:], in_=VS[h, :, sg * skc:(sg + 1) * skc, :]),
                                 [], [va.b], dma=True)
                            for kl2 in range(0, skc, 2):
                                pS = bank()
                                p = pT[pcnt % 2]
                                pcnt += 1
                                for sub in range(2):
                                    kl = kl2 + sub
                                    kc = sg * skc + kl
                                    o_ = pS[:, sub * TG:(sub + 1) * TG]
                                    masked = kc >= 4 * i * NT
                                    P.op("pe", lambda e, kl=kl, kt=kt, h=h, o_=o_, sub=sub: e.matmul(
                                        o_, lhsT=kt[:, kl * 128:(kl + 1) * 128], rhs=qnT[:, h, :], start=(sub == 0), stop=False,
                                        skip_group_check=True), [kt.b, qnT.b], [pS.b])
                                    P.op("pe", lambda e, kc=kc, h=h, o_=o_, half=half, masked=masked: e.matmul(
                                        o_, lhsT=krT2[half:half + 64, kc * 128:(kc + 1) * 128], rhs=qrT[half:half + 64, h // 2, :],
                                        start=False, stop=(not masked), skip_group_check=True), [krT2.b, qrT.b], [pS.b])
                                    if masked:
                                        rr = kc // NT - 4 * i
                                        kcin = kc % NT
                                        P.op("pe", lambda e, rr=rr, o_=o_: e.matmul(o_, lhsT=sidb[:, rr, :], rhs=negfull[:, :],
                                                                                    start=False, stop=False, skip_group_check=True),
                                             [sidb.b, negfull.b], [pS.b])
                                        P.op("pe", lambda e, rr=rr, kcin=kcin, o_=o_: e.matmul(
                                            o_, lhsT=sidb[:, 4 + rr, :], rhs=maskdiag[:, kcin, :], start=False, stop=True,
                                            skip_group_check=True), [sidb.b, maskdiag.b], [pS.b])
                                P.op("act", lambda e, p=p, pS=pS: e.activation(out=p[:], in_=pS[:, 0:2 * TG], func=AF.Exp, scale=sc),
                                     [pS.b], [p.b])
                                for sub in range(2):
                                    kl = kl2 + sub
                                    kc = sg * skc + kl
                                    for qb in range(NT):
                                        P.op("pe", lambda e, p=p, qb=qb, kc=kc, kl=kl, va=va, sub=sub: e.matmul(
                                            pov[:, qb, 0:129], lhsT=p[:, sub * TG + qb * 128:sub * TG + (qb + 1) * 128], rhs=va[:, kl, 0:129],
                                            start=(kc == 0 and qb == 0), stop=(kc == nkc - 1), skip_group_check=True), [p.b, va.b], [po.b])
                        for qb in range(NT):
                            P.op("dve", lambda e, qb=qb: e.reciprocal(out=rs[:, qb:qb + 1], in_=pov[:, qb, 128:129]), [po.b], [rs.b])
                            P.op("dve", lambda e, qb=qb, h=h: e.tensor_scalar(
                                out=omla[:, qb, h * 128:(h + 1) * 128], in0=pov[:, qb, 0:128], scalar1=rs[:, qb:qb + 1], scalar2=None,
                                op0=ALU.mult), [po.b, rs.b], [omla.b])
                    bank_lo[0] = 0
                    junk = sb(es, "junk4", [128, 2048], BF16)
                    ssq = sb(es, "ssq4", [128, NT], F32)
                    for tt in range(NT):
                        P.op("act", lambda e, tt=tt: e.activation(out=junk[:], in_=omla[:, tt, :], func=AF.Square,
                                                                  accum_out=ssq[:, tt:tt + 1]), [omla.b], [junk.b, ssq.b])
                    rstd = rstd_from_ssq(es, ssq, 2048, "mo")
                    for tt in range(NT):
                        P.op("dve", lambda e, tt=tt: e.tensor_scalar(out=junk[:], in0=omla[:, tt, :], scalar1=rstd[:, tt:tt + 1],
                                                                     scalar2=None, op0=ALU.mult), [omla.b, rstd.b], [junk.b])
                        transposes(Src(junk, lambda kc: junk[:, kc * 128:(kc + 1) * 128]), 16, mixedT, tt, V_MO)
                    P.emit()

        def own_gla(i, nT, mixedT):
            with contextlib.ExitStack() as oes:
                r = gla_alloc(oes, True)
                with contextlib.ExitStack() as es:
                    wb = [sb(es, "wb%d" % k, [128, 32, 512], BF16) for k in range(2)]
                    gla_proj(r, wb, nT, True)
                    P.emit()
                ost = sb(oes, "ost", [128, 8, 512], F32)
                ostbf = sb(oes, "ostbf", [128, 8, 512], BF16)
                bank_lo[0] = 4
                for tt in range(NT):
                    with contextlib.ExitStack() as es:
                        if tt == 0:
                            P.op("dve", lambda e: e.tensor_copy(out=ost[:].rearrange("p a b -> p (a b)"),
                                                                in_=snap[:].rearrange("p a b -> p (a b)")), [snap.b], [ost.b])
                            P.op("act", lambda e: e.copy(out=ostbf[:].rearrange("p a b -> p (a b)"),
                                                         in_=snap[:].rearrange("p a b -> p (a b)")), [snap.b], [ostbf.b])
                        gla_tile(es, tt, r, ost, {"ostbf": ostbf, "mixedT": mixedT}, "o%d" % tt)
                        P.emit()
                bank_lo[0] = 0

        def linear_res(wb, es, actT, KC, wdram, src, dst, tagn):
            rb = [sb(es, "rb%d" % k, [128, 512], F32) for k in range(2)]
            ob = [sb(es, "ob%d" % k, [128, 512], F32) for k in range(2)]
            dstb = Buf(tagn)
            cnt = [0]

            def ev(ci, tt, pbk, csz):
                k = cnt[0] % 2
                cnt[0] += 1
                P.op("sp", lambda e: e.dma_start(out=rb[k][:], in_=src[tt * 128:(tt + 1) * 128, ci * 512:(ci + 1) * 512]),
                     [], [rb[k].b], dma=True)
                P.op("dve", lambda e: e.tensor_tensor(out=ob[k][:], in0=pbk[:, 0:512], in1=rb[k][:], op=ALU.add),
                     [pbk.b, rb[k].b], [ob[k].b])
                P.op("sp", lambda e: e.dma_start(out=dst[tt * 128:(tt + 1) * 128, ci * 512:(ci + 1) * 512], in_=ob[k][:]),
                     [ob[k].b], [dstb], dma=True, nowaw=True)
            linear(wb, actT, KC, wcols(wdram, 0), [512] * 8, NT, ev)

        def own_wout(i, mixedT):
            with contextlib.ExitStack() as es:
                wb = [sb(es, "wb%d" % k, [128, 32, 512], BF16) for k in range(2)]
                linear_res(wb, es, mixedT, 32, w_out_b, x_own[i * TG:(i + 1) * TG, :], hA, "hA")
                P.emit()

        def own_cross(i):
            with contextlib.ExitStack() as oes:
                nT2 = sb(oes, "nT2", [128, 32, TG], BF16)
                stage_norm(lambda tt: hA[tt * 128:(tt + 1) * 128, :], NT, V_CROSS, nT2, "cr")
                ocT = sb(oes, "ocT", [128, 8, TG], BF16)
                with contextlib.ExitStack() as es:
                    wb = [sb(es, "wb%d" % k, [128, 32, 512], BF16) for k in range(2)]
                    qcT = sb(es, "qcT", [128, 8, TG], BF16)

                    def ev_q(cb, pbk):
                        P.op("act", lambda e: e.copy(out=qcT[:, cb, :], in_=pbk[:, 0:TG]), [pbk.b], [qcT.b])
                    linearT(wb, nT2, 32, wcols(w_cq_b, 0), 2, TG, ev_q)
                    pT2 = sb(es, "pT2", [128, 2, TG], BF16)
                    oc = sb(es, "oc", [128, NT, 1024], BF16)
                    rs = sb(es, "rs2", [128, 1], F32)
                    for h in range(4):
                        for mc in range(2):
                            pS = bank()
                            for hf in range(2):
                                P.op("pe", lambda e, h=h, mc=mc, hf=hf, pS=pS: e.matmul(
                                    pS[:, 0:TG], lhsT=KmT[:, h * 2 + hf, mc * 128:(mc + 1) * 128], rhs=qcT[:, h * 2 + hf, :],
                                    start=(hf == 0), stop=(hf == 1)), [KmT.b, qcT.b], [pS.b])
                            P.op("act", lambda e, mc=mc, pS=pS: e.activation(out=pT2[:, mc, :], in_=pS[:, 0:TG], func=AF.Exp,
                                                                            scale=1.0 / 16), [pS.b], [pT2.b])
                        for tt in range(NT):
                            po = bank()
                            for mc in range(2):
                                P.op("pe", lambda e, h=h, mc=mc, tt=tt, po=po: e.matmul(
                                    po[:, 0:257], lhsT=pT2[:, mc, tt * 128:(tt + 1) * 128], rhs=Vm[:, mc, h, 0:257],
                                    start=(mc == 0), stop=(mc == 1)), [pT2.b, Vm.b], [po.b])
                            P.op("dve", lambda e, po=po: e.reciprocal(out=rs[:], in_=po[:, 256:257]), [po.b], [rs.b])
                            P.op("dve", lambda e, po=po, h=h, tt=tt: e.tensor_scalar(
                                out=oc[:, tt, h * 256:(h + 1) * 256], in0=po[:, 0:256], scalar1=rs[:, 0:1], scalar2=None,
                                op0=ALU.mult), [po.b, rs.b], [oc.b])
                    for tt in range(NT):
                        transposes(Src(oc, lambda kc, tt=tt: oc[:, tt, kc * 128:(kc + 1) * 128]), 8, ocT, tt)
                    P.emit()
                with contextlib.ExitStack() as es:
                    wb = [sb(es, "wb%d" % k, [128, 32, 512], BF16) for k in range(2)]
                    linear_res(wb, es, ocT, 8, w_co_b, hA, hB, "hB")
                    P.emit()

        def own_peer(i):
            with contextlib.ExitStack() as oes:
                x3T = sb(oes, "x3T", [128, 32, TG], BF16)
                stage_norm(lambda tt: hB[tt * 128:(tt + 1) * 128, :], NT, V_FFN, x3T, "pe")
                s2 = sb(oes, "s2", [128, NT, 8, 128], F32)
                thr1 = sb(oes, "thr1", [128, NT, 8, 128], F32)
                w2 = sb(oes, "w2", [128, NT, 8, 128], BF16)
                w1c = sb(oes, "w1c", [128, NT, 8, 128], F32)
                with contextlib.ExitStack() as es:
                    qpT = sb(es, "qpT", [128, 16, TG], BF16)
                    with contextlib.ExitStack() as es2:
                        wb = [sb(es2, "wb%d" % k, [128, 32, 512], BF16) for k in range(2)]

                        def ev_q(cb, pbk):
                            P.op("act", lambda e: e.copy(out=qpT[:, cb, :], in_=pbk[:, 0:TG]), [pbk.b], [qpT.b])
                        linearT(wb, x3T, 32, wcols(w_pq_b, 0), 4, TG, ev_q)
                        P.emit()
                    sc = sb(es, "sc", [128, 16, 128], F32)
                    scr = sb(es, "scr", [128, 16, 128], F32)
                    v = sb(es, "v16", [128, 16, 16], F32)
                    cand = sb(es, "cand", [128, 8, 16, 16], F32)
                    cand2 = sb(es, "cand2", [128, 8, 256], F32)
                    cs_ = sb(es, "cs16", [128, 8, 16], F32)
                    small = sb(es, "small", [128, 8, 8], F32)
                    ejunk = sb(es, "ejunk", [128, 16], F32)
                    for tt in range(NT):
                        for q4 in range(4):
                            pS = bank()
                            for k in range(4):
                                hp = q4 * 4 + k
                                P.op("pe", lambda e, hp=hp, k=k, tt=tt, pS=pS: e.matmul(
                                    pS[:, k * 128:(k + 1) * 128], lhsT=qpT[:, hp, tt * 128:(tt + 1) * 128], rhs=subkT[:, hp, :],
                                    start=True, stop=True), [qpT.b, subkT.b], [pS.b])
                            P.op("act", lambda e, q4=q4, pS=pS: e.copy(out=sc[:, q4 * 4:(q4 + 1) * 4, :].rearrange("p a b -> p (a b)"),
                                                                       in_=pS[:, 0:512]), [pS.b], [sc.b])
                        for hp in range(16):
                            P.op("dve", lambda e, hp=hp: e.max(out=v[:, hp, 0:8], in_=sc[:, hp, :]), [sc.b], [v.b])
                            P.op("dve", lambda e, hp=hp: e.match_replace(out=scr[:, hp, :], in_to_replace=v[:, hp, 0:8],
                                                                         in_values=sc[:, hp, :], imm_value=-1e30), [sc.b, v.b], [scr.b])
                            P.op("dve", lambda e, hp=hp: e.max(out=v[:, hp, 8:16], in_=scr[:, hp, :]), [scr.b], [v.b])
                        vv = v[:].rearrange("p (h t) k -> p h t k", t=2)
                        P.op("dve", lambda e: e.tensor_tensor(
                            out=cand[:], in0=vv[:, :, 0, :].unsqueeze(3).to_broadcast([128, 8, 16, 16]),
                            in1=vv[:, :, 1, :].unsqueeze(2).to_broadcast([128, 8, 16, 16]), op=ALU.add), [v.b], [cand.b])
                        for h in range(8):
                            cf = cand[:, h].rearrange("p a b -> p (a b)")
                            P.op("dve", lambda e, h=h, cf=cf: e.max(out=cs_[:, h, 0:8], in_=cf), [cand.b], [cs_.b])
                            P.op("dve", lambda e, h=h, cf=cf: e.match_replace(out=cand2[:, h, :], in_to_replace=cs_[:, h, 0:8],
                                                                             in_values=cf, imm_value=-1e30), [cand.b, cs_.b], [cand2.b])
                            P.op("dve", lambda e, h=h: e.max(out=cs_[:, h, 8:16], in_=cand2[:, h, :]), [cand2.b], [cs_.b])
                        P.op("dve", lambda e: e.tensor_scalar(out=small[:, :, 0], in0=cs_[:, :, 15], scalar1=-1e-4, scalar2=None,
                                                              op0=ALU.add), [cs_.b], [small.b])
                        P.op("dve", lambda e: e.tensor_scalar(out=small[:, :, 1], in0=cs_[:, :, 0], scalar1=-1.0, scalar2=None,
                                                              op0=ALU.mult), [cs_.b], [small.b])
                        for h in range(8):
                            P.op("act", lambda e, h=h: e.activation(out=ejunk[:], in_=cs_[:, h, :], func=AF.Exp, bias=small[:, h, 1:2],
                                                                    accum_out=small[:, h, 2:3]), [cs_.b, small.b], [ejunk.b, small.b])
                        P.op("dve", lambda e: e.reciprocal(out=small[:, :, 3], in_=small[:, :, 2]), [small.b], [small.b])
                        for h in range(8):
                            P.op("dve", lambda e, h=h, tt=tt: e.tensor_copy(out=s2[:, tt, h, :], in_=sc[:, 2 * h + 1, :]), [sc.b], [s2.b])
                            P.op("act", lambda e, h=h, tt=tt: e.activation(out=w2[:, tt, h, :], in_=sc[:, 2 * h + 1, :], func=AF.Exp,
                                                                          bias=small[:, h, 1:2]), [sc.b, small.b], [w2.b])
                            P.op("act", lambda e, h=h, tt=tt: e.activation(out=scr[:, h, :], in_=sc[:, 2 * h, :], func=AF.Exp),
                                 [sc.b], [scr.b])
                            P.op("dve", lambda e, h=h, tt=tt: e.tensor_scalar(out=w1c[:, tt, h, :], in0=scr[:, h, :],
                                                                             scalar1=small[:, h, 3:4], scalar2=None, op0=ALU.mult),
                                 [scr.b, small.b], [w1c.b])
                            P.op("dve", lambda e, h=h, tt=tt: e.tensor_scalar(out=thr1[:, tt, h, :], in0=sc[:, 2 * h, :], scalar1=-1.0,
                                                                             scalar2=small[:, h, 0:1], op0=ALU.mult, op1=ALU.add),
                                 [sc.b, small.b], [thr1.b])
                    P.emit()
                outacc = sb(oes, "outacc", [128, NT, D], F32)
                with contextlib.ExitStack() as es:
                    ub = [sb(es, "ub%d" % k, [128, 32, 256], BF16) for k in range(2)]
                    vb = [sb(es, "vb%d" % k, [128, 16, 256], BF16) for k in range(2)]
                    NGH = 16
                    ghs = [sb(es, "gh%d" % k, [128, 128], BF16) for k in range(NGH)]
                    dgs = [sb(es, "dg%d" % k, [128, 128], BF16) for k in range(NGH)]
                    gls = [sb(es, "gl%d" % k, [128, TG], F32) for k in range(2)]
                    ATs = [sb(es, "AT%d" % k, [128, 16, TG], BF16) for k in range(2)]
                    puT = peer_uT.rearrange("(c p) n -> p c n", p=128)
                    pvv = peer_v.rearrange("(c p) n -> p c n", p=128)
                    cnts = {"u": 0, "v": 0, "a": 0, "g": 0}
                    ublk = {}
                    pGs = {}

                    puT_f = peer_uT.rearrange("(c p) n -> p c n", p=128)
                    pv_f = peer_v.rearrange("(c p) n -> p c n", p=128)
                    dbu, dbv = Buf("cv_u"), Buf("cv_v")

                    def load_u(blk):
                        w = ub[cnts["u"] % 2]
                        cnts["u"] += 1
                        if i == 0:
                            P.op("pool", lambda e, w=w, blk=blk: e.dma_start(out=w[:], in_=puT_f[:, :, blk * 256:(blk + 1) * 256]),
                                 [], [w.b], dma=True)
                            P.op("sp", lambda e, w=w, blk=blk: e.dma_start(out=Ubf[blk], in_=w[:].rearrange("p c n -> p (c n)")),
                                 [w.b], [dbu], dma=True, nowaw=True, semname="cvout")
                        else:
                            P.op("pool", lambda e, w=w, blk=blk: e.dma_start(
                                out=w[:].rearrange("p c n -> p (c n)"), in_=Ubf[blk]), [], [w.b], dma=True)
                        ublk[blk] = w

                    def g_stuff(idx):
                        pG = pb[idx % 2]
                        pGs[idx] = pG
                        first = True
                        for tt in range(NT):
                            for h in range(8):
                                gh, dg = ghs[cnts["g"] % NGH], dgs[cnts["g"] % NGH]
                                cnts["g"] += 1
                                P.op("dve", lambda e, h=h, tt=tt, gh=gh: e.scalar_tensor_tensor(
                                    out=gh[:], in0=s2[:, tt, h, :], scalar=thr1[:, tt, h, idx:idx + 1], in1=w2[:, tt, h, :],
                                    op0=ALU.is_ge, op1=ALU.mult), [s2.b, thr1.b, w2.b], [gh.b])
                                P.op("act", lambda e, h=h, tt=tt, dg=dg: e.activation(
                                    out=dg[:], in_=identb[:], func=AF.Copy, scale=w1c[:, tt, h, idx:idx + 1]),
                                    [identb.b, w1c.b], [dg.b])
                                P.op("pe", lambda e, h=h, tt=tt, gh=gh, dg=dg, first=first: e.matmul(
                                    pG[:, tt * 128:(tt + 1) * 128], lhsT=gh[:], rhs=dg[:], start=first, stop=(h == 7),
                                    skip_group_check=True), [gh.b, dg.b], [pG.b])
                                first = False

                    def u_mm(idx):
                        blk, sub = divmod(idx, 2)
                        if sub == 0:
                            if blk not in ublk:
                                load_u(blk)
                            if blk + 1 < 64:
                                load_u(blk + 1)
                        w = ublk[blk]
                        pH = bank()
                        for kc in range(32):
                            P.op("pe", lambda e, kc=kc: e.matmul(
                                pH[:, 0:TG], lhsT=w[:, kc, sub * 128:(sub + 1) * 128], rhs=x3T[:, kc, :],
                                start=(kc == 0), stop=(kc == 31)), [x3T.b, w.b], [pH.b])
                        return pH

                    def a_mult(idx, pH):
                        eg, eb_ = divmod(idx, 16)
                        AT = ATs[eg % 2]
                        gl = gls[cnts["a"] % 2]
                        cnts["a"] += 1
                        pG = pGs.pop(idx)
                        P.op("act", lambda e: e.activation(out=gl[:], in_=pH[:, 0:TG], func=AF.Gelu), [pH.b], [gl.b])
                        P.op("dve", lambda e: e.tensor_tensor(out=AT[:, eb_, :], in0=pG[:, 0:TG], in1=gl[:], op=ALU.mult),
                             [gl.b, pG.b], [AT.b])

                    def v_phase(eg):
                        AT = ATs[eg % 2]
                        for db in range(16):
                            w = vb[cnts["v"] % 2]
                            cnts["v"] += 1
                            if i == 0:
                                P.op("pool", lambda e, w=w, db=db: e.dma_start(
                                    out=w[:], in_=pv_f[:, eg * 16:(eg + 1) * 16, db * 256:(db + 1) * 256]), [], [w.b], dma=True)
                                P.op("sp", lambda e, w=w, pc=eg * 16 + db: e.dma_start(
                                    out=Vbf[pc], in_=w[:].rearrange("p c n -> p (c n)")), [w.b], [dbv], dma=True, nowaw=True,
                                    semname="cvout")
                            else:
                                P.op("pool", lambda e, w=w, pc=eg * 16 + db: e.dma_start(
                                    out=w[:].rearrange("p c n -> p (c n)"), in_=Vbf[pc]), [], [w.b], dma=True)
                            for tt in range(NT):
                                pO = bank()
                                for eb_ in range(16):
                                    P.op("pe", lambda e, eb_=eb_, tt=tt, w=w, pO=pO: e.matmul(
                                        pO[:, 0:256], lhsT=AT[:, eb_, tt * 128:(tt + 1) * 128], rhs=w[:, eb_, :],
                                        start=(eb_ == 0), stop=(eb_ == 15)), [AT.b, w.b], [pO.b])
                                o = outacc[:, tt, db * 256:(db + 1) * 256]
                                if eg == 0:
                                    P.op("act", lambda e, o=o, pO=pO: e.copy(out=o, in_=pO[:, 0:256]), [pO.b], [outacc.b])
                                else:
                                    P.op("dve", lambda e, o=o, pO=pO: e.tensor_tensor(out=o, in0=pO[:, 0:256], in1=o, op=ALU.add),
                                         [pO.b, outacc.b], [outacc.b])

                    bank_lo[0] = 2
                    g_stuff(0)
                    for idx in range(128):
                        pH = u_mm(idx)
                        if idx + 1 < 128:
                            g_stuff(idx + 1)
                        a_mult(idx, pH)
                        if idx % 16 == 15:
                            v_phase(idx // 16)
                    bank_lo[0] = 0
                    P.emit()
                with contextlib.ExitStack() as es:
                    gfin = sb(es, "gfin", [128, D], F32)
                    P.op("sp", lambda e: e.dma_start(out=gfin[:], in_=gfin_in.partition_broadcast(128)), [], [gfin.b], dma=True)
                    hb_ = sb(es, "hbt", [128, D], F32)
                    junk = sb(es, "junk5", [128, D], BF16)
                    ssq = sb(es, "ssq5", [128, NT], F32)
                    outb = Buf("out")
                    for tt in range(NT):
                        P.op("sp", lambda e, tt=tt: e.dma_start(out=hb_[:], in_=hB[tt * 128:(tt + 1) * 128, :]), [], [hb_.b], dma=True)
                        P.op("dve", lambda e, tt=tt: e.tensor_tensor(out=outacc[:, tt, :], in0=outacc[:, tt, :], in1=hb_[:], op=ALU.add),
                             [outacc.b, hb_.b], [outacc.b])
                        P.op("act", lambda e, tt=tt: e.activation(out=junk[:], in_=outacc[:, tt, :], func=AF.Square,
                                                                  accum_out=ssq[:, tt:tt + 1]), [outacc.b], [junk.b, ssq.b])
                    rstd = rstd_from_ssq(es, ssq, D, "fin")
                    for tt in range(NT):
                        P.op("dve", lambda e, tt=tt: e.scalar_tensor_tensor(
                            out=outacc[:, tt, :], in0=outacc[:, tt, :], scalar=rstd[:, tt:tt + 1], in1=gfin[:],
                            op0=ALU.mult, op1=ALU.mult), [outacc.b, rstd.b, gfin.b], [outacc.b])
                        P.op("sp", lambda e, tt=tt: e.dma_start(out=out[i * TG + tt * 128: i * TG + (tt + 1) * 128, :], in_=outacc[:, tt, :]),
                             [outacc.b], [outb], dma=True, nowaw=True)
                    P.emit()

        stop_after = dbg if isinstance(dbg, str) else None
        for i in range(NOWN):
            for rr in range(4):
                g = 4 * i + rr
                with contextlib.ExitStack() as gs:
                    nT = sb(gs, "nT", [128, 32, TG], BF16)
                    stage_norm(lambda tt, g=g: x_all[g * TG + tt * 128: g * TG + (tt + 1) * 128, :], NT, V_MIX, nT, "sw")
                    sweep_gla(g, nT)
            with contextlib.ExitStack() as gs:
                mixedT = sb(gs, "mixedT", [128, 32, TG], BF16)
                nT = sb(gs, "nT", [128, 32, TG], BF16)
                stage_norm(lambda tt, i=i: x_own[i * TG + tt * 128: i * TG + (tt + 1) * 128, :], NT, V_MIX, nT, "ow")
                own_mla(i, nT, mixedT)
                own_gla(i, nT, mixedT)
                own_wout(i, mixedT)
            if stop_after == "mixer":
                P.op("sp", lambda e, i=i: e.dma_start(out=out[i * TG:(i + 1) * TG, :], in_=hA), [], [Buf("out")], dma=True)
                P.emit()
                continue
            own_cross(i)
            if stop_after == "cross":
                P.op("sp", lambda e, i=i: e.dma_start(out=out[i * TG:(i + 1) * TG, :], in_=hB), [], [Buf("out")], dma=True)
                P.emit()
                continue
            own_peer(i)
    return nc


def host_consts():
    cst = np.zeros((128, 1024), np.float32)
    cst[:, 0:128] = np.eye(128)
    j = np.arange(128)[:, None]
    c = np.arange(128)[None, :]
    same = (j // 64) == (c // 64)
    cst[:, 128:256] = np.where(same & (j > c), -1.0 / 16, 0.0)
    cst[:, 256:384] = np.where(same & (j <= c), -1.0 / 16, 0.0)
    cst[:, 384:512] = np.where(same & (j <= c), 1.0, 0.0)
    cst[:, 512] = np.where(np.arange(128) < 64, -1.0 / 16, 0.0)
    cst[:, 513] = np.where(np.arange(128) >= 64, -1.0 / 16, 0.0)
    inv = 10000.0 ** (-np.arange(0, 64, 2, dtype=np.float32) / 64)
    cst[:, 514:546] = (inv / (2 * np.pi))[None, :]
    cst[:, 640:768] = np.where(j <= c, 1.0, 0.0)
    return cst


def tvec(g):
    g = np.asarray(g, np.float32).reshape(-1)
    return np.ascontiguousarray(g.reshape(-1, 128).T)


def core_inputs(b, j, S, NOWN, x, mem, positions, norm_mem, norm_mix, w_in, mla_q_norm, w_uq, mla_kv_norm, w_ukv,
                mla_out_norm, w_gate_up, b_gate, gla_out_norm, w_out, norm_cross, w_cq, w_ck, w_cv, w_co,
                norm_ffn, w_peer_q, peer_sub_keys, peer_u, peer_v, norm_final, shared):
    f = lambda a: np.ascontiguousarray(np.asarray(a, np.float32))
    xb = f(x[b])
    own_rows = np.concatenate([np.arange((4 * i + j) * TG, (4 * i + j + 1) * TG) for i in range(NOWN)])
    pos = np.asarray(positions[b], np.int32).reshape(S, 1)
    corec = np.zeros((128, 1028), np.float32)
    corec[:, j] = 1.0
    sid = np.zeros((8, 128, 128), np.float32)
    for r in range(4):
        if r > j:
            sid[r] = np.eye(128)
        if r == j:
            sid[4 + r] = np.eye(128)
    corec[:, 4:1028] = sid.transpose(1, 0, 2).reshape(128, 1024)
    d = dict(shared)
    d.update(x_all=xb, x_own=np.ascontiguousarray(xb[own_rows]), pos_all=pos, pos_own=np.ascontiguousarray(pos[own_rows]),
             mem=f(mem[b]), corec=corec)
    return d


def shared_inputs(norm_mem, norm_mix, w_in, mla_q_norm, w_uq, mla_kv_norm, w_ukv, mla_out_norm, w_gate_up, b_gate,
                  gla_out_norm, w_out, norm_cross, w_cq, w_ck, w_cv, w_co, norm_ffn, w_peer_q, peer_sub_keys,
                  peer_u, peer_v, norm_final):
    f = lambda a: np.ascontiguousarray(np.asarray(a, np.float32))
    vecs = np.zeros((128, 160), np.float32)
    vecs[:, 0:32] = tvec(norm_mix[0])
    vecs[:, 32:64] = tvec(norm_cross[0])
    vecs[:, 64:96] = tvec(norm_ffn[0])
    vecs[:, 96:128] = tvec(norm_mem)
    vecs[:, 128:136] = tvec(mla_q_norm[0])
    vecs[:, 136:140] = tvec(mla_kv_norm[0])
    vecs[:, 140:156] = tvec(mla_out_norm[0])
    vecs[:, 156:160] = tvec(gla_out_norm[0])
    wg = np.concatenate([f(w_gate_up[0]), f(b_gate[0]).reshape(1, 1024)], axis=0)
    sk = np.asarray(peer_sub_keys[0], np.float32).reshape(16, 128, 128)
    subk = np.ascontiguousarray(sk.transpose(2, 0, 1))
    return dict(w_in=f(w_in[0]), w_uq=f(w_uq[0]), w_ukv=f(w_ukv[0]), wg=wg, w_out=f(w_out[0]), w_cq=f(w_cq[0]),
                w_ck=f(w_ck[0]), w_cv=f(w_cv[0]), w_co=f(w_co[0]), w_pq=f(w_peer_q[0]), subk=subk,
                peer_uT=np.ascontiguousarray(np.asarray(peer_u[0], np.float32).T), peer_v=f(peer_v[0]),
                vecs=vecs, gfin=f(norm_final).reshape(1, D), cst=host_consts())


def kernel(x, mem, positions, norm_mem, norm_mix, w_in, mla_q_norm, w_uq, mla_kv_norm, w_ukv,
           mla_out_norm, w_gate_up, b_gate, gla_out_norm, w_out, norm_cross, w_cq, w_ck, w_cv, w_co,
           norm_ffn, w_peer_q, peer_sub_keys, peer_u, peer_v, norm_final):
    x = np.asarray(x)
    B, S, _ = x.shape
    NOWN = S // TG // 4
    shared = shared_inputs(norm_mem, norm_mix, w_in, mla_q_norm, w_uq, mla_kv_norm, w_ukv, mla_out_norm, w_gate_up,
                           b_gate, gla_out_norm, w_out, norm_cross, w_cq, w_ck, w_cv, w_co, norm_ffn, w_peer_q,
                           peer_sub_keys, peer_u, peer_v, norm_final)
    in_maps = []
    for c in range(8):
        b, j = divmod(c, 4)
        in_maps.append(core_inputs(b, j, S, NOWN, x, mem, positions, norm_mem, norm_mix, w_in, mla_q_norm, w_uq,
                                   mla_kv_norm, w_ukv, mla_out_norm, w_gate_up, b_gate, gla_out_norm, w_out, norm_cross,
                                   w_cq, w_ck, w_cv, w_co, norm_ffn, w_peer_q, peer_sub_keys, peer_u, peer_v, norm_final,
                                   shared))
    nc = build(S, NOWN)
    res = run_bass_kernel_spmd(nc, in_maps, core_ids=list(range(8)))
    outp = np.zeros((B, S, D), np.float32)
    for c in range(8):
        b, j = divmod(c, 4)
        o = np.asarray(res.results[c]["out"])
        for i in range(NOWN):
            g = 4 * i + j
            outp[b, g * TG:(g + 1) * TG] = o[i * TG:(i + 1) * TG]
    return outp
```

```python
import contextlib
import numpy as np
import concourse.bass as bass
import concourse.mybir as mybir
from concourse.bass_utils import run_bass_kernel_spmd

F32 = mybir.dt.float32
BF16 = mybir.dt.bfloat16
I32 = mybir.dt.int32
AF = mybir.ActivationFunctionType
ALU = mybir.AluOpType

D = 4096
NT = 2
TG = NT * 128
EPS = 1e-6
NEG = -30000.0


class Buf:
    __slots__ = ("name", "w", "rd")

    def __init__(self, name):
        self.name = name
        self.w = None
        self.rd = []


class Op:
    __slots__ = ("eng", "fn", "deps", "is_dma", "semkey", "count", "needs_inc", "idx")


class Prog:
    ENGS = ("pe", "act", "dve", "pool", "sp")

    def __init__(self, nc, es):
        self.nc = nc
        self.es = es
        self.ops = []
        self.start = 0
        self.sems = {}
        self.counts = {}
        self.waited = {e: {} for e in self.ENGS}

    def op(self, eng, fn, reads=(), writes=(), dma=False, nowaw=False, semname=None):
        o = Op()
        o.eng, o.fn, o.is_dma, o.idx = eng, fn, dma, len(self.ops)
        o.needs_inc, o.count = False, None
        deps = {}
        for b in reads:
            if b.w is not None and b.w >= self.start:
                deps[b.w] = "raw"
        for b in writes:
            if b.w is not None and b.w >= self.start and b.w not in deps:
                if not (nowaw and dma and self.ops[b.w].is_dma and not b.rd):
                    deps[b.w] = "waw"
            last = {}
            for r in b.rd:
                if r < self.start:
                    continue
                ro = self.ops[r]
                if ro.is_dma:
                    if r not in deps:
                        deps[r] = "war"
                else:
                    last[ro.eng] = max(last.get(ro.eng, -1), r)
            for r in last.values():
                if r not in deps:
                    deps[r] = "war"
        o.deps = []
        for d, kind in deps.items():
            po = self.ops[d]
            if (not dma) and (not po.is_dma) and po.eng == eng and (kind != "raw" or eng == "pe"):
                continue
            o.deps.append(d)
        if dma:
            o.semkey = ("dma", semname or writes[0].name)
        else:
            o.semkey = ("eng", eng)
        for b in writes:
            b.w = o.idx
            b.rd = []
        for b in reads:
            b.rd.append(o.idx)
        self.ops.append(o)
        return o.idx

    def _sem(self, k):
        if k not in self.sems:
            self.sems[k] = self.es.enter_context(self.nc.semaphore("s%d" % len(self.sems)))
        return self.sems[k]

    def emit(self, final_waits=()):
        nc = self.nc
        ops = self.ops
        cur = ops[self.start:]
        for o in cur:
            for d in o.deps:
                ops[d].needs_inc = True
        for d in final_waits:
            ops[d].needs_inc = True
        for o in cur:
            if o.is_dma:
                self.counts[o.semkey] = self.counts.get(o.semkey, 0) + 16
                o.count = self.counts[o.semkey]
                self._sem(o.semkey)
            elif o.needs_inc:
                self.counts[o.semkey] = self.counts.get(o.semkey, 0) + 1
                o.count = self.counts[o.semkey]
                self._sem(o.semkey)
        per = {e: [o for o in cur if o.eng == e] for e in self.ENGS}
        dma_last = {}
        for o in cur:
            if o.is_dma:
                dma_last[o.semkey] = o.count
        sems = self.sems

        def run(engname, eng):
            waited = self.waited[engname]
            for o in per[engname]:
                need = {}
                for d in o.deps:
                    po = ops[d]
                    if need.get(po.semkey, 0) < po.count:
                        need[po.semkey] = po.count
                for k, v in need.items():
                    if waited.get(k, 0) >= v:
                        continue
                    eng.wait_ge(sems[k], v)
                    waited[k] = v
                ins = o.fn(eng)
                if o.is_dma:
                    ins.then_inc(sems[o.semkey], 16)
                elif o.needs_inc:
                    ins.then_inc(sems[o.semkey], 1)
            if engname == "sp":
                for k, v in dma_last.items():
                    if waited.get(k, 0) < v:
                        eng.wait_ge(sems[k], v)
                        waited[k] = v

        with nc.Block(no_gpsimd_drain=True) as block:
            @block.tensor
            def _(e):
                run("pe", e)

            @block.scalar
            def _(e):
                run("act", e)

            @block.vector
            def _(e):
                run("dve", e)

            @block.gpsimd
            def _(e):
                run("pool", e)

            @block.sync
            def _(e):
                run("sp", e)
        self.start = len(ops)


class TB:
    def __init__(self, t, name):
        self.t = t
        self.b = Buf(name)

    def __getitem__(self, k):
        return self.t[k]


def build(S, NOWN, dbg=False):
    NGRP = S // TG
    assert NGRP == 4 * NOWN
    nc = bass.Bass("TRN2", target_bir_lowering=False)

    def din(name, shape, dt=F32):
        return nc.dram_tensor(name, list(shape), dt, kind="ExternalInput").ap()

    x_all = din("x_all", [S, D])
    x_own = din("x_own", [NOWN * TG, D])
    pos_all = din("pos_all", [S, 1], I32)
    pos_own = din("pos_own", [NOWN * TG, 1], I32)
    mem = din("mem", [256, D])
    w_in = din("w_in", [D, 7760])
    w_uq = din("w_uq", [1024, 3072])
    w_ukv = din("w_ukv", [512, 4096])
    wg_in = din("wg", [17, 1024])
    w_out = din("w_out", [D, D])
    w_cq = din("w_cq", [D, 1024])
    w_ck = din("w_ck", [D, 1024])
    w_cv = din("w_cv", [D, 1024])
    w_co = din("w_co", [1024, D])
    w_pq = din("w_pq", [D, 2048])
    subk = din("subk", [128, 16, 128])
    peer_uT = din("peer_uT", [D, 16384])
    peer_v = din("peer_v", [16384, D])
    vecs_in = din("vecs", [128, 160])
    gfin_in = din("gfin", [1, D])
    cst_in = din("cst", [128, 1024])
    core_in = din("corec", [128, 1028])
    out = nc.dram_tensor("out", [NOWN * TG, D], F32, kind="ExternalOutput").ap()
    KnS = nc.dram_tensor("KnS", [16, 128, S], BF16, kind="Internal").ap()
    KrS = nc.dram_tensor("KrS", [128, S], BF16, kind="Internal").ap()
    VS = nc.dram_tensor("VS", [16, 128, S // 128, 130], BF16, kind="Internal").ap()
    w_in_b = nc.dram_tensor("w_in_b", [D, 7760], BF16, kind="Internal").ap()
    w_uq_b = nc.dram_tensor("w_uq_b", [1024, 3072], BF16, kind="Internal").ap()
    w_ukv_b = nc.dram_tensor("w_ukv_b", [512, 4096], BF16, kind="Internal").ap()
    w_out_b = nc.dram_tensor("w_out_b", [D, D], BF16, kind="Internal").ap()
    w_cq_b = nc.dram_tensor("w_cq_b", [D, 1024], BF16, kind="Internal").ap()
    w_co_b = nc.dram_tensor("w_co_b", [1024, D], BF16, kind="Internal").ap()
    w_pq_b = nc.dram_tensor("w_pq_b", [D, 2048], BF16, kind="Internal").ap()
    Ubf = nc.dram_tensor("Ubf", [64, 128, 32 * 256], BF16, kind="Internal").ap()
    Vbf = nc.dram_tensor("Vbf", [128, 128, 16 * 256], BF16, kind="Internal").ap()
    hA = nc.dram_tensor("hA", [TG, D], F32, kind="Internal").ap()
    hB = nc.dram_tensor("hB", [TG, D], F32, kind="Internal").ap()
    dbg_out = None
    if dbg:
        dbg_out = nc.dram_tensor("dbg", [NOWN * TG, D], F32, kind="ExternalOutput").ap()

    with contextlib.ExitStack() as ges:
        P = Prog(nc, ges)

        uid = [0]

        def sb(es, name, shape, dt):
            uid[0] += 1
            return TB(es.enter_context(nc.sbuf_tensor("s%d_%s" % (uid[0], name), list(shape), dt)), name)

        def ps(es, name, shape, dt):
            return TB(es.enter_context(nc.psum_tensor("p_" + name, list(shape), dt)), name)

        vecs = sb(ges, "vecs", [128, 160], F32)
        cst = sb(ges, "cst", [128, 1024], F32)
        corec = sb(ges, "corec", [128, 4], F32)
        identb = sb(ges, "identb", [128, 128], BF16)
        sidb = sb(ges, "sidb", [128, 8, 128], BF16)
        negfull = sb(ges, "negfull", [128, TG], BF16)
        maskdiag = sb(ges, "maskdiag", [128, NT, TG], BF16)
        state = sb(ges, "state", [128, 8, 512], F32)
        snap = sb(ges, "snap", [128, 8, 512], F32)
        KmT = sb(ges, "KmT", [128, 8, 256], BF16)
        Vm = sb(ges, "Vm", [128, 2, 4, 258], BF16)
        subkT = sb(ges, "subkT", [128, 16, 128], BF16)
        wg = sb(ges, "wg", [32, 1024], BF16)
        glrT = sb(ges, "glrT", [32, TG], BF16)
        pb = [ps(ges, "pb%d" % i, [128, 512], F32) for i in range(6)]
        pt = [ps(ges, "pt%d" % i, [128, 1024], BF16) for i in range(2)]
        rot = {"a": 0, "t": 0}

        bank_lo = [0]

        def bank():
            n = 6 - bank_lo[0]
            rot["a"] = (rot["a"] + 1) % n
            return pb[bank_lo[0] + rot["a"]]

        def tbank():
            rot["t"] = (rot["t"] + 1) % 2
            return pt[rot["t"]]

        V_MIX, V_CROSS, V_FFN, V_MEM, V_Q, V_KV, V_MO, V_GO = 0, 32, 64, 96, 128, 136, 140, 156
        C_ID, C_M1, C_TRI, C_CM, C_CIND, C_ROPE = 0, 128, 256, 384, 512, 514

        def act_copy(o, i, reads, writes, scale=None):
            if scale is None:
                P.op("act", lambda e: e.copy(out=o, in_=i), reads, writes)
            else:
                P.op("act", lambda e: e.activation(out=o, in_=i, func=AF.Copy, scale=scale), reads, writes)

        def rstd_from_ssq(es, ssq, n, tag):
            k = ssq.t.shape[1]
            r = sb(es, "rstd_" + tag, [128, k], F32)
            P.op("dve", lambda e: e.tensor_scalar(out=r[:], in0=ssq[:], scalar1=1.0 / n, scalar2=EPS,
                                                  op0=ALU.mult, op1=ALU.add), [ssq.b], [r.b])
            P.op("act", lambda e: e.activation(out=r[:], in_=r[:], func=AF.Sqrt), [r.b], [r.b])
            P.op("dve", lambda e: e.reciprocal(out=r[:], in_=r[:]), [r.b], [r.b])
            return r

        def transposes(src, nk, dstT, tt, gcol=None, kc0=0):
            for kc in range(nk):
                tb = tbank()
                P.op("pe", lambda e, kc=kc, tb=tb: e.transpose(out=tb[:, 0:128], in_=src(kc), identity=identb[:]),
                     [src.tb.b, identb.b], [tb.b])
                o = dstT[:, kc0 + kc, tt * 128:(tt + 1) * 128]
                if gcol is None:
                    P.op("dve", lambda e, o=o, tb=tb: e.tensor_copy(out=o, in_=tb[:, 0:128]), [tb.b], [dstT.b])
                else:
                    P.op("dve", lambda e, o=o, tb=tb, c=gcol + kc: e.tensor_scalar(
                        out=o, in0=tb[:, 0:128], scalar1=vecs[:, c:c + 1], scalar2=None, op0=ALU.mult),
                        [tb.b, vecs.b], [dstT.b])

        class Src:
            def __init__(self, tb, f):
                self.tb, self.f = tb, f

            def __call__(self, kc):
                return self.f(kc)

        def norm_T(es, rows, ntile, gcol, dstT, tag):
            xt = [sb(es, "xt%d_%s" % (i, tag), [128, D], F32) for i in range(2)]
            xs = sb(es, "xs_" + tag, [128, D], BF16)
            junk = xs
            ssq = sb(es, "ssq_" + tag, [128, ntile], F32)
            for tt in range(ntile):
                x = xt[tt % 2]
                P.op("sp", lambda e, x=x, tt=tt: e.dma_start(out=x[:], in_=rows(tt)), [], [x.b], dma=True)
                P.op("act", lambda e, x=x, tt=tt: e.activation(out=junk[:], in_=x[:], func=AF.Square,
                                                              accum_out=ssq[:, tt:tt + 1]), [x.b], [junk.b, ssq.b])
            rstd = rstd_from_ssq(es, ssq, D, tag)
            for tt in range(ntile):
                x = xt[tt % 2]
                if tt >= 2:
                    P.op("sp", lambda e, x=x, tt=tt: e.dma_start(out=x[:], in_=rows(tt)), [], [x.b], dma=True)
                P.op("dve", lambda e, x=x, tt=tt: e.tensor_scalar(out=xs[:], in0=x[:], scalar1=rstd[:, tt:tt + 1],
                                                                 scalar2=None, op0=ALU.mult), [x.b, rstd.b], [xs.b])
                transposes(Src(xs, lambda kc: xs[:, kc * 128:(kc + 1) * 128]), 32, dstT, tt, gcol)

        wrot = {"i": 0}

        def linear(wb, actT, KC, wap, blocks, ntile, evac):
            for ci, csz in enumerate(blocks):
                wrot["i"] ^= 1
                w = wb[wrot["i"]]
                P.op("pool", lambda e, w=w, ci=ci, csz=csz: e.dma_start(out=w[:, 0:KC, 0:csz], in_=wap(ci)),
                     [], [w.b], dma=True)
                for tt in range(ntile):
                    pbk = bank()
                    for kc in range(KC):
                        P.op("pe", lambda e, kc=kc, tt=tt, w=w, pbk=pbk, csz=csz: e.matmul(
                            pbk[:, 0:csz], lhsT=actT[:, kc, tt * 128:(tt + 1) * 128], rhs=w[:, kc, 0:csz],
                            start=(kc == 0), stop=(kc == KC - 1)), [actT.b, w.b], [pbk.b])
                    evac(ci, tt, pbk, csz)

        def linearT(wb, actT, KC, wap, nblk, ncols, evac):
            for ci in range(nblk):
                wrot["i"] ^= 1
                w = wb[wrot["i"]]
                P.op("pool", lambda e, w=w, ci=ci: e.dma_start(out=w[:, 0:KC, 0:512], in_=wap(ci)), [], [w.b], dma=True)
                for cb in range(4):
                    pbk = bank()
                    for kc in range(KC):
                        P.op("pe", lambda e, kc=kc, cb=cb, w=w, pbk=pbk: e.matmul(
                            pbk[:, 0:ncols], lhsT=w[:, kc, cb * 128:(cb + 1) * 128], rhs=actT[:, kc, 0:ncols],
                            start=(kc == 0), stop=(kc == KC - 1)), [actT.b, w.b], [pbk.b])
                    evac(ci * 4 + cb, pbk)

        def wcols(wdram, c0):
            v = wdram.rearrange("(c p) n -> p c n", p=128)
            return lambda ci, c0=c0: v[:, :, c0 + ci * 512: c0 + ci * 512 + 512]

        with contextlib.ExitStack() as es:
            P.op("sp", lambda e: e.dma_start(out=vecs[:], in_=vecs_in), [], [vecs.b], dma=True)
            P.op("sp", lambda e: e.dma_start(out=cst[:], in_=cst_in), [], [cst.b], dma=True)
            P.op("sp", lambda e: e.dma_start(out=corec[:], in_=core_in[:, 0:4]), [], [corec.b], dma=True)
            sidf = sb(es, "sidf", [128, 1024], F32)
            P.op("sp", lambda e: e.dma_start(out=sidf[:], in_=core_in[:, 4:1028]), [], [sidf.b], dma=True)
            P.op("pool", lambda e: e.dma_start(out=subkT[:], in_=subk), [], [subkT.b], dma=True)
            P.op("dve", lambda e: e.memset(wg[:], 0.0), [], [wg.b])
            P.op("pool", lambda e: e.dma_start(out=wg[0:17, :], in_=wg_in), [], [wg.b], dma=True)
            P.op("dve", lambda e: e.memset(glrT[:], 1.0), [], [glrT.b])
            P.op("dve", lambda e: e.tensor_copy(out=identb[:], in_=cst[:, C_ID:C_ID + 128]), [cst.b], [identb.b])
            P.op("dve", lambda e: e.tensor_copy(out=sidb[:].rearrange("p a b -> p (a b)"), in_=sidf[:]),
                 [sidf.b], [sidb.b])
            P.op("dve", lambda e: e.memset(negfull[:], NEG), [], [negfull.b])
            P.op("dve", lambda e: e.memset(maskdiag[:], 0.0), [], [maskdiag.b])
            for kcin in range(NT):
                for qb in range(NT):
                    if qb < kcin:
                        P.op("dve", lambda e, kcin=kcin, qb=qb: e.memset(maskdiag[:, kcin, qb * 128:(qb + 1) * 128], NEG),
                             [], [maskdiag.b])
                    elif qb == kcin:
                        P.op("dve", lambda e, kcin=kcin, qb=qb: e.tensor_scalar(
                            out=maskdiag[:, kcin, qb * 128:(qb + 1) * 128], in0=cst[:, 640:768],
                            scalar1=-NEG, scalar2=NEG, op0=ALU.mult, op1=ALU.add), [cst.b], [maskdiag.b])
            P.op("dve", lambda e: e.memset(state[:], 0.0), [], [state.b])
            P.op("dve", lambda e: e.memset(snap[:], 0.0), [], [snap.b])
            P.op("dve", lambda e: e.memset(Vm[:], 1.0), [], [Vm.b])
            wb = [sb(es, "wb%d" % i, [128, 32, 512], BF16) for i in range(2)]
            mnT = sb(es, "mnT", [128, 32, 256], BF16)
            norm_T(es, lambda tt: mem[tt * 128:(tt + 1) * 128, :], 2, V_MEM, mnT, "mem")

            def ev_k(cb, pbk):
                P.op("act", lambda e: e.copy(out=KmT[:, cb, :], in_=pbk[:, 0:256]), [pbk.b], [KmT.b])
            linearT(wb, mnT, 32, wcols(w_ck, 0), 2, 256, ev_k)

            def ev_v(ci, tt, pbk, csz):
                P.op("act", lambda e: e.copy(out=Vm[:, tt, ci * 2:(ci + 1) * 2, 0:256],
                                             in_=pbk[:, 0:512].rearrange("p (h d) -> p h d", d=256)), [pbk.b], [Vm.b])
            linear(wb, mnT, 32, wcols(w_cv, 0), [512, 512], 2, ev_v)
            P.emit()

        with contextlib.ExitStack() as es:
            stg = [sb(es, "stg%d" % i, [128, 32, 512], BF16) for i in range(2)]
            cn = [0]

            def convert(src, dst, K, N):
                KC = K // 128
                sv = src.rearrange("(c p) n -> p c n", p=128)
                dv = dst.rearrange("(c p) n -> p c n", p=128)
                db_ = Buf("cv_" + str(cn[0]))
                for c0 in range(0, N, 512):
                    csz = min(512, N - c0)
                    st = stg[cn[0] % 2]
                    cn[0] += 1
                    P.op("pool", lambda e, st=st, c0=c0, csz=csz: e.dma_start(out=st[:, 0:KC, 0:csz], in_=sv[:, :, c0:c0 + csz]),
                         [], [st.b], dma=True)
                    P.op("sp", lambda e, st=st, c0=c0, csz=csz: e.dma_start(out=dv[:, :, c0:c0 + csz], in_=st[:, 0:KC, 0:csz]),
                         [st.b], [db_], dma=True, nowaw=True, semname="cvo_" + st.b.name)
            convert(w_in, w_in_b, D, 7760)
            convert(w_uq, w_uq_b, 1024, 3072)
            convert(w_ukv, w_ukv_b, 512, 4096)
            convert(w_out, w_out_b, D, D)
            convert(w_cq, w_cq_b, D, 1024)
            convert(w_co, w_co_b, 1024, D)
            convert(w_pq, w_pq_b, D, 2048)
            P.emit()

        def rope_tables(es, posrows, ntile, tag):
            cs = sb(es, "cs_" + tag, [128, ntile, 2, 32], F32)
            pi_ = sb(es, "posi_" + tag, [128, ntile], I32)
            pf = sb(es, "posf_" + tag, [128, ntile], F32)
            y = sb(es, "ry_" + tag, [128, 32], F32)
            ki = sb(es, "rk_" + tag, [128, 32], I32)
            kf = sb(es, "rkf_" + tag, [128, 32], F32)
            m = sb(es, "rm_" + tag, [128, 32], F32)
            for tt in range(ntile):
                P.op("sp", lambda e, tt=tt: e.dma_start(out=pi_[:, tt:tt + 1], in_=posrows(tt)), [], [pi_.b], dma=True)
            P.op("dve", lambda e: e.tensor_copy(out=pf[:], in_=pi_[:]), [pi_.b], [pf.b])
            for tt in range(ntile):
                for which, off in ((0, 0.25), (1, 0.0)):
                    P.op("dve", lambda e, tt=tt, off=off: e.tensor_scalar(
                        out=y[:], in0=cst[:, C_ROPE:C_ROPE + 32], scalar1=pf[:, tt:tt + 1], scalar2=off,
                        op0=ALU.mult, op1=ALU.add), [cst.b, pf.b], [y.b])
                    P.op("dve", lambda e: e.tensor_copy(out=ki[:], in_=y[:]), [y.b], [ki.b])
                    P.op("dve", lambda e: e.tensor_copy(out=kf[:], in_=ki[:]), [ki.b], [kf.b])
                    P.op("dve", lambda e: e.tensor_tensor(out=y[:], in0=y[:], in1=kf[:], op=ALU.subtract), [y.b, kf.b], [y.b])
                    P.op("dve", lambda e: e.tensor_scalar(out=m[:], in0=y[:], scalar1=0.5, scalar2=None, op0=ALU.is_gt),
                         [y.b], [m.b])
                    P.op("dve", lambda e: e.tensor_tensor(out=y[:], in0=y[:], in1=m[:], op=ALU.subtract), [y.b, m.b], [y.b])
                    P.op("dve", lambda e: e.tensor_scalar(out=m[:], in0=y[:], scalar1=-0.5, scalar2=None, op0=ALU.is_lt),
                         [y.b], [m.b])
                    P.op("dve", lambda e: e.tensor_tensor(out=y[:], in0=y[:], in1=m[:], op=ALU.add), [y.b, m.b], [y.b])
                    P.op("act", lambda e, tt=tt, which=which: e.activation(
                        out=cs[:, tt, which, :], in_=y[:], func=AF.Sin, scale=2.0 * np.pi), [y.b], [cs.b])
            return cs

        def apply_rope(es, src, dst, cs, tt, nh, tag):
            t1 = sb(es, "rt1_" + tag, [128, nh, 32], F32)
            t2 = sb(es, "rt2_" + tag, [128, nh, 32], F32)
            cosb = cs[:, tt, 0, :].unsqueeze(1).to_broadcast([128, nh, 32])
            sinb = cs[:, tt, 1, :].unsqueeze(1).to_broadcast([128, nh, 32])
            x1 = src[0][:, :, 0:32]
            x2 = src[0][:, :, 32:64]
            sbuf_src = src[1]
            P.op("dve", lambda e: e.tensor_tensor(out=t1[:], in0=x1, in1=cosb, op=ALU.mult), [sbuf_src, cs.b], [t1.b])
            P.op("dve", lambda e: e.tensor_tensor(out=t2[:], in0=x2, in1=sinb, op=ALU.mult), [sbuf_src, cs.b], [t2.b])
            P.op("dve", lambda e: e.tensor_tensor(out=dst[0][:, :, 0:32], in0=t1[:], in1=t2[:], op=ALU.subtract),
                 [t1.b, t2.b], [dst[1]])
            P.op("dve", lambda e: e.tensor_tensor(out=t1[:], in0=x2, in1=cosb, op=ALU.mult), [sbuf_src, cs.b], [t1.b])
            P.op("dve", lambda e: e.tensor_tensor(out=t2[:], in0=x1, in1=sinb, op=ALU.mult), [sbuf_src, cs.b], [t2.b])
            P.op("dve", lambda e: e.tensor_tensor(out=dst[0][:, :, 32:64], in0=t1[:], in1=t2[:], op=ALU.add),
                 [t1.b, t2.b], [dst[1]])


        w_in_v = w_in_b.rearrange("(c p) n -> p c n", p=128)

        def stage_norm(rows, ntile, gcol, dstT, tag):
            with contextlib.ExitStack() as es:
                norm_T(es, rows, ntile, gcol, dstT, tag)
                P.emit()

        def sweep_mla(g, nT, es, wb):
            if True:
                ckv = sb(es, "ckv", [128, NT, 512], F32)
                kr = sb(es, "kr", [128, NT, 1, 64], F32)

                def ev_ckv(ci, tt, pbk, csz):
                    if ci == 0:
                        P.op("act", lambda e: e.copy(out=ckv[:, tt, :], in_=pbk[:, 0:512]), [pbk.b], [ckv.b])
                    else:
                        P.op("act", lambda e: e.copy(out=kr[:, tt, 0, :], in_=pbk[:, 0:64]), [pbk.b], [kr.b])
                linear(wb, nT, 32, lambda ci: (w_in_v[:, :, 1024:1536] if ci == 0 else w_in_v[:, :, 1536:1600]),
                       [512, 64], NT, ev_ckv)
                junk = sb(es, "junk2", [128, 512], BF16)
                ssq = sb(es, "ssq2", [128, NT], F32)
                for tt in range(NT):
                    P.op("act", lambda e, tt=tt: e.activation(out=junk[:], in_=ckv[:, tt, :], func=AF.Square,
                                                              accum_out=ssq[:, tt:tt + 1]), [ckv.b], [junk.b, ssq.b])
                rstd = rstd_from_ssq(es, ssq, 512, "kv")
                ckvs = sb(es, "ckvs", [128, 512], BF16)
                ckvnT = sb(es, "ckvnT", [128, 4, TG], BF16)
                for tt in range(NT):
                    P.op("dve", lambda e, tt=tt: e.tensor_scalar(out=ckvs[:], in0=ckv[:, tt, :], scalar1=rstd[:, tt:tt + 1],
                                                                 scalar2=None, op0=ALU.mult), [ckv.b, rstd.b], [ckvs.b])
                    transposes(Src(ckvs, lambda kc: ckvs[:, kc * 128:(kc + 1) * 128]), 4, ckvnT, tt, V_KV)
                cs = rope_tables(es, lambda tt: pos_all[g * TG + tt * 128: g * TG + (tt + 1) * 128, :], NT, "sw")
                krr = sb(es, "krr", [128, 2, 1, 64], BF16)
                krT = sb(es, "krT", [128, 1, TG], BF16)
                for tt in range(NT):
                    apply_rope(es, (kr[:, tt], kr.b), (krr[:, 0], krr.b), cs, tt, 1, "k%d" % tt)
                    P.op("dve", lambda e: e.tensor_copy(out=krr[:, 1], in_=krr[:, 0]), [krr.b], [krr.b])
                    transposes(Src(krr, lambda kc: krr[:].rearrange("p a b c -> p (a b c)")), 1, krT, tt)
                P.op("sp", lambda e: e.dma_start(out=KrS[:, g * TG:(g + 1) * TG], in_=krT[:, 0, :]), [krT.b], [Buf("KrS")], dma=True)
                wkv = wb[1]
                wkv_f = wkv[:].rearrange("p a b -> p (a b)").rearrange("p (c n) -> p c n", c=4)
                P.op("pool", lambda e: e.dma_start(out=wkv_f, in_=w_ukv_b.rearrange("(c p) n -> p c n", p=128)), [], [wkv.b], dma=True)
                wkv_h = wkv_f.rearrange("p c (h t d) -> p c h t d", t=2, d=128)
                kout = sb(es, "kout", [128, 16, TG], BF16)
                for h in range(16):
                    pbk = bank()
                    for c in range(4):
                        P.op("pe", lambda e, c=c, h=h, pbk=pbk: e.matmul(pbk[:, 0:TG], lhsT=wkv_h[:, c, h, 0, :], rhs=ckvnT[:, c, :],
                                                                        start=(c == 0), stop=(c == 3)), [wkv.b, ckvnT.b], [pbk.b])
                    P.op("act", lambda e, h=h, pbk=pbk: e.copy(out=kout[:, h, :], in_=pbk[:, 0:TG]), [pbk.b], [kout.b])
                P.op("sp", lambda e: e.dma_start(out=KnS[:, :, g * TG:(g + 1) * TG].rearrange("h p s -> p h s"), in_=kout[:]),
                     [kout.b], [Buf("KnS")], dma=True)
                vout = sb(es, "vout", [128, NT, 16, 130], BF16)
                P.op("dve", lambda e: e.memset(vout[:], 1.0), [], [vout.b])
                for tt in range(NT):
                    for hb in range(4):
                        pbk = bank()
                        for c in range(4):
                            P.op("pe", lambda e, c=c, hb=hb, tt=tt, pbk=pbk: e.matmul(
                                pbk[:, 0:512].rearrange("p (h d) -> p h d", d=128), lhsT=ckvnT[:, c, tt * 128:(tt + 1) * 128],
                                rhs=wkv_h[:, c, hb * 4:(hb + 1) * 4, 1, :], start=(c == 0), stop=(c == 3)), [wkv.b, ckvnT.b], [pbk.b])
                        P.op("act", lambda e, hb=hb, tt=tt, pbk=pbk: e.copy(
                            out=vout[:, tt, hb * 4:(hb + 1) * 4, 0:128], in_=pbk[:, 0:512].rearrange("p (h d) -> p h d", d=128)),
                            [pbk.b], [vout.b])
                vsb = Buf("VS")
                for tt in range(NT):
                    P.op("sp", lambda e, tt=tt: e.dma_start(out=VS[:, :, g * NT + tt, :].rearrange("h p e -> p h e"), in_=vout[:, tt, :, :]),
                         [vout.b], [vsb], dma=True, nowaw=True)

        def gla_alloc(es, own):
            r = {"gk": sb(es, "gk", [128, NT, 1024], F32), "gv": sb(es, "gv", [128, NT, 2048], BF16)}
            if own:
                r["gq"] = sb(es, "gq", [128, NT, 1024], F32)
                r["sog"] = sb(es, "sog", [128, NT, 2048], BF16)
            return r

        def gla_proj(r, wb, nT, own):
            gk, gv = r["gk"], r["gv"]

            def ev(ci, tt, pbk, csz):
                if own and ci < 2:
                    P.op("act", lambda e: e.copy(out=r["gq"][:, tt, ci * 512:(ci + 1) * 512], in_=pbk[:, 0:512]), [pbk.b], [r["gq"].b])
                    return
                c2 = ci - (2 if own else 0)
                if c2 < 2:
                    P.op("act", lambda e: e.copy(out=gk[:, tt, c2 * 512:(c2 + 1) * 512], in_=pbk[:, 0:512]), [pbk.b], [gk.b])
                elif c2 < 6:
                    P.op("act", lambda e: e.copy(out=gv[:, tt, (c2 - 2) * 512:(c2 - 1) * 512], in_=pbk[:, 0:512]), [pbk.b], [gv.b])
                else:
                    P.op("act", lambda e: e.activation(out=r["sog"][:, tt, (c2 - 6) * 512:(c2 - 5) * 512], in_=pbk[:, 0:512],
                                                       func=AF.Silu), [pbk.b], [r["sog"].b])
            c0 = 1600 if own else 2624
            nblk = (8 if own else 6)
            blocks = [512] * nblk
            if own:
                def wap(ci):
                    if ci < 8:
                        return w_in_v[:, :, 1600 + ci * 512:1600 + (ci + 1) * 512]
                    return w_in_v[:, :, 5712 + (ci - 8) * 512:5712 + (ci - 7) * 512]
                linear(wb, nT, 32, wap, [512] * 12, NT, ev)
            else:
                linear(wb, nT, 32, lambda ci: w_in_v[:, :, 2624 + ci * 512:2624 + (ci + 1) * 512], [512] * 6, NT, ev)
            w = wb[0]
            P.op("pool", lambda e: e.dma_start(out=w[:, 0:32, 0:16], in_=w_in_v[:, :, 5696:5712]), [], [w.b], dma=True)
            pbk = bank()
            for kc in range(32):
                P.op("pe", lambda e, kc=kc: e.matmul(pbk[0:16, 0:TG], lhsT=w[:, kc, 0:16], rhs=nT[:, kc, :],
                                                     start=(kc == 0), stop=(kc == 31)), [w.b, nT.b], [pbk.b])
            P.op("act", lambda e: e.copy(out=glrT[0:16, :], in_=pbk[0:16, 0:TG]), [pbk.b], [glrT.b])
            return r

        def gla_tile(es, tt, r, st, own, tag):
            gk, gv = r["gk"], r["gv"]
            lp = sb(es, "lp" + tag, [128, 1024], F32)
            for hf in range(2):
                pbk = bank()
                P.op("pe", lambda e, hf=hf, pbk=pbk: e.matmul(pbk[:, 0:512], lhsT=glrT[0:32, tt * 128:(tt + 1) * 128],
                                                             rhs=wg[0:32, hf * 512:(hf + 1) * 512], start=True, stop=True),
                     [glrT.b, wg.b], [pbk.b])
                P.op("act", lambda e, hf=hf, pbk=pbk: e.activation(out=lp[:, hf * 512:(hf + 1) * 512], in_=pbk[:, 0:512],
                                                                  func=AF.Exp, scale=-1.0), [pbk.b], [lp.b])
            P.op("act", lambda e: e.activation(out=lp[:], in_=lp[:], func=AF.Ln, bias=1.0), [lp.b], [lp.b])
            e1 = sb(es, "e1" + tag, [128, 1024], F32)
            kd = sb(es, "kd" + tag, [128, 1024], BF16)
            for hf in range(2):
                pbk = bank()
                P.op("pe", lambda e, hf=hf, pbk=pbk: e.matmul(pbk[:, 0:512], lhsT=cst[:, C_M1:C_M1 + 128],
                                                             rhs=lp[:, hf * 512:(hf + 1) * 512], start=True, stop=True),
                     [cst.b, lp.b], [pbk.b])
                P.op("act", lambda e, hf=hf, pbk=pbk: e.activation(out=e1[:, hf * 512:(hf + 1) * 512], in_=pbk[:, 0:512],
                                                                  func=AF.Exp), [pbk.b], [e1.b])
            P.op("dve", lambda e: e.tensor_tensor(out=kd[:], in0=gk[:, tt, :], in1=e1[:], op=ALU.mult), [gk.b, e1.b], [kd.b])
            decT = sb(es, "dec" + tag, [128, 16], F32)
            pbd = bank()
            for dc in range(8):
                P.op("pe", lambda e, dc=dc: e.matmul(pbd[:, dc * 2:dc * 2 + 2], lhsT=lp[:, dc * 128:(dc + 1) * 128],
                                                     rhs=cst[:, C_CIND:C_CIND + 2], start=True, stop=True), [lp.b, cst.b], [pbd.b])
            P.op("act", lambda e: e.activation(out=decT[:], in_=pbd[:, 0:16], func=AF.Exp), [pbd.b], [decT.b])
            if own is not None:
                ostbf, mixedT, sog, gq = own["ostbf"], own["mixedT"], r["sog"], r["gq"]
                eb = sb(es, "eb" + tag, [128, 1024], F32)
                enb = sb(es, "enb" + tag, [128, 1024], F32)
                for hf in range(2):
                    pbk = bank()
                    P.op("pe", lambda e, hf=hf, pbk=pbk: e.matmul(pbk[:, 0:512], lhsT=cst[:, C_TRI:C_TRI + 128],
                                                                 rhs=lp[:, hf * 512:(hf + 1) * 512], start=True, stop=True),
                         [cst.b, lp.b], [pbk.b])
                    P.op("act", lambda e, hf=hf, pbk=pbk: e.activation(out=eb[:, hf * 512:(hf + 1) * 512], in_=pbk[:, 0:512],
                                                                      func=AF.Exp), [pbk.b], [eb.b])
                    P.op("act", lambda e, hf=hf, pbk=pbk: e.activation(out=enb[:, hf * 512:(hf + 1) * 512], in_=pbk[:, 0:512],
                                                                      func=AF.Exp, scale=-1.0), [pbk.b], [enb.b])
                qe = sb(es, "qe" + tag, [128, 1024], BF16)
                ke = sb(es, "ke" + tag, [128, 1024], BF16)
                P.op("dve", lambda e: e.scalar_tensor_tensor(out=qe[:], in0=gq[:, tt, :], scalar=1.0 / 16, in1=eb[:],
                                                             op0=ALU.mult, op1=ALU.mult), [gq.b, eb.b], [qe.b])
                P.op("dve", lambda e: e.tensor_tensor(out=ke[:], in0=gk[:, tt, :], in1=enb[:], op=ALU.mult), [gk.b, enb.b], [ke.b])
                qeT = sb(es, "qeT" + tag, [128, 8, 128], BF16)
                keT = sb(es, "keT" + tag, [128, 8, 128], BF16)
                transposes(Src(qe, lambda kc: qe[:, kc * 128:(kc + 1) * 128]), 8, qeT, 0)
                transposes(Src(ke, lambda kc: ke[:, kc * 128:(kc + 1) * 128]), 8, keT, 0)
                qz = [sb(es, "qz%d" % n + tag, [128, 8, 128], BF16) for n in range(2)]
                for n in range(2):
                    P.op("dve", lambda e, n=n: e.memset(qz[n][:], 0.0), [], [qz[n].b])
                    P.op("dve", lambda e, n=n: e.tensor_copy(out=qz[n][:, :, n * 64:(n + 1) * 64], in_=qeT[:, :, n * 64:(n + 1) * 64]),
                         [qeT.b], [qz[n].b])
                attT = sb(es, "attT" + tag, [128, 4, 128], BF16)
                for h in range(4):
                    pa = bank()
                    for dc in range(2):
                        P.op("pe", lambda e, h=h, dc=dc, pa=pa: e.matmul(pa[:, 0:128], lhsT=keT[:, h * 2 + dc, :], rhs=qeT[:, h * 2 + dc, :],
                                                                        start=(dc == 0), stop=(dc == 1)), [keT.b, qeT.b], [pa.b])
                    P.op("dve", lambda e, h=h, pa=pa: e.tensor_tensor(out=attT[:, h, :], in0=pa[:, 0:128], in1=cst[:, C_CM:C_CM + 128],
                                                                     op=ALU.mult), [pa.b, cst.b], [attT.b])
                po = [pb[h] for h in range(4)]
                for h in range(4):
                    P.op("pe", lambda e, h=h: e.matmul(po[h][:, 0:512], lhsT=attT[:, h, :], rhs=gv[:, tt, h * 512:(h + 1) * 512],
                                                       start=True, stop=False), [attT.b, gv.b], [po[h].b])
            for n in range(2):
                if own is not None:
                    for h in range(4):
                        for dc in range(2):
                            P.op("pe", lambda e, h=h, dc=dc, n=n: e.matmul(
                                po[h][:, 0:512], lhsT=qz[n][:, h * 2 + dc, :], rhs=ostbf[:, h * 2 + dc, :],
                                start=False, stop=(n == 1 and dc == 1)), [qz[n].b, ostbf.b], [po[h].b])
                for hd in range(8):
                    h, dc = divmod(hd, 2)
                    pbk = bank()
                    P.op("pe", lambda e, h=h, dc=dc, n=n, pbk=pbk: e.matmul(
                        pbk[:, 0:512], lhsT=kd[n * 64:(n + 1) * 64, h * 256 + dc * 128:h * 256 + (dc + 1) * 128],
                        rhs=gv[n * 64:(n + 1) * 64, tt, h * 512:(h + 1) * 512], start=True, stop=True), [kd.b, gv.b], [pbk.b])
                    P.op("dve", lambda e, hd=hd, n=n, pbk=pbk: e.scalar_tensor_tensor(
                        out=st[:, hd, :], in0=st[:, hd, :], scalar=decT[:, hd * 2 + n:hd * 2 + n + 1], in1=pbk[:, 0:512],
                        op0=ALU.mult, op1=ALU.add), [st.b, decT.b, pbk.b], [st.b])
                if own is not None:
                    P.op("act", lambda e: e.copy(out=ostbf[:].rearrange("p a b -> p (a b)"), in_=st[:].rearrange("p a b -> p (a b)")),
                         [st.b], [ostbf.b])
            if own is not None:
                junk = sb(es, "gj" + tag, [128, 512], BF16)
                ssq = sb(es, "gss" + tag, [128, 4], F32)
                for h in range(4):
                    P.op("act", lambda e, h=h: e.activation(out=junk[:], in_=po[h][:, 0:512], func=AF.Square,
                                                            accum_out=ssq[:, h:h + 1]), [po[h].b], [junk.b, ssq.b])
                rstd = rstd_from_ssq(es, ssq, 512, "g" + tag)
                omix = sb(es, "omix" + tag, [128, 2048], BF16)
                for h in range(4):
                    P.op("dve", lambda e, h=h: e.scalar_tensor_tensor(
                        out=omix[:, h * 512:(h + 1) * 512], in0=po[h][:, 0:512], scalar=rstd[:, h:h + 1],
                        in1=sog[:, tt, h * 512:(h + 1) * 512], op0=ALU.mult, op1=ALU.mult), [po[h].b, rstd.b, sog.b], [omix.b])
                for h in range(4):
                    transposes(Src(omix, lambda kc, h=h: omix[:, h * 512 + kc * 128:h * 512 + (kc + 1) * 128]), 4, mixedT, tt,
                               V_GO, kc0=16 + h * 4)

        def sweep_gla(g, nT):
            with contextlib.ExitStack() as es:
                wb = [sb(es, "wb%d" % i, [128, 32, 512], BF16) for i in range(2)]
                sweep_mla(g, nT, es, wb)
                rr = g % 4
                if rr == 0:
                    P.op("dve", lambda e: e.tensor_scalar(out=snap[:], in0=state[:], scalar1=corec[:, 0:1], scalar2=None,
                                                          op0=ALU.mult), [state.b, corec.b], [snap.b])
                else:
                    P.op("dve", lambda e: e.scalar_tensor_tensor(out=snap[:].rearrange("p a b -> p (a b)"),
                                                                 in0=state[:].rearrange("p a b -> p (a b)"),
                                                                 scalar=corec[:, rr:rr + 1], in1=snap[:].rearrange("p a b -> p (a b)"),
                                                                 op0=ALU.mult, op1=ALU.add), [state.b, corec.b, snap.b], [snap.b])
                r = gla_alloc(es, False)
                gla_proj(r, wb, nT, False)
                for tt in range(NT):
                    gla_tile(es, tt, r, state, None, "s%d" % tt)
                P.emit()

        def own_mla(i, nT, mixedT):
            with contextlib.ExitStack() as oes:
                qnT = sb(oes, "qnT", [128, 16, TG], BF16)
                qrT = sb(oes, "qrT", [128, 8, TG], BF16)
                cqnT = sb(oes, "cqnT", [128, 8, TG], BF16)
                with contextlib.ExitStack() as es:
                    wb = [sb(es, "wb%d" % k, [128, 32, 512], BF16) for k in range(2)]
                    cq = sb(es, "cq", [128, NT, 1024], F32)

                    def ev(ci, tt, pbk, csz):
                        P.op("act", lambda e: e.copy(out=cq[:, tt, ci * 512:(ci + 1) * 512], in_=pbk[:, 0:512]), [pbk.b], [cq.b])
                    linear(wb, nT, 32, lambda ci: w_in_v[:, :, ci * 512:(ci + 1) * 512], [512, 512], NT, ev)
                    junk = sb(es, "junk3", [128, 1024], BF16)
                    ssq = sb(es, "ssq3", [128, NT], F32)
                    for tt in range(NT):
                        P.op("act", lambda e, tt=tt: e.activation(out=junk[:], in_=cq[:, tt, :], func=AF.Square,
                                                                  accum_out=ssq[:, tt:tt + 1]), [cq.b], [junk.b, ssq.b])
                    rstd = rstd_from_ssq(es, ssq, 1024, "q")
                    for tt in range(NT):
                        P.op("dve", lambda e, tt=tt: e.tensor_scalar(out=junk[:], in0=cq[:, tt, :], scalar1=rstd[:, tt:tt + 1],
                                                                     scalar2=None, op0=ALU.mult), [cq.b, rstd.b], [junk.b])
                        transposes(Src(junk, lambda kc: junk[:, kc * 128:(kc + 1) * 128]), 8, cqnT, tt, V_Q)
                    wq_v = w_uq_b.rearrange("(c p) (h e) -> p c h e", p=128, e=192)

                    def wap_n(ci):
                        return wq_v[:, :, ci * 4:(ci + 1) * 4, 0:128]

                    def ev_n(cb, pbk):
                        P.op("act", lambda e: e.copy(out=qnT[:, cb, :], in_=pbk[:, 0:TG]), [pbk.b], [qnT.b])
                    for ci in range(4):
                        wrot["i"] ^= 1
                        w = wb[wrot["i"]]
                        for hh in range(4):
                            P.op("pool", lambda e, w=w, ci=ci, hh=hh: e.dma_start(
                                out=w[:, 0:8, hh * 128:(hh + 1) * 128], in_=wq_v[:, :, ci * 4 + hh, 0:128]), [], [w.b],
                                dma=True, nowaw=True)
                        for cb in range(4):
                            pbk = bank()
                            for kc in range(8):
                                P.op("pe", lambda e, kc=kc, cb=cb, w=w, pbk=pbk: e.matmul(
                                    pbk[:, 0:TG], lhsT=w[:, kc, cb * 128:(cb + 1) * 128], rhs=cqnT[:, kc, :],
                                    start=(kc == 0), stop=(kc == 7)), [cqnT.b, w.b], [pbk.b])
                            ev_n(ci * 4 + cb, pbk)
                    qr = sb(es, "qr", [128, NT, 16, 64], F32)
                    for ci in range(2):
                        wrot["i"] ^= 1
                        w = wb[wrot["i"]]
                        for hh in range(8):
                            P.op("pool", lambda e, w=w, ci=ci, hh=hh: e.dma_start(
                                out=w[:, 0:8, hh * 64:(hh + 1) * 64], in_=wq_v[:, :, ci * 8 + hh, 128:192]), [], [w.b],
                                dma=True, nowaw=True)
                        for tt in range(NT):
                            pbk = bank()
                            for kc in range(8):
                                P.op("pe", lambda e, kc=kc, tt=tt, w=w, pbk=pbk: e.matmul(
                                    pbk[:, 0:512], lhsT=cqnT[:, kc, tt * 128:(tt + 1) * 128], rhs=w[:, kc, 0:512],
                                    start=(kc == 0), stop=(kc == 7)), [cqnT.b, w.b], [pbk.b])
                            P.op("act", lambda e, tt=tt, ci=ci, pbk=pbk: e.copy(
                                out=qr[:, tt, ci * 8:(ci + 1) * 8, :], in_=pbk[:, 0:512].rearrange("p (h e) -> p h e", e=64)),
                                [pbk.b], [qr.b])
                    cs = rope_tables(es, lambda tt: pos_own[i * TG + tt * 128: i * TG + (tt + 1) * 128, :], NT, "ow")
                    qrr = sb(es, "qrr", [128, 16, 64], BF16)
                    for tt in range(NT):
                        apply_rope(es, (qr[:, tt], qr.b), (qrr[:], qrr.b), cs, tt, 16, "q%d" % tt)
                        transposes(Src(qrr, lambda kc: qrr[:, kc * 2:(kc + 1) * 2, :].rearrange("p a b -> p (a b)")), 8, qrT, tt)
                    P.emit()
                with contextlib.ExitStack() as es:
                    nk = (4 * i + 4) * TG
                    nkc = nk // 128
                    krT2 = sb(es, "krT2", [128, nk], BF16)
                    P.op("sp", lambda e: e.dma_start(out=krT2[:], in_=KrS[:, 0:nk]), [], [krT2.b], dma=True)
                    SEG = nk if nk <= 4096 else nk // 2
                    nseg = nk // SEG
                    skc = SEG // 128
                    knT = [sb(es, "knT%d" % k, [128, SEG], BF16) for k in range(2)]
                    vaug = [sb(es, "vaug%d" % k, [128, skc, 130], BF16) for k in range(2)]
                    pT = [sb(es, "pT%d" % k, [128, 2 * TG], BF16) for k in range(2)]
                    omla = sb(es, "omla", [128, NT, 2048], F32)
                    rs = sb(es, "rsum", [128, NT], F32)
                    bank_lo[0] = 1
                    po = pb[0]
                    pov = po[:, 0:NT * 130].rearrange("p (a b) -> p a b", b=130)
                    sc = 192.0 ** -0.5
                    bcnt = 0
                    pcnt = 0
                    for h in range(16):
                        half = (h % 2) * 64
                        for sg in range(nseg):
                            kt, va = knT[bcnt % 2], vaug[bcnt % 2]
                            bcnt += 1
                            k0 = sg * SEG
                            P.op("sp", lambda e, kt=kt, h=h, k0=k0: e.dma_start(out=kt[:], in_=KnS[h, :, k0:k0 + SEG]), [], [kt.b], dma=True)
                            P.op("sp", lambda e, va=va, h=h, sg=sg: e.dma_start(out=va[:], in_=VS[h, :, sg * skc:(sg + 1) * skc, :]),
                                 [], [va.b], dma=True)
                            for kl2 in range(0, skc, 2):
                                pS = bank()
                                p = pT[pcnt % 2]
                                pcnt += 1
                                for sub in range(2):
                                    kl = kl2 + sub
                                    kc = sg * skc + kl
                                    o_ = pS[:, sub * TG:(sub + 1) * TG]
                                    masked = kc >= 4 * i * NT
                                    P.op("pe", lambda e, kl=kl, kt=kt, h=h, o_=o_, sub=sub: e.matmul(
                                        o_, lhsT=kt[:, kl * 128:(kl + 1) * 128], rhs=qnT[:, h, :], start=(sub == 0), stop=False,
                                        skip_group_check=True), [kt.b, qnT.b], [pS.b])
                                    P.op("pe", lambda e, kc=kc, h=h, o_=o_, half=half, masked=masked: e.matmul(
                                        o_, lhsT=krT2[half:half + 64, kc * 128:(kc + 1) * 128], rhs=qrT[half:half + 64, h // 2, :],
                                        start=False, stop=(not masked), skip_group_check=True), [krT2.b, qrT.b], [pS.b])
                                    if masked:
                                        rr = kc // NT - 4 * i
                                        kcin = kc % NT
                                        P.op("pe", lambda e, rr=rr, o_=o_: e.matmul(o_, lhsT=sidb[:, rr, :], rhs=negfull[:, :],
                                                                                    start=False, stop=False, skip_group_check=True),
                                             [sidb.b, negfull.b], [pS.b])
                                        P.op("pe", lambda e, rr=rr, kcin=kcin, o_=o_: e.matmul(
                                            o_, lhsT=sidb[:, 4 + rr, :], rhs=maskdiag[:, kcin, :], start=False, stop=True,
                                            skip_group_check=True), [sidb.b, maskdiag.b], [pS.b])
                                P.op("act", lambda e, p=p, pS=pS: e.activation(out=p[:], in_=pS[:, 0:2 * TG], func=AF.Exp, scale=sc),
                                     [pS.b], [p.b])
                                for sub in range(2):
                                    kl = kl2 + sub
                                    kc = sg * skc + kl
                                    for qb in range(NT):
                                        P.op("pe", lambda e, p=p, qb=qb, kc=kc, kl=kl, va=va, sub=sub: e.matmul(
                                            pov[:, qb, 0:129], lhsT=p[:, sub * TG + qb * 128:sub * TG + (qb + 1) * 128], rhs=va[:, kl, 0:129],
                                            start=(kc == 0 and qb == 0), stop=(kc == nkc - 1), skip_group_check=True), [p.b, va.b], [po.b])
                        for qb in range(NT):
                            P.op("dve", lambda e, qb=qb: e.reciprocal(out=rs[:, qb:qb + 1], in_=pov[:, qb, 128:129]), [po.b], [rs.b])
                            P.op("dve", lambda e, qb=qb, h=h: e.tensor_scalar(
                                out=omla[:, qb, h * 128:(h + 1) * 128], in0=pov[:, qb, 0:128], scalar1=rs[:, qb:qb + 1], scalar2=None,
                                op0=ALU.mult), [po.b, rs.b], [omla.b])
                    bank_lo[0] = 0
                    junk = sb(es, "junk4", [128, 2048], BF16)
                    ssq = sb(es, "ssq4", [128, NT], F32)
                    for tt in range(NT):
                        P.op("act", lambda e, tt=tt: e.activation(out=junk[:], in_=omla[:, tt, :], func=AF.Square,
                                                                  accum_out=ssq[:, tt:tt + 1]), [omla.b], [junk.b, ssq.b])
                    rstd = rstd_from_ssq(es, ssq, 2048, "mo")
                    for tt in range(NT):
                        P.op("dve", lambda e, tt=tt: e.tensor_scalar(out=junk[:], in0=omla[:, tt, :], scalar1=rstd[:, tt:tt + 1],
                                                                     scalar2=None, op0=ALU.mult), [omla.b, rstd.b], [junk.b])
                        transposes(Src(junk, lambda kc: junk[:, kc * 128:(kc + 1) * 128]), 16, mixedT, tt, V_MO)
                    P.emit()

        def own_gla(i, nT, mixedT):
            with contextlib.ExitStack() as oes:
                r = gla_alloc(oes, True)
                with contextlib.ExitStack() as es:
                    wb = [sb(es, "wb%d" % k, [128, 32, 512], BF16) for k in range(2)]
                    gla_proj(r, wb, nT, True)
                    P.emit()
                ost = sb(oes, "ost", [128, 8, 512], F32)
                ostbf = sb(oes, "ostbf", [128, 8, 512], BF16)
                bank_lo[0] = 4
                for tt in range(NT):
                    with contextlib.ExitStack() as es:
                        if tt == 0:
                            P.op("dve", lambda e: e.tensor_copy(out=ost[:].rearrange("p a b -> p (a b)"),
                                                                in_=snap[:].rearrange("p a b -> p (a b)")), [snap.b], [ost.b])
                            P.op("act", lambda e: e.copy(out=ostbf[:].rearrange("p a b -> p (a b)"),
                                                         in_=snap[:].rearrange("p a b -> p (a b)")), [snap.b], [ostbf.b])
                        gla_tile(es, tt, r, ost, {"ostbf": ostbf, "mixedT": mixedT}, "o%d" % tt)
                        P.emit()
                bank_lo[0] = 0

        def linear_res(wb, es, actT, KC, wdram, src, dst, tagn):
            rb = [sb(es, "rb%d" % k, [128, 512], F32) for k in range(2)]
            ob = [sb(es, "ob%d" % k, [128, 512], F32) for k in range(2)]
            dstb = Buf(tagn)
            cnt = [0]

            def ev(ci, tt, pbk, csz):
                k = cnt[0] % 2
                cnt[0] += 1
                P.op("sp", lambda e: e.dma_start(out=rb[k][:], in_=src[tt * 128:(tt + 1) * 128, ci * 512:(ci + 1) * 512]),
                     [], [rb[k].b], dma=True)
                P.op("dve", lambda e: e.tensor_tensor(out=ob[k][:], in0=pbk[:, 0:512], in1=rb[k][:], op=ALU.add),
                     [pbk.b, rb[k].b], [ob[k].b])
                P.op("sp", lambda e: e.dma_start(out=dst[tt * 128:(tt + 1) * 128, ci * 512:(ci + 1) * 512], in_=ob[k][:]),
                     [ob[k].b], [dstb], dma=True, nowaw=True, semname=tagn + "_%d" % k)
            linear(wb, actT, KC, wcols(wdram, 0), [512] * 8, NT, ev)

        def own_wout(i, mixedT):
            with contextlib.ExitStack() as es:
                wb = [sb(es, "wb%d" % k, [128, 32, 512], BF16) for k in range(2)]
                linear_res(wb, es, mixedT, 32, w_out_b, x_own[i * TG:(i + 1) * TG, :], hA, "hA")
                P.emit()

        def own_cross(i):
            with contextlib.ExitStack() as oes:
                nT2 = sb(oes, "nT2", [128, 32, TG], BF16)
                stage_norm(lambda tt: hA[tt * 128:(tt + 1) * 128, :], NT, V_CROSS, nT2, "cr")
                ocT = sb(oes, "ocT", [128, 8, TG], BF16)
                with contextlib.ExitStack() as es:
                    wb = [sb(es, "wb%d" % k, [128, 32, 512], BF16) for k in range(2)]
                    qcT = sb(es, "qcT", [128, 8, TG], BF16)

                    def ev_q(cb, pbk):
                        P.op("act", lambda e: e.copy(out=qcT[:, cb, :], in_=pbk[:, 0:TG]), [pbk.b], [qcT.b])
                    linearT(wb, nT2, 32, wcols(w_cq_b, 0), 2, TG, ev_q)
                    pT2 = sb(es, "pT2", [128, 2, TG], BF16)
                    oc = sb(es, "oc", [128, NT, 1024], BF16)
                    rs = sb(es, "rs2", [128, 1], F32)
                    for h in range(4):
                        for mc in range(2):
                            pS = bank()
                            for hf in range(2):
                                P.op("pe", lambda e, h=h, mc=mc, hf=hf, pS=pS: e.matmul(
                                    pS[:, 0:TG], lhsT=KmT[:, h * 2 + hf, mc * 128:(mc + 1) * 128], rhs=qcT[:, h * 2 + hf, :],
                                    start=(hf == 0), stop=(hf == 1)), [KmT.b, qcT.b], [pS.b])
                            P.op("act", lambda e, mc=mc, pS=pS: e.activation(out=pT2[:, mc, :], in_=pS[:, 0:TG], func=AF.Exp,
                                                                            scale=1.0 / 16), [pS.b], [pT2.b])
                        for tt in range(NT):
                            po = bank()
                            for mc in range(2):
                                P.op("pe", lambda e, h=h, mc=mc, tt=tt, po=po: e.matmul(
                                    po[:, 0:257], lhsT=pT2[:, mc, tt * 128:(tt + 1) * 128], rhs=Vm[:, mc, h, 0:257],
                                    start=(mc == 0), stop=(mc == 1)), [pT2.b, Vm.b], [po.b])
                            P.op("dve", lambda e, po=po: e.reciprocal(out=rs[:], in_=po[:, 256:257]), [po.b], [rs.b])
                            P.op("dve", lambda e, po=po, h=h, tt=tt: e.tensor_scalar(
                                out=oc[:, tt, h * 256:(h + 1) * 256], in0=po[:, 0:256], scalar1=rs[:, 0:1], scalar2=None,
                                op0=ALU.mult), [po.b, rs.b], [oc.b])
                    for tt in range(NT):
                        transposes(Src(oc, lambda kc, tt=tt: oc[:, tt, kc * 128:(kc + 1) * 128]), 8, ocT, tt)
                    P.emit()
                with contextlib.ExitStack() as es:
                    wb = [sb(es, "wb%d" % k, [128, 32, 512], BF16) for k in range(2)]
                    linear_res(wb, es, ocT, 8, w_co_b, hA, hB, "hB")
                    P.emit()

        def own_peer(i):
            with contextlib.ExitStack() as oes:
                x3T = sb(oes, "x3T", [128, 32, TG], BF16)
                stage_norm(lambda tt: hB[tt * 128:(tt + 1) * 128, :], NT, V_FFN, x3T, "pe")
                s2 = sb(oes, "s2", [128, NT, 8, 128], F32)
                thr1 = sb(oes, "thr1", [128, NT, 8, 128], F32)
                w2 = sb(oes, "w2", [128, NT, 8, 128], BF16)
                w1c = sb(oes, "w1c", [128, NT, 8, 128], F32)
                with contextlib.ExitStack() as es:
                    qpT = sb(es, "qpT", [128, 16, TG], BF16)
                    with contextlib.ExitStack() as es2:
                        wb = [sb(es2, "wb%d" % k, [128, 32, 512], BF16) for k in range(2)]

                        def ev_q(cb, pbk):
                            P.op("act", lambda e: e.copy(out=qpT[:, cb, :], in_=pbk[:, 0:TG]), [pbk.b], [qpT.b])
                        linearT(wb, x3T, 32, wcols(w_pq_b, 0), 4, TG, ev_q)
                        P.emit()
                    sc = sb(es, "sc", [128, 16, 128], F32)
                    scr = sb(es, "scr", [128, 16, 128], F32)
                    v = sb(es, "v16", [128, 16, 16], F32)
                    cand = sb(es, "cand", [128, 8, 16, 16], F32)
                    cand2 = sb(es, "cand2", [128, 8, 256], F32)
                    cs_ = sb(es, "cs16", [128, 8, 16], F32)
                    small = sb(es, "small", [128, 8, 8], F32)
                    ejunk = sb(es, "ejunk", [128, 16], F32)
                    for tt in range(NT):
                        for q4 in range(4):
                            pS = bank()
                            for k in range(4):
                                hp = q4 * 4 + k
                                P.op("pe", lambda e, hp=hp, k=k, tt=tt, pS=pS: e.matmul(
                                    pS[:, k * 128:(k + 1) * 128], lhsT=qpT[:, hp, tt * 128:(tt + 1) * 128], rhs=subkT[:, hp, :],
                                    start=True, stop=True), [qpT.b, subkT.b], [pS.b])
                            P.op("act", lambda e, q4=q4, pS=pS: e.copy(out=sc[:, q4 * 4:(q4 + 1) * 4, :].rearrange("p a b -> p (a b)"),
                                                                       in_=pS[:, 0:512]), [pS.b], [sc.b])
                        for hp in range(16):
                            P.op("dve", lambda e, hp=hp: e.max(out=v[:, hp, 0:8], in_=sc[:, hp, :]), [sc.b], [v.b])
                            P.op("dve", lambda e, hp=hp: e.match_replace(out=scr[:, hp, :], in_to_replace=v[:, hp, 0:8],
                                                                         in_values=sc[:, hp, :], imm_value=-1e30), [sc.b, v.b], [scr.b])
                            P.op("dve", lambda e, hp=hp: e.max(out=v[:, hp, 8:16], in_=scr[:, hp, :]), [scr.b], [v.b])
                        vv = v[:].rearrange("p (h t) k -> p h t k", t=2)
                        P.op("dve", lambda e: e.tensor_tensor(
                            out=cand[:], in0=vv[:, :, 0, :].unsqueeze(3).to_broadcast([128, 8, 16, 16]),
                            in1=vv[:, :, 1, :].unsqueeze(2).to_broadcast([128, 8, 16, 16]), op=ALU.add), [v.b], [cand.b])
                        for h in range(8):
                            cf = cand[:, h].rearrange("p a b -> p (a b)")
                            P.op("dve", lambda e, h=h, cf=cf: e.max(out=cs_[:, h, 0:8], in_=cf), [cand.b], [cs_.b])
                            P.op("dve", lambda e, h=h, cf=cf: e.match_replace(out=cand2[:, h, :], in_to_replace=cs_[:, h, 0:8],
                                                                             in_values=cf, imm_value=-1e30), [cand.b, cs_.b], [cand2.b])
                            P.op("dve", lambda e, h=h: e.max(out=cs_[:, h, 8:16], in_=cand2[:, h, :]), [cand2.b], [cs_.b])
                        P.op("dve", lambda e: e.tensor_scalar(out=small[:, :, 0], in0=cs_[:, :, 15], scalar1=-1e-4, scalar2=None,
                                                              op0=ALU.add), [cs_.b], [small.b])
                        P.op("dve", lambda e: e.tensor_scalar(out=small[:, :, 1], in0=cs_[:, :, 0], scalar1=-1.0, scalar2=None,
                                                              op0=ALU.mult), [cs_.b], [small.b])
                        for h in range(8):
                            P.op("act", lambda e, h=h: e.activation(out=ejunk[:], in_=cs_[:, h, :], func=AF.Exp, bias=small[:, h, 1:2],
                                                                    accum_out=small[:, h, 2:3]), [cs_.b, small.b], [ejunk.b, small.b])
                        P.op("dve", lambda e: e.reciprocal(out=small[:, :, 3], in_=small[:, :, 2]), [small.b], [small.b])
                        for h in range(8):
                            P.op("dve", lambda e, h=h, tt=tt: e.tensor_copy(out=s2[:, tt, h, :], in_=sc[:, 2 * h + 1, :]), [sc.b], [s2.b])
                            P.op("act", lambda e, h=h, tt=tt: e.activation(out=w2[:, tt, h, :], in_=sc[:, 2 * h + 1, :], func=AF.Exp,
                                                                          bias=small[:, h, 1:2]), [sc.b, small.b], [w2.b])
                            P.op("act", lambda e, h=h, tt=tt: e.activation(out=scr[:, h, :], in_=sc[:, 2 * h, :], func=AF.Exp),
                                 [sc.b], [scr.b])
                            P.op("dve", lambda e, h=h, tt=tt: e.tensor_scalar(out=w1c[:, tt, h, :], in0=scr[:, h, :],
                                                                             scalar1=small[:, h, 3:4], scalar2=None, op0=ALU.mult),
                                 [scr.b, small.b], [w1c.b])
                            P.op("dve", lambda e, h=h, tt=tt: e.tensor_scalar(out=thr1[:, tt, h, :], in0=sc[:, 2 * h, :], scalar1=-1.0,
                                                                             scalar2=small[:, h, 0:1], op0=ALU.mult, op1=ALU.add),
                                 [sc.b, small.b], [thr1.b])
                    P.emit()
                outacc = sb(oes, "outacc", [128, NT, D], F32)
                with contextlib.ExitStack() as es:
                    ub = [sb(es, "ub%d" % k, [128, 32, 256], BF16) for k in range(2)]
                    vb = [sb(es, "vb%d" % k, [128, 16, 256], BF16) for k in range(2)]
                    NGH = 16
                    ghs = [sb(es, "gh%d" % k, [128, 128], BF16) for k in range(NGH)]
                    dgs = [sb(es, "dg%d" % k, [128, 128], BF16) for k in range(NGH)]
                    gls = [sb(es, "gl%d" % k, [128, TG], F32) for k in range(2)]
                    ATs = [sb(es, "AT%d" % k, [128, 16, TG], BF16) for k in range(2)]
                    puT = peer_uT.rearrange("(c p) n -> p c n", p=128)
                    pvv = peer_v.rearrange("(c p) n -> p c n", p=128)
                    cnts = {"u": 0, "v": 0, "a": 0, "g": 0}
                    ublk = {}
                    pGs = {}

                    puT_f = peer_uT.rearrange("(c p) n -> p c n", p=128)
                    pv_f = peer_v.rearrange("(c p) n -> p c n", p=128)
                    dbu, dbv = Buf("cv_u"), Buf("cv_v")

                    def load_u(blk):
                        w = ub[cnts["u"] % 2]
                        cnts["u"] += 1
                        if i == 0:
                            P.op("pool", lambda e, w=w, blk=blk: e.dma_start(out=w[:], in_=puT_f[:, :, blk * 256:(blk + 1) * 256]),
                                 [], [w.b], dma=True)
                            P.op("sp", lambda e, w=w, blk=blk: e.dma_start(out=Ubf[blk], in_=w[:].rearrange("p c n -> p (c n)")),
                                 [w.b], [dbu], dma=True, nowaw=True, semname="cvu_" + w.b.name)
                        else:
                            P.op("pool", lambda e, w=w, blk=blk: e.dma_start(
                                out=w[:].rearrange("p c n -> p (c n)"), in_=Ubf[blk]), [], [w.b], dma=True)
                        ublk[blk] = w

                    def g_stuff(idx):
                        pG = pb[idx % 2]
                        pGs[idx] = pG
                        first = True
                        for tt in range(NT):
                            for h in range(8):
                                gh, dg = ghs[cnts["g"] % NGH], dgs[cnts["g"] % NGH]
                                cnts["g"] += 1
                                P.op("dve", lambda e, h=h, tt=tt, gh=gh: e.scalar_tensor_tensor(
                                    out=gh[:], in0=s2[:, tt, h, :], scalar=thr1[:, tt, h, idx:idx + 1], in1=w2[:, tt, h, :],
                                    op0=ALU.is_ge, op1=ALU.mult), [s2.b, thr1.b, w2.b], [gh.b])
                                P.op("act", lambda e, h=h, tt=tt, dg=dg: e.activation(
                                    out=dg[:], in_=identb[:], func=AF.Copy, scale=w1c[:, tt, h, idx:idx + 1]),
                                    [identb.b, w1c.b], [dg.b])
                                P.op("pe", lambda e, h=h, tt=tt, gh=gh, dg=dg, first=first: e.matmul(
                                    pG[:, tt * 128:(tt + 1) * 128], lhsT=gh[:], rhs=dg[:], start=first, stop=(h == 7),
                                    skip_group_check=True), [gh.b, dg.b], [pG.b])
                                first = False

                    def u_mm(idx):
                        blk, sub = divmod(idx, 2)
                        if sub == 0:
                            if blk not in ublk:
                                load_u(blk)
                            if blk + 1 < 64:
                                load_u(blk + 1)
                        w = ublk[blk]
                        pH = bank()
                        for kc in range(32):
                            P.op("pe", lambda e, kc=kc: e.matmul(
                                pH[:, 0:TG], lhsT=w[:, kc, sub * 128:(sub + 1) * 128], rhs=x3T[:, kc, :],
                                start=(kc == 0), stop=(kc == 31)), [x3T.b, w.b], [pH.b])
                        return pH

                    def a_mult(idx, pH):
                        eg, eb_ = divmod(idx, 16)
                        AT = ATs[eg % 2]
                        gl = gls[cnts["a"] % 2]
                        cnts["a"] += 1
                        pG = pGs.pop(idx)
                        P.op("act", lambda e: e.activation(out=gl[:], in_=pH[:, 0:TG], func=AF.Gelu), [pH.b], [gl.b])
                        P.op("dve", lambda e: e.tensor_tensor(out=AT[:, eb_, :], in0=pG[:, 0:TG], in1=gl[:], op=ALU.mult),
                             [gl.b, pG.b], [AT.b])

                    def v_phase(eg):
                        AT = ATs[eg % 2]
                        for db in range(16):
                            w = vb[cnts["v"] % 2]
                            cnts["v"] += 1
                            if i == 0:
                                P.op("pool", lambda e, w=w, db=db: e.dma_start(
                                    out=w[:], in_=pv_f[:, eg * 16:(eg + 1) * 16, db * 256:(db + 1) * 256]), [], [w.b], dma=True)
                                P.op("sp", lambda e, w=w, pc=eg * 16 + db: e.dma_start(
                                    out=Vbf[pc], in_=w[:].rearrange("p c n -> p (c n)")), [w.b], [dbv], dma=True, nowaw=True,
                                    semname="cvv_" + w.b.name)
                            else:
                                P.op("pool", lambda e, w=w, pc=eg * 16 + db: e.dma_start(
                                    out=w[:].rearrange("p c n -> p (c n)"), in_=Vbf[pc]), [], [w.b], dma=True)
                            for tt in range(NT):
                                pO = bank()
                                for eb_ in range(16):
                                    P.op("pe", lambda e, eb_=eb_, tt=tt, w=w, pO=pO: e.matmul(
                                        pO[:, 0:256], lhsT=AT[:, eb_, tt * 128:(tt + 1) * 128], rhs=w[:, eb_, :],
                                        start=(eb_ == 0), stop=(eb_ == 15)), [AT.b, w.b], [pO.b])
                                o = outacc[:, tt, db * 256:(db + 1) * 256]
                                if eg == 0:
                                    P.op("act", lambda e, o=o, pO=pO: e.copy(out=o, in_=pO[:, 0:256]), [pO.b], [outacc.b])
                                else:
                                    P.op("dve", lambda e, o=o, pO=pO: e.tensor_tensor(out=o, in0=pO[:, 0:256], in1=o, op=ALU.add),
                                         [pO.b, outacc.b], [outacc.b])

                    bank_lo[0] = 2
                    g_stuff(0)
                    for idx in range(128):
                        pH = u_mm(idx)
                        if idx + 1 < 128:
                            g_stuff(idx + 1)
                        a_mult(idx, pH)
                        if idx % 16 == 15:
                            v_phase(idx // 16)
                    bank_lo[0] = 0
                    P.emit()
                with contextlib.ExitStack() as es:
                    gfin = sb(es, "gfin", [128, D], F32)
                    P.op("sp", lambda e: e.dma_start(out=gfin[:], in_=gfin_in.partition_broadcast(128)), [], [gfin.b], dma=True)
                    hb_ = sb(es, "hbt", [128, D], F32)
                    junk = sb(es, "junk5", [128, D], BF16)
                    ssq = sb(es, "ssq5", [128, NT], F32)
                    outb = Buf("out")
                    for tt in range(NT):
                        P.op("sp", lambda e, tt=tt: e.dma_start(out=hb_[:], in_=hB[tt * 128:(tt + 1) * 128, :]), [], [hb_.b], dma=True)
                        P.op("dve", lambda e, tt=tt: e.tensor_tensor(out=outacc[:, tt, :], in0=outacc[:, tt, :], in1=hb_[:], op=ALU.add),
                             [outacc.b, hb_.b], [outacc.b])
                        P.op("act", lambda e, tt=tt: e.activation(out=junk[:], in_=outacc[:, tt, :], func=AF.Square,
                                                                  accum_out=ssq[:, tt:tt + 1]), [outacc.b], [junk.b, ssq.b])
                    rstd = rstd_from_ssq(es, ssq, D, "fin")
                    for tt in range(NT):
                        P.op("dve", lambda e, tt=tt: e.scalar_tensor_tensor(
                            out=outacc[:, tt, :], in0=outacc[:, tt, :], scalar=rstd[:, tt:tt + 1], in1=gfin[:],
                            op0=ALU.mult, op1=ALU.mult), [outacc.b, rstd.b, gfin.b], [outacc.b])
                        P.op("sp", lambda e, tt=tt: e.dma_start(out=out[i * TG + tt * 128: i * TG + (tt + 1) * 128, :], in_=outacc[:, tt, :]),
                             [outacc.b], [outb], dma=True, nowaw=True)
                    P.emit()

        stop_after = dbg if isinstance(dbg, str) else None
        for i in range(NOWN):
            for rr in range(4):
                g = 4 * i + rr
                with contextlib.ExitStack() as gs:
                    nT = sb(gs, "nT", [128, 32, TG], BF16)
                    stage_norm(lambda tt, g=g: x_all[g * TG + tt * 128: g * TG + (tt + 1) * 128, :], NT, V_MIX, nT, "sw")
                    sweep_gla(g, nT)
            with contextlib.ExitStack() as gs:
                mixedT = sb(gs, "mixedT", [128, 32, TG], BF16)
                nT = sb(gs, "nT", [128, 32, TG], BF16)
                stage_norm(lambda tt, i=i: x_own[i * TG + tt * 128: i * TG + (tt + 1) * 128, :], NT, V_MIX, nT, "ow")
                own_mla(i, nT, mixedT)
                own_gla(i, nT, mixedT)
                own_wout(i, mixedT)
            if stop_after == "mixer":
                P.op("sp", lambda e, i=i: e.dma_start(out=out[i * TG:(i + 1) * TG, :], in_=hA), [], [Buf("out")], dma=True)
                P.emit()
                continue
            own_cross(i)
            if stop_after == "cross":
                P.op("sp", lambda e, i=i: e.dma_start(out=out[i * TG:(i + 1) * TG, :], in_=hB), [], [Buf("out")], dma=True)
                P.emit()
                continue
            own_peer(i)
    return nc


def host_consts():
    cst = np.zeros((128, 1024), np.float32)
    cst[:, 0:128] = np.eye(128)
    j = np.arange(128)[:, None]
    c = np.arange(128)[None, :]
    same = (j // 64) == (c // 64)
    cst[:, 128:256] = np.where(same & (j > c), -1.0 / 16, 0.0)
    cst[:, 256:384] = np.where(same & (j <= c), -1.0 / 16, 0.0)
    cst[:, 384:512] = np.where(same & (j <= c), 1.0, 0.0)
    cst[:, 512] = np.where(np.arange(128) < 64, -1.0 / 16, 0.0)
    cst[:, 513] = np.where(np.arange(128) >= 64, -1.0 / 16, 0.0)
    inv = 10000.0 ** (-np.arange(0, 64, 2, dtype=np.float32) / 64)
    cst[:, 514:546] = (inv / (2 * np.pi))[None, :]
    cst[:, 640:768] = np.where(j <= c, 1.0, 0.0)
    return cst


def tvec(g):
    g = np.asarray(g, np.float32).reshape(-1)
    return np.ascontiguousarray(g.reshape(-1, 128).T)


def core_inputs(b, j, S, NOWN, x, mem, positions, norm_mem, norm_mix, w_in, mla_q_norm, w_uq, mla_kv_norm, w_ukv,
                mla_out_norm, w_gate_up, b_gate, gla_out_norm, w_out, norm_cross, w_cq, w_ck, w_cv, w_co,
                norm_ffn, w_peer_q, peer_sub_keys, peer_u, peer_v, norm_final, shared):
    f = lambda a: np.ascontiguousarray(np.asarray(a, np.float32))
    xb = f(x[b])
    own_rows = np.concatenate([np.arange((4 * i + j) * TG, (4 * i + j + 1) * TG) for i in range(NOWN)])
    pos = np.asarray(positions[b], np.int32).reshape(S, 1)
    corec = np.zeros((128, 1028), np.float32)
    corec[:, j] = 1.0
    sid = np.zeros((8, 128, 128), np.float32)
    for r in range(4):
        if r > j:
            sid[r] = np.eye(128)
        if r == j:
            sid[4 + r] = np.eye(128)
    corec[:, 4:1028] = sid.transpose(1, 0, 2).reshape(128, 1024)
    d = dict(shared)
    d.update(x_all=xb, x_own=np.ascontiguousarray(xb[own_rows]), pos_all=pos, pos_own=np.ascontiguousarray(pos[own_rows]),
             mem=f(mem[b]), corec=corec)
    return d


def shared_inputs(norm_mem, norm_mix, w_in, mla_q_norm, w_uq, mla_kv_norm, w_ukv, mla_out_norm, w_gate_up, b_gate,
                  gla_out_norm, w_out, norm_cross, w_cq, w_ck, w_cv, w_co, norm_ffn, w_peer_q, peer_sub_keys,
                  peer_u, peer_v, norm_final):
    f = lambda a: np.ascontiguousarray(np.asarray(a, np.float32))
    vecs = np.zeros((128, 160), np.float32)
    vecs[:, 0:32] = tvec(norm_mix[0])
    vecs[:, 32:64] = tvec(norm_cross[0])
    vecs[:, 64:96] = tvec(norm_ffn[0])
    vecs[:, 96:128] = tvec(norm_mem)
    vecs[:, 128:136] = tvec(mla_q_norm[0])
    vecs[:, 136:140] = tvec(mla_kv_norm[0])
    vecs[:, 140:156] = tvec(mla_out_norm[0])
    vecs[:, 156:160] = tvec(gla_out_norm[0])
    wg = np.concatenate([f(w_gate_up[0]), f(b_gate[0]).reshape(1, 1024)], axis=0)
    sk = np.asarray(peer_sub_keys[0], np.float32).reshape(16, 128, 128)
    subk = np.ascontiguousarray(sk.transpose(2, 0, 1))
    return dict(w_in=f(w_in[0]), w_uq=f(w_uq[0]), w_ukv=f(w_ukv[0]), wg=wg, w_out=f(w_out[0]), w_cq=f(w_cq[0]),
                w_ck=f(w_ck[0]), w_cv=f(w_cv[0]), w_co=f(w_co[0]), w_pq=f(w_peer_q[0]), subk=subk,
                peer_uT=np.ascontiguousarray(np.asarray(peer_u[0], np.float32).T), peer_v=f(peer_v[0]),
                vecs=vecs, gfin=f(norm_final).reshape(1, D), cst=host_consts())


def kernel(x, mem, positions, norm_mem, norm_mix, w_in, mla_q_norm, w_uq, mla_kv_norm, w_ukv,
           mla_out_norm, w_gate_up, b_gate, gla_out_norm, w_out, norm_cross, w_cq, w_ck, w_cv, w_co,
           norm_ffn, w_peer_q, peer_sub_keys, peer_u, peer_v, norm_final):
    x = np.asarray(x)
    B, S, _ = x.shape
    NOWN = S // TG // 4
    shared = shared_inputs(norm_mem, norm_mix, w_in, mla_q_norm, w_uq, mla_kv_norm, w_ukv, mla_out_norm, w_gate_up,
                           b_gate, gla_out_norm, w_out, norm_cross, w_cq, w_ck, w_cv, w_co, norm_ffn, w_peer_q,
                           peer_sub_keys, peer_u, peer_v, norm_final)
    in_maps = []
    for c in range(8):
        b, j = divmod(c, 4)
        in_maps.append(core_inputs(b, j, S, NOWN, x, mem, positions, norm_mem, norm_mix, w_in, mla_q_norm, w_uq,
                                   mla_kv_norm, w_ukv, mla_out_norm, w_gate_up, b_gate, gla_out_norm, w_out, norm_cross,
                                   w_cq, w_ck, w_cv, w_co, norm_ffn, w_peer_q, peer_sub_keys, peer_u, peer_v, norm_final,
                                   shared))
    nc = build(S, NOWN)
    res = run_bass_kernel_spmd(nc, in_maps, core_ids=list(range(8)))
    outp = np.zeros((B, S, D), np.float32)
    for c in range(8):
        b, j = divmod(c, 4)
        o = np.asarray(res.results[c]["out"])
        for i in range(NOWN):
            g = 4 * i + j
            outp[b, g * TG:(g + 1) * TG] = o[i * TG:(i + 1) * TG]
    return outp
```

```python
import contextlib
import numpy as np
import concourse.bass as bass
import concourse.mybir as mybir
from concourse.bass_utils import run_bass_kernel_spmd

F32 = mybir.dt.float32
BF16 = mybir.dt.bfloat16
I32 = mybir.dt.int32
AF = mybir.ActivationFunctionType
ALU = mybir.AluOpType

D = 4096
NT = 2
TG = NT * 128
EPS = 1e-6
NEG = -30000.0


class Buf:
    __slots__ = ("name", "w", "rd")

    def __init__(self, name):
        self.name = name
        self.w = None
        self.rd = []


class Op:
    __slots__ = ("eng", "fn", "deps", "is_dma", "semkey", "count", "needs_inc", "idx")


class Prog:
    ENGS = ("pe", "act", "dve", "pool", "sp")

    def __init__(self, nc, es):
        self.nc = nc
        self.es = es
        self.ops = []
        self.start = 0
        self.sems = {}
        self.counts = {}
        self.waited = {e: {} for e in self.ENGS}

    def op(self, eng, fn, reads=(), writes=(), dma=False, nowaw=False, semname=None):
        o = Op()
        o.eng, o.fn, o.is_dma, o.idx = eng, fn, dma, len(self.ops)
        o.needs_inc, o.count = False, None
        deps = {}
        for b in reads:
            if b.w is not None and b.w >= self.start:
                deps[b.w] = "raw"
        for b in writes:
            if b.w is not None and b.w >= self.start and b.w not in deps:
                if not (nowaw and dma and self.ops[b.w].is_dma and not b.rd):
                    deps[b.w] = "waw"
            last = {}
            for r in b.rd:
                if r < self.start:
                    continue
                ro = self.ops[r]
                if ro.is_dma:
                    if r not in deps:
                        deps[r] = "war"
                else:
                    last[ro.eng] = max(last.get(ro.eng, -1), r)
            for r in last.values():
                if r not in deps:
                    deps[r] = "war"
        o.deps = []
        for d, kind in deps.items():
            po = self.ops[d]
            if (not dma) and (not po.is_dma) and po.eng == eng and (kind != "raw" or eng == "pe"):
                continue
            o.deps.append(d)
        if dma:
            o.semkey = ("dma", semname or writes[0].name)
        else:
            o.semkey = ("eng", eng)
        for b in writes:
            b.w = o.idx
            b.rd = []
        for b in reads:
            b.rd.append(o.idx)
        self.ops.append(o)
        return o.idx

    def _sem(self, k):
        if k not in self.sems:
            self.sems[k] = self.es.enter_context(self.nc.semaphore("s%d" % len(self.sems)))
        return self.sems[k]

    def emit(self, final_waits=()):
        nc = self.nc
        ops = self.ops
        cur = ops[self.start:]
        for o in cur:
            for d in o.deps:
                ops[d].needs_inc = True
        for d in final_waits:
            ops[d].needs_inc = True
        for o in cur:
            if o.is_dma:
                self.counts[o.semkey] = self.counts.get(o.semkey, 0) + 16
                o.count = self.counts[o.semkey]
                self._sem(o.semkey)
            elif o.needs_inc:
                self.counts[o.semkey] = self.counts.get(o.semkey, 0) + 1
                o.count = self.counts[o.semkey]
                self._sem(o.semkey)
        per = {e: [o for o in cur if o.eng == e] for e in self.ENGS}
        dma_last = {}
        for o in cur:
            if o.is_dma:
                dma_last[o.semkey] = o.count
        sems = self.sems

        def run(engname, eng):
            waited = self.waited[engname]
            for o in per[engname]:
                need = {}
                for d in o.deps:
                    po = ops[d]
                    if need.get(po.semkey, 0) < po.count:
                        need[po.semkey] = po.count
                for k, v in need.items():
                    if waited.get(k, 0) >= v:
                        continue
                    eng.wait_ge(sems[k], v)
                    waited[k] = v
                ins = o.fn(eng)
                if o.is_dma:
                    ins.then_inc(sems[o.semkey], 16)
                elif o.needs_inc:
                    ins.then_inc(sems[o.semkey], 1)
            if engname == "sp":
                for k, v in dma_last.items():
                    if waited.get(k, 0) < v:
                        eng.wait_ge(sems[k], v)
                        waited[k] = v

        with nc.Block(no_gpsimd_drain=True) as block:
            @block.tensor
            def _(e):
                run("pe", e)

            @block.scalar
            def _(e):
                run("act", e)

            @block.vector
            def _(e):
                run("dve", e)

            @block.gpsimd
            def _(e):
                run("pool", e)

            @block.sync
            def _(e):
                run("sp", e)
        self.start = len(ops)


class TB:
    def __init__(self, t, name):
        self.t = t
        self.b = Buf(name)

    def __getitem__(self, k):
        return self.t[k]


def build(S, NOWN, dbg=False):
    NGRP = S // TG
    assert NGRP == 4 * NOWN
    nc = bass.Bass("TRN2", target_bir_lowering=False)

    def din(name, shape, dt=F32):
        return nc.dram_tensor(name, list(shape), dt, kind="ExternalInput").ap()

    x_all = din("x_all", [S, D])
    x_own = din("x_own", [NOWN * TG, D])
    pos_all = din("pos_all", [S, 1], I32)
    pos_own = din("pos_own", [NOWN * TG, 1], I32)
    mem = din("mem", [256, D])
    w_in = din("w_in", [D, 7760])
    w_uq = din("w_uq", [1024, 3072])
    w_ukv = din("w_ukv", [512, 4096])
    wg_in = din("wg", [17, 1024])
    w_out = din("w_out", [D, D])
    w_cq = din("w_cq", [D, 1024])
    w_ck = din("w_ck", [D, 1024])
    w_cv = din("w_cv", [D, 1024])
    w_co = din("w_co", [1024, D])
    w_pq = din("w_pq", [D, 2048])
    subk = din("subk", [128, 16, 128])
    peer_uT = din("peer_uT", [D, 16384])
    peer_v = din("peer_v", [16384, D])
    vecs_in = din("vecs", [128, 160])
    gfin_in = din("gfin", [1, D])
    cst_in = din("cst", [128, 1024])
    core_in = din("corec", [128, 1028])
    out = nc.dram_tensor("out", [NOWN * TG, D], F32, kind="ExternalOutput").ap()
    KnS = nc.dram_tensor("KnS", [16, 128, S], BF16, kind="Internal").ap()
    KrS = nc.dram_tensor("KrS", [128, S], BF16, kind="Internal").ap()
    VS = nc.dram_tensor("VS", [16, 128, S // 128, 130], BF16, kind="Internal").ap()
    w_in_b = nc.dram_tensor("w_in_b", [D, 7760], BF16, kind="Internal").ap()
    w_uq_b = nc.dram_tensor("w_uq_b", [1024, 3072], BF16, kind="Internal").ap()
    w_ukv_b = nc.dram_tensor("w_ukv_b", [512, 4096], BF16, kind="Internal").ap()
    w_out_b = nc.dram_tensor("w_out_b", [D, D], BF16, kind="Internal").ap()
    w_cq_b = nc.dram_tensor("w_cq_b", [D, 1024], BF16, kind="Internal").ap()
    w_co_b = nc.dram_tensor("w_co_b", [1024, D], BF16, kind="Internal").ap()
    w_pq_b = nc.dram_tensor("w_pq_b", [D, 2048], BF16, kind="Internal").ap()
    Ubf = nc.dram_tensor("Ubf", [64, 128, 32 * 256], BF16, kind="Internal").ap()
    Vbf = nc.dram_tensor("Vbf", [128, 128, 16 * 256], BF16, kind="Internal").ap()
    hA = nc.dram_tensor("hA", [TG, D], F32, kind="Internal").ap()
    hB = nc.dram_tensor("hB", [TG, D], F32, kind="Internal").ap()
    dbg_out = None
    if dbg:
        dbg_out = nc.dram_tensor("dbg", [NOWN * TG, D], F32, kind="ExternalOutput").ap()

    with contextlib.ExitStack() as ges:
        P = Prog(nc, ges)

        uid = [0]

        def sb(es, name, shape, dt):
            uid[0] += 1
            return TB(es.enter_context(nc.sbuf_tensor("s%d_%s" % (uid[0], name), list(shape), dt)), name)

        def ps(es, name, shape, dt):
            return TB(es.enter_context(nc.psum_tensor("p_" + name, list(shape), dt)), name)

        vecs = sb(ges, "vecs", [128, 160], F32)
        cst = sb(ges, "cst", [128, 1024], F32)
        corec = sb(ges, "corec", [128, 4], F32)
        identb = sb(ges, "identb", [128, 128], BF16)
        sidb = sb(ges, "sidb", [128, 8, 128], BF16)
        negfull = sb(ges, "negfull", [128, TG], BF16)
        maskdiag = sb(ges, "maskdiag", [128, NT, TG], BF16)
        state = sb(ges, "state", [128, 8, 512], F32)
        snap = sb(ges, "snap", [128, 8, 512], F32)
        KmT = sb(ges, "KmT", [128, 8, 256], BF16)
        Vm = sb(ges, "Vm", [128, 2, 4, 258], BF16)
        subkT = sb(ges, "subkT", [128, 16, 128], BF16)
        wg = sb(ges, "wg", [32, 1024], BF16)
        glrT = sb(ges, "glrT", [32, TG], BF16)
        pb = [ps(ges, "pb%d" % i, [128, 512], F32) for i in range(6)]
        pt = [ps(ges, "pt%d" % i, [128, 1024], BF16) for i in range(2)]
        rot = {"a": 0, "t": 0}

        bank_lo = [0]

        def bank():
            n = 6 - bank_lo[0]
            rot["a"] = (rot["a"] + 1) % n
            return pb[bank_lo[0] + rot["a"]]

        def tbank():
            rot["t"] = (rot["t"] + 1) % 2
            return pt[rot["t"]]

        V_MIX, V_CROSS, V_FFN, V_MEM, V_Q, V_KV, V_MO, V_GO = 0, 32, 64, 96, 128, 136, 140, 156
        C_ID, C_M1, C_TRI, C_CM, C_CIND, C_ROPE = 0, 128, 256, 384, 512, 514

        def act_copy(o, i, reads, writes, scale=None):
            if scale is None:
                P.op("act", lambda e: e.copy(out=o, in_=i), reads, writes)
            else:
                P.op("act", lambda e: e.activation(out=o, in_=i, func=AF.Copy, scale=scale), reads, writes)

        def rstd_from_ssq(es, ssq, n, tag):
            k = ssq.t.shape[1]
            r = sb(es, "rstd_" + tag, [128, k], F32)
            P.op("dve", lambda e: e.tensor_scalar(out=r[:], in0=ssq[:], scalar1=1.0 / n, scalar2=EPS,
                                                  op0=ALU.mult, op1=ALU.add), [ssq.b], [r.b])
            P.op("act", lambda e: e.activation(out=r[:], in_=r[:], func=AF.Sqrt), [r.b], [r.b])
            P.op("dve", lambda e: e.reciprocal(out=r[:], in_=r[:]), [r.b], [r.b])
            return r

        def transposes(src, nk, dstT, tt, gcol=None, kc0=0):
            for kc in range(nk):
                tb = tbank()
                P.op("pe", lambda e, kc=kc, tb=tb: e.transpose(out=tb[:, 0:128], in_=src(kc), identity=identb[:]),
                     [src.tb.b, identb.b], [tb.b])
                o = dstT[:, kc0 + kc, tt * 128:(tt + 1) * 128]
                if gcol is None:
                    P.op("dve", lambda e, o=o, tb=tb: e.tensor_copy(out=o, in_=tb[:, 0:128]), [tb.b], [dstT.b])
                else:
                    P.op("dve", lambda e, o=o, tb=tb, c=gcol + kc: e.tensor_scalar(
                        out=o, in0=tb[:, 0:128], scalar1=vecs[:, c:c + 1], scalar2=None, op0=ALU.mult),
                        [tb.b, vecs.b], [dstT.b])

        class Src:
            def __init__(self, tb, f):
                self.tb, self.f = tb, f

            def __call__(self, kc):
                return self.f(kc)

        def norm_T(es, rows, ntile, gcol, dstT, tag, nbuf=2, xalias=None):
            if xalias is not None:
                class _V:
                    def __init__(self, ap, b):
                        self.ap, self.b = ap, b

                    def __getitem__(self, k):
                        return self.ap
                xt = [_V(xalias[:].rearrange("p a b -> p (a b)").bitcast(F32)[:, 0:D], xalias.b)]
                nbuf = 1
            else:
                xt = [sb(es, "xt%d_%s" % (i, tag), [128, D], F32) for i in range(nbuf)]
            xs = sb(es, "xs_" + tag, [128, D], BF16)
            junk = xs
            ssq = sb(es, "ssq_" + tag, [128, ntile], F32)
            for tt in range(ntile):
                x = xt[tt % nbuf]
                P.op("sp", lambda e, x=x, tt=tt: e.dma_start(out=x[:], in_=rows(tt)), [], [x.b], dma=True)
                P.op("act", lambda e, x=x, tt=tt: e.activation(out=junk[:], in_=x[:], func=AF.Square,
                                                              accum_out=ssq[:, tt:tt + 1]), [x.b], [junk.b, ssq.b])
            rstd = rstd_from_ssq(es, ssq, D, tag)
            for tt in range(ntile):
                x = xt[tt % nbuf]
                if ntile > nbuf:
                    P.op("sp", lambda e, x=x, tt=tt: e.dma_start(out=x[:], in_=rows(tt)), [], [x.b], dma=True)
                P.op("dve", lambda e, x=x, tt=tt: e.tensor_scalar(out=xs[:], in0=x[:], scalar1=rstd[:, tt:tt + 1],
                                                                 scalar2=None, op0=ALU.mult), [x.b, rstd.b], [xs.b])
                transposes(Src(xs, lambda kc: xs[:, kc * 128:(kc + 1) * 128]), 32, dstT, tt, gcol)

        wrot = {"i": 0}

        def linear(wb, actT, KC, wap, blocks, ntile, evac):
            for ci, csz in enumerate(blocks):
                wrot["i"] ^= 1
                w = wb[wrot["i"]]
                P.op("pool", lambda e, w=w, ci=ci, csz=csz: e.dma_start(out=w[:, 0:KC, 0:csz], in_=wap(ci)),
                     [], [w.b], dma=True)
                for tt in range(ntile):
                    pbk = bank()
                    for kc in range(KC):
                        P.op("pe", lambda e, kc=kc, tt=tt, w=w, pbk=pbk, csz=csz: e.matmul(
                            pbk[:, 0:csz], lhsT=actT[:, kc, tt * 128:(tt + 1) * 128], rhs=w[:, kc, 0:csz],
                            start=(kc == 0), stop=(kc == KC - 1)), [actT.b, w.b], [pbk.b])
                    evac(ci, tt, pbk, csz)

        def linearT(wb, actT, KC, wap, nblk, ncols, evac):
            for ci in range(nblk):
                wrot["i"] ^= 1
                w = wb[wrot["i"]]
                P.op("pool", lambda e, w=w, ci=ci: e.dma_start(out=w[:, 0:KC, 0:512], in_=wap(ci)), [], [w.b], dma=True)
                for cb in range(4):
                    pbk = bank()
                    for kc in range(KC):
                        P.op("pe", lambda e, kc=kc, cb=cb, w=w, pbk=pbk: e.matmul(
                            pbk[:, 0:ncols], lhsT=w[:, kc, cb * 128:(cb + 1) * 128], rhs=actT[:, kc, 0:ncols],
                            start=(kc == 0), stop=(kc == KC - 1)), [actT.b, w.b], [pbk.b])
                    evac(ci * 4 + cb, pbk)

        def wcols(wdram, c0):
            v = wdram.rearrange("(c p) n -> p c n", p=128)
            return lambda ci, c0=c0: v[:, :, c0 + ci * 512: c0 + ci * 512 + 512]

        with contextlib.ExitStack() as es:
            P.op("sp", lambda e: e.dma_start(out=vecs[:], in_=vecs_in), [], [vecs.b], dma=True)
            P.op("sp", lambda e: e.dma_start(out=cst[:], in_=cst_in), [], [cst.b], dma=True)
            P.op("sp", lambda e: e.dma_start(out=corec[:], in_=core_in[:, 0:4]), [], [corec.b], dma=True)
            sidf = sb(es, "sidf", [128, 1024], F32)
            P.op("sp", lambda e: e.dma_start(out=sidf[:], in_=core_in[:, 4:1028]), [], [sidf.b], dma=True)
            P.op("pool", lambda e: e.dma_start(out=subkT[:], in_=subk), [], [subkT.b], dma=True)
            P.op("dve", lambda e: e.memset(wg[:], 0.0), [], [wg.b])
            P.op("pool", lambda e: e.dma_start(out=wg[0:17, :], in_=wg_in), [], [wg.b], dma=True)
            P.op("dve", lambda e: e.memset(glrT[:], 1.0), [], [glrT.b])
            P.op("dve", lambda e: e.tensor_copy(out=identb[:], in_=cst[:, C_ID:C_ID + 128]), [cst.b], [identb.b])
            P.op("dve", lambda e: e.tensor_copy(out=sidb[:].rearrange("p a b -> p (a b)"), in_=sidf[:]),
                 [sidf.b], [sidb.b])
            P.op("dve", lambda e: e.memset(negfull[:], NEG), [], [negfull.b])
            P.op("dve", lambda e: e.memset(maskdiag[:], 0.0), [], [maskdiag.b])
            for kcin in range(NT):
                for qb in range(NT):
                    if qb < kcin:
                        P.op("dve", lambda e, kcin=kcin, qb=qb: e.memset(maskdiag[:, kcin, qb * 128:(qb + 1) * 128], NEG),
                             [], [maskdiag.b])
                    elif qb == kcin:
                        P.op("dve", lambda e, kcin=kcin, qb=qb: e.tensor_scalar(
                            out=maskdiag[:, kcin, qb * 128:(qb + 1) * 128], in0=cst[:, 640:768],
                            scalar1=-NEG, scalar2=NEG, op0=ALU.mult, op1=ALU.add), [cst.b], [maskdiag.b])
            P.op("dve", lambda e: e.memset(state[:], 0.0), [], [state.b])
            P.op("dve", lambda e: e.memset(snap[:], 0.0), [], [snap.b])
            P.op("dve", lambda e: e.memset(Vm[:], 1.0), [], [Vm.b])
            wb = [sb(es, "wb%d" % i, [128, 32, 512], BF16) for i in range(2)]
            mnT = sb(es, "mnT", [128, 32, 256], BF16)
            norm_T(es, lambda tt: mem[tt * 128:(tt + 1) * 128, :], 2, V_MEM, mnT, "mem")

            def ev_k(cb, pbk):
                P.op("act", lambda e: e.copy(out=KmT[:, cb, :], in_=pbk[:, 0:256]), [pbk.b], [KmT.b])
            linearT(wb, mnT, 32, wcols(w_ck, 0), 2, 256, ev_k)

            def ev_v(ci, tt, pbk, csz):
                P.op("act", lambda e: e.copy(out=Vm[:, tt, ci * 2:(ci + 1) * 2, 0:256],
                                             in_=pbk[:, 0:512].rearrange("p (h d) -> p h d", d=256)), [pbk.b], [Vm.b])
            linear(wb, mnT, 32, wcols(w_cv, 0), [512, 512], 2, ev_v)
            P.emit()

        with contextlib.ExitStack() as es:
            stg = [sb(es, "stg%d" % i, [128, 32, 512], BF16) for i in range(2)]
            cn = [0]

            def convert(src, dst, K, N):
                KC = K // 128
                sv = src.rearrange("(c p) n -> p c n", p=128)
                dv = dst.rearrange("(c p) n -> p c n", p=128)
                db_ = Buf("cv_" + str(cn[0]))
                for c0 in range(0, N, 512):
                    csz = min(512, N - c0)
                    st = stg[cn[0] % 2]
                    cn[0] += 1
                    P.op("pool", lambda e, st=st, c0=c0, csz=csz: e.dma_start(out=st[:, 0:KC, 0:csz], in_=sv[:, :, c0:c0 + csz]),
                         [], [st.b], dma=True)
                    P.op("sp", lambda e, st=st, c0=c0, csz=csz: e.dma_start(out=dv[:, :, c0:c0 + csz], in_=st[:, 0:KC, 0:csz]),
                         [st.b], [db_], dma=True, nowaw=True, semname="cvo_" + st.b.name)
            convert(w_in, w_in_b, D, 7760)
            convert(w_uq, w_uq_b, 1024, 3072)
            convert(w_ukv, w_ukv_b, 512, 4096)
            convert(w_out, w_out_b, D, D)
            convert(w_cq, w_cq_b, D, 1024)
            convert(w_co, w_co_b, 1024, D)
            convert(w_pq, w_pq_b, D, 2048)
            P.emit()

        def rope_tables(es, posrows, ntile, tag):
            cs = sb(es, "cs_" + tag, [128, ntile, 2, 32], F32)
            pi_ = sb(es, "posi_" + tag, [128, ntile], I32)
            pf = sb(es, "posf_" + tag, [128, ntile], F32)
            y = sb(es, "ry_" + tag, [128, 32], F32)
            ki = sb(es, "rk_" + tag, [128, 32], I32)
            kf = sb(es, "rkf_" + tag, [128, 32], F32)
            m = sb(es, "rm_" + tag, [128, 32], F32)
            for tt in range(ntile):
                P.op("sp", lambda e, tt=tt: e.dma_start(out=pi_[:, tt:tt + 1], in_=posrows(tt)), [], [pi_.b], dma=True)
            P.op("dve", lambda e: e.tensor_copy(out=pf[:], in_=pi_[:]), [pi_.b], [pf.b])
            for tt in range(ntile):
                for which, off in ((0, 0.25), (1, 0.0)):
                    P.op("dve", lambda e, tt=tt, off=off: e.tensor_scalar(
                        out=y[:], in0=cst[:, C_ROPE:C_ROPE + 32], scalar1=pf[:, tt:tt + 1], scalar2=off,
                        op0=ALU.mult, op1=ALU.add), [cst.b, pf.b], [y.b])
                    P.op("dve", lambda e: e.tensor_copy(out=ki[:], in_=y[:]), [y.b], [ki.b])
                    P.op("dve", lambda e: e.tensor_copy(out=kf[:], in_=ki[:]), [ki.b], [kf.b])
                    P.op("dve", lambda e: e.tensor_tensor(out=y[:], in0=y[:], in1=kf[:], op=ALU.subtract), [y.b, kf.b], [y.b])
                    P.op("dve", lambda e: e.tensor_scalar(out=m[:], in0=y[:], scalar1=0.5, scalar2=None, op0=ALU.is_gt),
                         [y.b], [m.b])
                    P.op("dve", lambda e: e.tensor_tensor(out=y[:], in0=y[:], in1=m[:], op=ALU.subtract), [y.b, m.b], [y.b])
                    P.op("dve", lambda e: e.tensor_scalar(out=m[:], in0=y[:], scalar1=-0.5, scalar2=None, op0=ALU.is_lt),
                         [y.b], [m.b])
                    P.op("dve", lambda e: e.tensor_tensor(out=y[:], in0=y[:], in1=m[:], op=ALU.add), [y.b, m.b], [y.b])
                    P.op("act", lambda e, tt=tt, which=which: e.activation(
                        out=cs[:, tt, which, :], in_=y[:], func=AF.Sin, scale=2.0 * np.pi), [y.b], [cs.b])
            return cs

        def apply_rope(es, src, dst, cs, tt, nh, tag):
            t1 = sb(es, "rt1_" + tag, [128, nh, 32], F32)
            t2 = sb(es, "rt2_" + tag, [128, nh, 32], F32)
            cosb = cs[:, tt, 0, :].unsqueeze(1).to_broadcast([128, nh, 32])
            sinb = cs[:, tt, 1, :].unsqueeze(1).to_broadcast([128, nh, 32])
            x1 = src[0][:, :, 0:32]
            x2 = src[0][:, :, 32:64]
            sbuf_src = src[1]
            P.op("dve", lambda e: e.tensor_tensor(out=t1[:], in0=x1, in1=cosb, op=ALU.mult), [sbuf_src, cs.b], [t1.b])
            P.op("dve", lambda e: e.tensor_tensor(out=t2[:], in0=x2, in1=sinb, op=ALU.mult), [sbuf_src, cs.b], [t2.b])
            P.op("dve", lambda e: e.tensor_tensor(out=dst[0][:, :, 0:32], in0=t1[:], in1=t2[:], op=ALU.subtract),
                 [t1.b, t2.b], [dst[1]])
            P.op("dve", lambda e: e.tensor_tensor(out=t1[:], in0=x2, in1=cosb, op=ALU.mult), [sbuf_src, cs.b], [t1.b])
            P.op("dve", lambda e: e.tensor_tensor(out=t2[:], in0=x1, in1=sinb, op=ALU.mult), [sbuf_src, cs.b], [t2.b])
            P.op("dve", lambda e: e.tensor_tensor(out=dst[0][:, :, 32:64], in0=t1[:], in1=t2[:], op=ALU.add),
                 [t1.b, t2.b], [dst[1]])


        w_in_v = w_in_b.rearrange("(c p) n -> p c n", p=128)

        def stage_norm(rows, ntile, gcol, dstT, tag):
            with contextlib.ExitStack() as es:
                norm_T(es, rows, ntile, gcol, dstT, tag)
                P.emit()

        def sweep_mla(g, nT, es, wb):
            if True:
                ckv = sb(es, "ckv", [128, NT, 512], F32)
                kr = sb(es, "kr", [128, NT, 1, 64], F32)

                def ev_ckv(ci, tt, pbk, csz):
                    if ci == 0:
                        P.op("act", lambda e: e.copy(out=ckv[:, tt, :], in_=pbk[:, 0:512]), [pbk.b], [ckv.b])
                    else:
                        P.op("act", lambda e: e.copy(out=kr[:, tt, 0, :], in_=pbk[:, 0:64]), [pbk.b], [kr.b])
                linear(wb, nT, 32, lambda ci: (w_in_v[:, :, 1024:1536] if ci == 0 else w_in_v[:, :, 1536:1600]),
                       [512, 64], NT, ev_ckv)
                junk = sb(es, "junk2", [128, 512], BF16)
                ssq = sb(es, "ssq2", [128, NT], F32)
                for tt in range(NT):
                    P.op("act", lambda e, tt=tt: e.activation(out=junk[:], in_=ckv[:, tt, :], func=AF.Square,
                                                              accum_out=ssq[:, tt:tt + 1]), [ckv.b], [junk.b, ssq.b])
                rstd = rstd_from_ssq(es, ssq, 512, "kv")
                ckvs = sb(es, "ckvs", [128, 512], BF16)
                ckvnT = sb(es, "ckvnT", [128, 4, TG], BF16)
                for tt in range(NT):
                    P.op("dve", lambda e, tt=tt: e.tensor_scalar(out=ckvs[:], in0=ckv[:, tt, :], scalar1=rstd[:, tt:tt + 1],
                                                                 scalar2=None, op0=ALU.mult), [ckv.b, rstd.b], [ckvs.b])
                    transposes(Src(ckvs, lambda kc: ckvs[:, kc * 128:(kc + 1) * 128]), 4, ckvnT, tt, V_KV)
                cs = rope_tables(es, lambda tt: pos_all[g * TG + tt * 128: g * TG + (tt + 1) * 128, :], NT, "sw")
                krr = sb(es, "krr", [128, 2, 1, 64], BF16)
                krT = sb(es, "krT", [128, 1, TG], BF16)
                for tt in range(NT):
                    apply_rope(es, (kr[:, tt], kr.b), (krr[:, 0], krr.b), cs, tt, 1, "k%d" % tt)
                    P.op("dve", lambda e: e.tensor_copy(out=krr[:, 1], in_=krr[:, 0]), [krr.b], [krr.b])
                    transposes(Src(krr, lambda kc: krr[:].rearrange("p a b c -> p (a b c)")), 1, krT, tt)
                P.op("sp", lambda e: e.dma_start(out=KrS[:, g * TG:(g + 1) * TG], in_=krT[:, 0, :]), [krT.b], [Buf("KrS")], dma=True)
                wkv = wb[1]
                wkv_f = wkv[:].rearrange("p a b -> p (a b)").rearrange("p (c n) -> p c n", c=4)
                P.op("pool", lambda e: e.dma_start(out=wkv_f, in_=w_ukv_b.rearrange("(c p) n -> p c n", p=128)), [], [wkv.b], dma=True)
                wkv_h = wkv_f.rearrange("p c (h t d) -> p c h t d", t=2, d=128)
                kout = sb(es, "kout", [128, 16, TG], BF16)
                for h in range(16):
                    pbk = bank()
                    for c in range(4):
                        P.op("pe", lambda e, c=c, h=h, pbk=pbk: e.matmul(pbk[:, 0:TG], lhsT=wkv_h[:, c, h, 0, :], rhs=ckvnT[:, c, :],
                                                                        start=(c == 0), stop=(c == 3)), [wkv.b, ckvnT.b], [pbk.b])
                    P.op("act", lambda e, h=h, pbk=pbk: e.copy(out=kout[:, h, :], in_=pbk[:, 0:TG]), [pbk.b], [kout.b])
                P.op("sp", lambda e: e.dma_start(out=KnS[:, :, g * TG:(g + 1) * TG].rearrange("h p s -> p h s"), in_=kout[:]),
                     [kout.b], [Buf("KnS")], dma=True)
                vout = sb(es, "vout", [128, NT, 16, 130], BF16)
                P.op("dve", lambda e: e.memset(vout[:], 1.0), [], [vout.b])
                for tt in range(NT):
                    for hb in range(4):
                        pbk = bank()
                        for c in range(4):
                            P.op("pe", lambda e, c=c, hb=hb, tt=tt, pbk=pbk: e.matmul(
                                pbk[:, 0:512].rearrange("p (h d) -> p h d", d=128), lhsT=ckvnT[:, c, tt * 128:(tt + 1) * 128],
                                rhs=wkv_h[:, c, hb * 4:(hb + 1) * 4, 1, :], start=(c == 0), stop=(c == 3)), [wkv.b, ckvnT.b], [pbk.b])
                        P.op("act", lambda e, hb=hb, tt=tt, pbk=pbk: e.copy(
                            out=vout[:, tt, hb * 4:(hb + 1) * 4, 0:128], in_=pbk[:, 0:512].rearrange("p (h d) -> p h d", d=128)),
                            [pbk.b], [vout.b])
                vsb = Buf("VS")
                for tt in range(NT):
                    P.op("sp", lambda e, tt=tt: e.dma_start(out=VS[:, :, g * NT + tt, :].rearrange("h p e -> p h e"), in_=vout[:, tt, :, :]),
                         [vout.b], [vsb], dma=True, nowaw=True)

        def gla_alloc(es, own):
            r = {"gk": sb(es, "gk", [128, NT, 1024], F32), "gv": sb(es, "gv", [128, NT, 2048], BF16)}
            if own:
                r["gq"] = sb(es, "gq", [128, NT, 1024], F32)
                r["sog"] = sb(es, "sog", [128, NT, 2048], BF16)
            return r

        def gla_proj(r, wb, nT, own):
            gk, gv = r["gk"], r["gv"]

            def ev(ci, tt, pbk, csz):
                if own and ci < 2:
                    P.op("act", lambda e: e.copy(out=r["gq"][:, tt, ci * 512:(ci + 1) * 512], in_=pbk[:, 0:512]), [pbk.b], [r["gq"].b])
                    return
                c2 = ci - (2 if own else 0)
                if c2 < 2:
                    P.op("act", lambda e: e.copy(out=gk[:, tt, c2 * 512:(c2 + 1) * 512], in_=pbk[:, 0:512]), [pbk.b], [gk.b])
                elif c2 < 6:
                    P.op("act", lambda e: e.copy(out=gv[:, tt, (c2 - 2) * 512:(c2 - 1) * 512], in_=pbk[:, 0:512]), [pbk.b], [gv.b])
                else:
                    P.op("act", lambda e: e.activation(out=r["sog"][:, tt, (c2 - 6) * 512:(c2 - 5) * 512], in_=pbk[:, 0:512],
                                                       func=AF.Silu), [pbk.b], [r["sog"].b])
            c0 = 1600 if own else 2624
            nblk = (8 if own else 6)
            blocks = [512] * nblk
            if own:
                def wap(ci):
                    if ci < 8:
                        return w_in_v[:, :, 1600 + ci * 512:1600 + (ci + 1) * 512]
                    return w_in_v[:, :, 5712 + (ci - 8) * 512:5712 + (ci - 7) * 512]
                linear(wb, nT, 32, wap, [512] * 12, NT, ev)
            else:
                linear(wb, nT, 32, lambda ci: w_in_v[:, :, 2624 + ci * 512:2624 + (ci + 1) * 512], [512] * 6, NT, ev)
            w = wb[0]
            P.op("pool", lambda e: e.dma_start(out=w[:, 0:32, 0:16], in_=w_in_v[:, :, 5696:5712]), [], [w.b], dma=True)
            pbk = bank()
            for kc in range(32):
                P.op("pe", lambda e, kc=kc: e.matmul(pbk[0:16, 0:TG], lhsT=w[:, kc, 0:16], rhs=nT[:, kc, :],
                                                     start=(kc == 0), stop=(kc == 31)), [w.b, nT.b], [pbk.b])
            P.op("act", lambda e: e.copy(out=glrT[0:16, :], in_=pbk[0:16, 0:TG]), [pbk.b], [glrT.b])
            return r

        def gla_tile(es, tt, r, st, own, tag):
            gk, gv = r["gk"], r["gv"]
            if own is None and "lp" in r:
                lp, e1, kd, decT = r["lp"], r["e1"], r["kd"], r["decT"]
            else:
                lp = e1 = kd = decT = None
            if lp is None:
                lp = sb(es, "lp" + tag, [128, 1024], F32)
            for hf in range(2):
                pbk = bank()
                P.op("pe", lambda e, hf=hf, pbk=pbk: e.matmul(pbk[:, 0:512], lhsT=glrT[0:32, tt * 128:(tt + 1) * 128],
                                                             rhs=wg[0:32, hf * 512:(hf + 1) * 512], start=True, stop=True),
                     [glrT.b, wg.b], [pbk.b])
                P.op("act", lambda e, hf=hf, pbk=pbk: e.activation(out=lp[:, hf * 512:(hf + 1) * 512], in_=pbk[:, 0:512],
                                                                  func=AF.Exp, scale=-1.0), [pbk.b], [lp.b])
            P.op("act", lambda e: e.activation(out=lp[:], in_=lp[:], func=AF.Ln, bias=1.0), [lp.b], [lp.b])
            if e1 is None:
                e1 = sb(es, "e1" + tag, [128, 1024], F32)
                kd = sb(es, "kd" + tag, [128, 1024], BF16)
            for hf in range(2):
                pbk = bank()
                P.op("pe", lambda e, hf=hf, pbk=pbk: e.matmul(pbk[:, 0:512], lhsT=cst[:, C_M1:C_M1 + 128],
                                                             rhs=lp[:, hf * 512:(hf + 1) * 512], start=True, stop=True),
                     [cst.b, lp.b], [pbk.b])
                P.op("act", lambda e, hf=hf, pbk=pbk: e.activation(out=e1[:, hf * 512:(hf + 1) * 512], in_=pbk[:, 0:512],
                                                                  func=AF.Exp), [pbk.b], [e1.b])
            P.op("dve", lambda e: e.tensor_tensor(out=kd[:], in0=gk[:, tt, :], in1=e1[:], op=ALU.mult), [gk.b, e1.b], [kd.b])
            if decT is None:
                decT = sb(es, "dec" + tag, [128, 16], F32)
            pbd = bank()
            for dc in range(8):
                P.op("pe", lambda e, dc=dc: e.matmul(pbd[:, dc * 2:dc * 2 + 2], lhsT=lp[:, dc * 128:(dc + 1) * 128],
                                                     rhs=cst[:, C_CIND:C_CIND + 2], start=True, stop=True), [lp.b, cst.b], [pbd.b])
            P.op("act", lambda e: e.activation(out=decT[:], in_=pbd[:, 0:16], func=AF.Exp), [pbd.b], [decT.b])
            if own is not None:
                ostbf, mixedT, sog, gq = own["ostbf"], own["mixedT"], r["sog"], r["gq"]
                eb = sb(es, "eb" + tag, [128, 1024], F32)
                enb = sb(es, "enb" + tag, [128, 1024], F32)
                for hf in range(2):
                    pbk = bank()
                    P.op("pe", lambda e, hf=hf, pbk=pbk: e.matmul(pbk[:, 0:512], lhsT=cst[:, C_TRI:C_TRI + 128],
                                                                 rhs=lp[:, hf * 512:(hf + 1) * 512], start=True, stop=True),
                         [cst.b, lp.b], [pbk.b])
                    P.op("act", lambda e, hf=hf, pbk=pbk: e.activation(out=eb[:, hf * 512:(hf + 1) * 512], in_=pbk[:, 0:512],
                                                                      func=AF.Exp), [pbk.b], [eb.b])
                    P.op("act", lambda e, hf=hf, pbk=pbk: e.activation(out=enb[:, hf * 512:(hf + 1) * 512], in_=pbk[:, 0:512],
                                                                      func=AF.Exp, scale=-1.0), [pbk.b], [enb.b])
                qe = sb(es, "qe" + tag, [128, 1024], BF16)
                ke = sb(es, "ke" + tag, [128, 1024], BF16)
                P.op("dve", lambda e: e.scalar_tensor_tensor(out=qe[:], in0=gq[:, tt, :], scalar=1.0 / 16, in1=eb[:],
                                                             op0=ALU.mult, op1=ALU.mult), [gq.b, eb.b], [qe.b])
                P.op("dve", lambda e: e.tensor_tensor(out=ke[:], in0=gk[:, tt, :], in1=enb[:], op=ALU.mult), [gk.b, enb.b], [ke.b])
                qeT = sb(es, "qeT" + tag, [128, 8, 128], BF16)
                keT = sb(es, "keT" + tag, [128, 8, 128], BF16)
                transposes(Src(qe, lambda kc: qe[:, kc * 128:(kc + 1) * 128]), 8, qeT, 0)
                transposes(Src(ke, lambda kc: ke[:, kc * 128:(kc + 1) * 128]), 8, keT, 0)
                qz = [sb(es, "qz%d" % n + tag, [128, 8, 128], BF16) for n in range(2)]
                for n in range(2):
                    P.op("dve", lambda e, n=n: e.memset(qz[n][:], 0.0), [], [qz[n].b])
                    P.op("dve", lambda e, n=n: e.tensor_copy(out=qz[n][:, :, n * 64:(n + 1) * 64], in_=qeT[:, :, n * 64:(n + 1) * 64]),
                         [qeT.b], [qz[n].b])
                attT = sb(es, "attT" + tag, [128, 4, 128], BF16)
                for h in range(4):
                    pa = bank()
                    for dc in range(2):
                        P.op("pe", lambda e, h=h, dc=dc, pa=pa: e.matmul(pa[:, 0:128], lhsT=keT[:, h * 2 + dc, :], rhs=qeT[:, h * 2 + dc, :],
                                                                        start=(dc == 0), stop=(dc == 1)), [keT.b, qeT.b], [pa.b])
                    P.op("dve", lambda e, h=h, pa=pa: e.tensor_tensor(out=attT[:, h, :], in0=pa[:, 0:128], in1=cst[:, C_CM:C_CM + 128],
                                                                     op=ALU.mult), [pa.b, cst.b], [attT.b])
                po = [pb[h] for h in range(4)]
                for h in range(4):
                    P.op("pe", lambda e, h=h: e.matmul(po[h][:, 0:512], lhsT=attT[:, h, :], rhs=gv[:, tt, h * 512:(h + 1) * 512],
                                                       start=True, stop=False), [attT.b, gv.b], [po[h].b])
            for n in range(2):
                if own is not None:
                    for h in range(4):
                        for dc in range(2):
                            P.op("pe", lambda e, h=h, dc=dc, n=n: e.matmul(
                                po[h][:, 0:512], lhsT=qz[n][:, h * 2 + dc, :], rhs=ostbf[:, h * 2 + dc, :],
                                start=False, stop=(n == 1 and dc == 1)), [qz[n].b, ostbf.b], [po[h].b])
                for hd in range(8):
                    h, dc = divmod(hd, 2)
                    pbk = bank()
                    P.op("pe", lambda e, h=h, dc=dc, n=n, pbk=pbk: e.matmul(
                        pbk[:, 0:512], lhsT=kd[n * 64:(n + 1) * 64, h * 256 + dc * 128:h * 256 + (dc + 1) * 128],
                        rhs=gv[n * 64:(n + 1) * 64, tt, h * 512:(h + 1) * 512], start=True, stop=True), [kd.b, gv.b], [pbk.b])
                    P.op("dve", lambda e, hd=hd, n=n, pbk=pbk: e.scalar_tensor_tensor(
                        out=st[:, hd, :], in0=st[:, hd, :], scalar=decT[:, hd * 2 + n:hd * 2 + n + 1], in1=pbk[:, 0:512],
                        op0=ALU.mult, op1=ALU.add), [st.b, decT.b, pbk.b], [st.b])
                if own is not None:
                    P.op("act", lambda e: e.copy(out=ostbf[:].rearrange("p a b -> p (a b)"), in_=st[:].rearrange("p a b -> p (a b)")),
                         [st.b], [ostbf.b])
            if own is not None:
                junk = sb(es, "gj" + tag, [128, 512], BF16)
                ssq = sb(es, "gss" + tag, [128, 4], F32)
                for h in range(4):
                    P.op("act", lambda e, h=h: e.activation(out=junk[:], in_=po[h][:, 0:512], func=AF.Square,
                                                            accum_out=ssq[:, h:h + 1]), [po[h].b], [junk.b, ssq.b])
                rstd = rstd_from_ssq(es, ssq, 512, "g" + tag)
                omix = sb(es, "omix" + tag, [128, 2048], BF16)
                for h in range(4):
                    P.op("dve", lambda e, h=h: e.scalar_tensor_tensor(
                        out=omix[:, h * 512:(h + 1) * 512], in0=po[h][:, 0:512], scalar=rstd[:, h:h + 1],
                        in1=sog[:, tt, h * 512:(h + 1) * 512], op0=ALU.mult, op1=ALU.mult), [po[h].b, rstd.b, sog.b], [omix.b])
                for h in range(4):
                    transposes(Src(omix, lambda kc, h=h: omix[:, h * 512 + kc * 128:h * 512 + (kc + 1) * 128]), 4, mixedT, tt,
                               V_GO, kc0=16 + h * 4)

        def sweep_gla(g, nT):
            with contextlib.ExitStack() as es:
                wb = [sb(es, "wb%d" % i, [128, 32, 512], BF16) for i in range(2)]
                norm_T(es, lambda tt, g=g: x_all[g * TG + tt * 128: g * TG + (tt + 1) * 128, :], NT, V_MIX, nT, "sw", xalias=wb[1])
                sweep_mla(g, nT, es, wb)
                rr = g % 4
                if rr == 0:
                    P.op("dve", lambda e: e.tensor_scalar(out=snap[:], in0=state[:], scalar1=corec[:, 0:1], scalar2=None,
                                                          op0=ALU.mult), [state.b, corec.b], [snap.b])
                else:
                    P.op("dve", lambda e: e.scalar_tensor_tensor(out=snap[:].rearrange("p a b -> p (a b)"),
                                                                 in0=state[:].rearrange("p a b -> p (a b)"),
                                                                 scalar=corec[:, rr:rr + 1], in1=snap[:].rearrange("p a b -> p (a b)"),
                                                                 op0=ALU.mult, op1=ALU.add), [state.b, corec.b, snap.b], [snap.b])
                r = gla_alloc(es, False)
                r["lp"] = sb(es, "lp_sw", [128, 1024], F32)
                r["e1"] = sb(es, "e1_sw", [128, 1024], F32)
                r["kd"] = sb(es, "kd_sw", [128, 1024], BF16)
                r["decT"] = sb(es, "dec_sw", [128, 16], F32)
                gla_proj(r, wb, nT, False)
                for tt in range(NT):
                    gla_tile(es, tt, r, state, None, "s%d" % tt)
                P.emit()

        def own_mla(i, nT, mixedT):
            with contextlib.ExitStack() as oes:
                qnT = sb(oes, "qnT", [128, 16, TG], BF16)
                qrT = sb(oes, "qrT", [128, 8, TG], BF16)
                cqnT = sb(oes, "cqnT", [128, 8, TG], BF16)
                with contextlib.ExitStack() as es:
                    wb = [sb(es, "wb%d" % k, [128, 32, 512], BF16) for k in range(2)]
                    cq = sb(es, "cq", [128, NT, 1024], F32)

                    def ev(ci, tt, pbk, csz):
                        P.op("act", lambda e: e.copy(out=cq[:, tt, ci * 512:(ci + 1) * 512], in_=pbk[:, 0:512]), [pbk.b], [cq.b])
                    linear(wb, nT, 32, lambda ci: w_in_v[:, :, ci * 512:(ci + 1) * 512], [512, 512], NT, ev)
                    junk = sb(es, "junk3", [128, 1024], BF16)
                    ssq = sb(es, "ssq3", [128, NT], F32)
                    for tt in range(NT):
                        P.op("act", lambda e, tt=tt: e.activation(out=junk[:], in_=cq[:, tt, :], func=AF.Square,
                                                                  accum_out=ssq[:, tt:tt + 1]), [cq.b], [junk.b, ssq.b])
                    rstd = rstd_from_ssq(es, ssq, 1024, "q")
                    for tt in range(NT):
                        P.op("dve", lambda e, tt=tt: e.tensor_scalar(out=junk[:], in0=cq[:, tt, :], scalar1=rstd[:, tt:tt + 1],
                                                                     scalar2=None, op0=ALU.mult), [cq.b, rstd.b], [junk.b])
                        transposes(Src(junk, lambda kc: junk[:, kc * 128:(kc + 1) * 128]), 8, cqnT, tt, V_Q)
                    wq_v = w_uq_b.rearrange("(c p) (h e) -> p c h e", p=128, e=192)

                    def wap_n(ci):
                        return wq_v[:, :, ci * 4:(ci + 1) * 4, 0:128]

                    def ev_n(cb, pbk):
                        P.op("act", lambda e: e.copy(out=qnT[:, cb, :], in_=pbk[:, 0:TG]), [pbk.b], [qnT.b])
                    for ci in range(4):
                        wrot["i"] ^= 1
                        w = wb[wrot["i"]]
                        for hh in range(4):
                            P.op("pool", lambda e, w=w, ci=ci, hh=hh: e.dma_start(
                                out=w[:, 0:8, hh * 128:(hh + 1) * 128], in_=wq_v[:, :, ci * 4 + hh, 0:128]), [], [w.b],
                                dma=True, nowaw=True)
                        for cb in range(4):
                            pbk = bank()
                            for kc in range(8):
                                P.op("pe", lambda e, kc=kc, cb=cb, w=w, pbk=pbk: e.matmul(
                                    pbk[:, 0:TG], lhsT=w[:, kc, cb * 128:(cb + 1) * 128], rhs=cqnT[:, kc, :],
                                    start=(kc == 0), stop=(kc == 7)), [cqnT.b, w.b], [pbk.b])
                            ev_n(ci * 4 + cb, pbk)
                    qr = sb(es, "qr", [128, NT, 16, 64], F32)
                    for ci in range(2):
                        wrot["i"] ^= 1
                        w = wb[wrot["i"]]
                        for hh in range(8):
                            P.op("pool", lambda e, w=w, ci=ci, hh=hh: e.dma_start(
                                out=w[:, 0:8, hh * 64:(hh + 1) * 64], in_=wq_v[:, :, ci * 8 + hh, 128:192]), [], [w.b],
                                dma=True, nowaw=True)
                        for tt in range(NT):
                            pbk = bank()
                            for kc in range(8):
                                P.op("pe", lambda e, kc=kc, tt=tt, w=w, pbk=pbk: e.matmul(
                                    pbk[:, 0:512], lhsT=cqnT[:, kc, tt * 128:(tt + 1) * 128], rhs=w[:, kc, 0:512],
                                    start=(kc == 0), stop=(kc == 7)), [cqnT.b, w.b], [pbk.b])
                            P.op("act", lambda e, tt=tt, ci=ci, pbk=pbk: e.copy(
                                out=qr[:, tt, ci * 8:(ci + 1) * 8, :], in_=pbk[:, 0:512].rearrange("p (h e) -> p h e", e=64)),
                                [pbk.b], [qr.b])
                    cs = rope_tables(es, lambda tt: pos_own[i * TG + tt * 128: i * TG + (tt + 1) * 128, :], NT, "ow")
                    qrr = sb(es, "qrr", [128, 16, 64], BF16)
                    for tt in range(NT):
                        apply_rope(es, (qr[:, tt], qr.b), (qrr[:], qrr.b), cs, tt, 16, "q%d" % tt)
                        transposes(Src(qrr, lambda kc: qrr[:, kc * 2:(kc + 1) * 2, :].rearrange("p a b -> p (a b)")), 8, qrT, tt)
                    P.emit()
                with contextlib.ExitStack() as es:
                    nk = (4 * i + 4) * TG
                    nkc = nk // 128
                    krT2 = sb(es, "krT2", [128, nk], BF16)
                    P.op("sp", lambda e: e.dma_start(out=krT2[:], in_=KrS[:, 0:nk]), [], [krT2.b], dma=True)
                    SEG = nk if nk <= 4096 else nk // 2
                    nseg = nk // SEG
                    skc = SEG // 128
                    knT = [sb(es, "knT%d" % k, [128, SEG], BF16) for k in range(2)]
                    vaug = [sb(es, "vaug%d" % k, [128, skc, 130], BF16) for k in range(2)]
                    pT = [sb(es, "pT%d" % k, [128, 2 * TG], BF16) for k in range(2)]
                    omla = sb(es, "omla", [128, NT, 2048], F32)
                    rs = sb(es, "rsum", [128, NT], F32)
                    bank_lo[0] = 1
                    po = pb[0]
                    pov = po[:, 0:NT * 130].rearrange("p (a b) -> p a b", b=130)
                    sc = 192.0 ** -0.5
                    bcnt = 0
                    pcnt = 0
                    for h in range(16):
                        half = (h % 2) * 64
                        for sg in range(nseg):
                            kt, va = knT[bcnt % 2], vaug[bcnt % 2]
                            bcnt += 1
                            k0 = sg * SEG
                            P.op("sp", lambda e, kt=kt, h=h, k0=k0: e.dma_start(out=kt[:], in_=KnS[h, :, k0:k0 + SEG]), [], [kt.b], dma=True)
                            P.op("sp", lambda e, va=va, h=h, sg=sg: e.dma_start(out=va[:], in_=VS[h, :, sg * skc:(sg + 1) * skc, :]),
                                 [], [va.b], dma=True)
                            for kl2 in range(0, skc, 2):
                                pS = bank()
                                p = pT[pcnt % 2]
                                pcnt += 1
                                for sub in range(2):
                                    kl = kl2 + sub
                                    kc = sg * skc + kl
                                    o_ = pS[:, sub * TG:(sub + 1) * TG]
                                    masked = kc >= 4 * i * NT
                                    P.op("pe", lambda e, kl=kl, kt=kt, h=h, o_=o_, sub=sub: e.matmul(
                                        o_, lhsT=kt[:, kl * 128:(kl + 1) * 128], rhs=qnT[:, h, :], start=(sub == 0), stop=False,
                                        skip_group_check=True), [kt.b, qnT.b], [pS.b])
                                    P.op("pe", lambda e, kc=kc, h=h, o_=o_, half=half, masked=masked: e.matmul(
                                        o_, lhsT=krT2[half:half + 64, kc * 128:(kc + 1) * 128], rhs=qrT[half:half + 64, h // 2, :],
                                        start=False, stop=(not masked), skip_group_check=True), [krT2.b, qrT.b], [pS.b])
                                    if masked:
                                        rr = kc // NT - 4 * i
                                        kcin = kc % NT
                                        P.op("pe", lambda e, rr=rr, o_=o_: e.matmul(o_, lhsT=sidb[:, rr, :], rhs=negfull[:, :],
                                                                                    start=False, stop=False, skip_group_check=True),
                                             [sidb.b, negfull.b], [pS.b])
                                        P.op("pe", lambda e, rr=rr, kcin=kcin, o_=o_: e.matmul(
                                            o_, lhsT=sidb[:, 4 + rr, :], rhs=maskdiag[:, kcin, :], start=False, stop=True,
                                            skip_group_check=True), [sidb.b, maskdiag.b], [pS.b])
                                P.op("act", lambda e, p=p, pS=pS: e.activation(out=p[:], in_=pS[:, 0:2 * TG], func=AF.Exp, scale=sc),
                                     [pS.b], [p.b])
                                for sub in range(2):
                                    kl = kl2 + sub
                                    kc = sg * skc + kl
                                    for qb in range(NT):
                                        P.op("pe", lambda e, p=p, qb=qb, kc=kc, kl=kl, va=va, sub=sub: e.matmul(
                                            pov[:, qb, 0:129], lhsT=p[:, sub * TG + qb * 128:sub * TG + (qb + 1) * 128], rhs=va[:, kl, 0:129],
                                            start=(kc == 0 and qb == 0), stop=(kc == nkc - 1), skip_group_check=True), [p.b, va.b], [po.b])
                        for qb in range(NT):
                            P.op("dve", lambda e, qb=qb: e.reciprocal(out=rs[:, qb:qb + 1], in_=pov[:, qb, 128:129]), [po.b], [rs.b])
                            P.op("dve", lambda e, qb=qb, h=h: e.tensor_scalar(
                                out=omla[:, qb, h * 128:(h + 1) * 128], in0=pov[:, qb, 0:128], scalar1=rs[:, qb:qb + 1], scalar2=None,
                                op0=ALU.mult), [po.b, rs.b], [omla.b])
                    bank_lo[0] = 0
                    junk = sb(es, "junk4", [128, 2048], BF16)
                    ssq = sb(es, "ssq4", [128, NT], F32)
                    for tt in range(NT):
                        P.op("act", lambda e, tt=tt: e.activation(out=junk[:], in_=omla[:, tt, :], func=AF.Square,
                                                                  accum_out=ssq[:, tt:tt + 1]), [omla.b], [junk.b, ssq.b])
                    rstd = rstd_from_ssq(es, ssq, 2048, "mo")
                    for tt in range(NT):
                        P.op("dve", lambda e, tt=tt: e.tensor_scalar(out=junk[:], in0=omla[:, tt, :], scalar1=rstd[:, tt:tt + 1],
                                                                     scalar2=None, op0=ALU.mult), [omla.b, rstd.b], [junk.b])
                        transposes(Src(junk, lambda kc: junk[:, kc * 128:(kc + 1) * 128]), 16, mixedT, tt, V_MO)
                    P.emit()

        def own_gla(i, nT, mixedT):
            with contextlib.ExitStack() as oes:
                r = gla_alloc(oes, True)
                with contextlib.ExitStack() as es:
                    wb = [sb(es, "wb%d" % k, [128, 32, 512], BF16) for k in range(2)]
                    gla_proj(r, wb, nT, True)
                    P.emit()
                ost = sb(oes, "ost", [128, 8, 512], F32)
                ostbf = sb(oes, "ostbf", [128, 8, 512], BF16)
                bank_lo[0] = 4
                for tt in range(NT):
                    with contextlib.ExitStack() as es:
                        if tt == 0:
                            P.op("dve", lambda e: e.tensor_copy(out=ost[:].rearrange("p a b -> p (a b)"),
                                                                in_=snap[:].rearrange("p a b -> p (a b)")), [snap.b], [ost.b])
                            P.op("act", lambda e: e.copy(out=ostbf[:].rearrange("p a b -> p (a b)"),
                                                         in_=snap[:].rearrange("p a b -> p (a b)")), [snap.b], [ostbf.b])
                        gla_tile(es, tt, r, ost, {"ostbf": ostbf, "mixedT": mixedT}, "o%d" % tt)
                        P.emit()
                bank_lo[0] = 0

        def linear_res(wb, es, actT, KC, wdram, src, dst, tagn):
            rb = [sb(es, "rb%d" % k, [128, 512], F32) for k in range(2)]
            ob = [sb(es, "ob%d" % k, [128, 512], F32) for k in range(2)]
            dstb = Buf(tagn)
            cnt = [0]

            def ev(ci, tt, pbk, csz):
                k = cnt[0] % 2
                cnt[0] += 1
                P.op("sp", lambda e: e.dma_start(out=rb[k][:], in_=src[tt * 128:(tt + 1) * 128, ci * 512:(ci + 1) * 512]),
                     [], [rb[k].b], dma=True)
                P.op("dve", lambda e: e.tensor_tensor(out=ob[k][:], in0=pbk[:, 0:512], in1=rb[k][:], op=ALU.add),
                     [pbk.b, rb[k].b], [ob[k].b])
                P.op("sp", lambda e: e.dma_start(out=dst[tt * 128:(tt + 1) * 128, ci * 512:(ci + 1) * 512], in_=ob[k][:]),
                     [ob[k].b], [dstb], dma=True, nowaw=True, semname=tagn + "_%d" % k)
            linear(wb, actT, KC, wcols(wdram, 0), [512] * 8, NT, ev)

        def own_wout(i, mixedT):
            with contextlib.ExitStack() as es:
                wb = [sb(es, "wb%d" % k, [128, 32, 512], BF16) for k in range(2)]
                linear_res(wb, es, mixedT, 32, w_out_b, x_own[i * TG:(i + 1) * TG, :], hA, "hA")
                P.emit()

        def own_cross(i):
            with contextlib.ExitStack() as oes:
                nT2 = sb(oes, "nT2", [128, 32, TG], BF16)
                stage_norm(lambda tt: hA[tt * 128:(tt + 1) * 128, :], NT, V_CROSS, nT2, "cr")
                ocT = sb(oes, "ocT", [128, 8, TG], BF16)
                with contextlib.ExitStack() as es:
                    wb = [sb(es, "wb%d" % k, [128, 32, 512], BF16) for k in range(2)]
                    qcT = sb(es, "qcT", [128, 8, TG], BF16)

                    def ev_q(cb, pbk):
                        P.op("act", lambda e: e.copy(out=qcT[:, cb, :], in_=pbk[:, 0:TG]), [pbk.b], [qcT.b])
                    linearT(wb, nT2, 32, wcols(w_cq_b, 0), 2, TG, ev_q)
                    pT2 = sb(es, "pT2", [128, 2, TG], BF16)
                    oc = sb(es, "oc", [128, NT, 1024], BF16)
                    rs = sb(es, "rs2", [128, 1], F32)
                    for h in range(4):
                        for mc in range(2):
                            pS = bank()
                            for hf in range(2):
                                P.op("pe", lambda e, h=h, mc=mc, hf=hf, pS=pS: e.matmul(
                                    pS[:, 0:TG], lhsT=KmT[:, h * 2 + hf, mc * 128:(mc + 1) * 128], rhs=qcT[:, h * 2 + hf, :],
                                    start=(hf == 0), stop=(hf == 1)), [KmT.b, qcT.b], [pS.b])
                            P.op("act", lambda e, mc=mc, pS=pS: e.activation(out=pT2[:, mc, :], in_=pS[:, 0:TG], func=AF.Exp,
                                                                            scale=1.0 / 16), [pS.b], [pT2.b])
                        for tt in range(NT):
                            po = bank()
                            for mc in range(2):
                                P.op("pe", lambda e, h=h, mc=mc, tt=tt, po=po: e.matmul(
                                    po[:, 0:257], lhsT=pT2[:, mc, tt * 128:(tt + 1) * 128], rhs=Vm[:, mc, h, 0:257],
                                    start=(mc == 0), stop=(mc == 1)), [pT2.b, Vm.b], [po.b])
                            P.op("dve", lambda e, po=po: e.reciprocal(out=rs[:], in_=po[:, 256:257]), [po.b], [rs.b])
                            P.op("dve", lambda e, po=po, h=h, tt=tt: e.tensor_scalar(
                                out=oc[:, tt, h * 256:(h + 1) * 256], in0=po[:, 0:256], scalar1=rs[:, 0:1], scalar2=None,
                                op0=ALU.mult), [po.b, rs.b], [oc.b])
                    for tt in range(NT):
                        transposes(Src(oc, lambda kc, tt=tt: oc[:, tt, kc * 128:(kc + 1) * 128]), 8, ocT, tt)
                    P.emit()
                with contextlib.ExitStack() as es:
                    wb = [sb(es, "wb%d" % k, [128, 32, 512], BF16) for k in range(2)]
                    linear_res(wb, es, ocT, 8, w_co_b, hA, hB, "hB")
                    P.emit()

        def own_peer(i):
            with contextlib.ExitStack() as oes:
                x3T = sb(oes, "x3T", [128, 32, TG], BF16)
                stage_norm(lambda tt: hB[tt * 128:(tt + 1) * 128, :], NT, V_FFN, x3T, "pe")
                s2 = sb(oes, "s2", [128, NT, 8, 128], F32)
                thr1 = sb(oes, "thr1", [128, NT, 8, 128], F32)
                w2 = sb(oes, "w2", [128, NT, 8, 128], BF16)
                w1c = sb(oes, "w1c", [128, NT, 8, 128], F32)
                with contextlib.ExitStack() as es:
                    qpT = sb(es, "qpT", [128, 16, TG], BF16)
                    with contextlib.ExitStack() as es2:
                        wb = [sb(es2, "wb%d" % k, [128, 32, 512], BF16) for k in range(2)]

                        def ev_q(cb, pbk):
                            P.op("act", lambda e: e.copy(out=qpT[:, cb, :], in_=pbk[:, 0:TG]), [pbk.b], [qpT.b])
                        linearT(wb, x3T, 32, wcols(w_pq_b, 0), 4, TG, ev_q)
                        P.emit()
                    sc = sb(es, "sc", [128, 16, 128], F32)
                    scr = sb(es, "scr", [128, 16, 128], F32)
                    v = sb(es, "v16", [128, 16, 16], F32)
                    cand = sb(es, "cand", [128, 8, 16, 16], F32)
                    cand2 = sb(es, "cand2", [128, 8, 256], F32)
                    cs_ = sb(es, "cs16", [128, 8, 16], F32)
                    small = sb(es, "small", [128, 8, 8], F32)
                    ejunk = sb(es, "ejunk", [128, 16], F32)
                    for tt in range(NT):
                        for q4 in range(4):
                            pS = bank()
                            for k in range(4):
                                hp = q4 * 4 + k
                                P.op("pe", lambda e, hp=hp, k=k, tt=tt, pS=pS: e.matmul(
                                    pS[:, k * 128:(k + 1) * 128], lhsT=qpT[:, hp, tt * 128:(tt + 1) * 128], rhs=subkT[:, hp, :],
                                    start=True, stop=True), [qpT.b, subkT.b], [pS.b])
                            P.op("act", lambda e, q4=q4, pS=pS: e.copy(out=sc[:, q4 * 4:(q4 + 1) * 4, :].rearrange("p a b -> p (a b)"),
                                                                       in_=pS[:, 0:512]), [pS.b], [sc.b])
                        for hp in range(16):
                            P.op("dve", lambda e, hp=hp: e.max(out=v[:, hp, 0:8], in_=sc[:, hp, :]), [sc.b], [v.b])
                            P.op("dve", lambda e, hp=hp: e.match_replace(out=scr[:, hp, :], in_to_replace=v[:, hp, 0:8],
                                                                         in_values=sc[:, hp, :], imm_value=-1e30), [sc.b, v.b], [scr.b])
                            P.op("dve", lambda e, hp=hp: e.max(out=v[:, hp, 8:16], in_=scr[:, hp, :]), [scr.b], [v.b])
                        vv = v[:].rearrange("p (h t) k -> p h t k", t=2)
                        P.op("dve", lambda e: e.tensor_tensor(
                            out=cand[:], in0=vv[:, :, 0, :].unsqueeze(3).to_broadcast([128, 8, 16, 16]),
                            in1=vv[:, :, 1, :].unsqueeze(2).to_broadcast([128, 8, 16, 16]), op=ALU.add), [v.b], [cand.b])
                        for h in range(8):
                            cf = cand[:, h].rearrange("p a b -> p (a b)")
                            P.op("dve", lambda e, h=h, cf=cf: e.max(out=cs_[:, h, 0:8], in_=cf), [cand.b], [cs_.b])
                            P.op("dve", lambda e, h=h, cf=cf: e.match_replace(out=cand2[:, h, :], in_to_replace=cs_[:, h, 0:8],
                                                                             in_values=cf, imm_value=-1e30), [cand.b, cs_.b], [cand2.b])
                            P.op("dve", lambda e, h=h: e.max(out=cs_[:, h, 8:16], in_=cand2[:, h, :]), [cand2.b], [cs_.b])
                        P.op("dve", lambda e: e.tensor_scalar(out=small[:, :, 0], in0=cs_[:, :, 15], scalar1=-1e-4, scalar2=None,
                                                              op0=ALU.add), [cs_.b], [small.b])
                        P.op("dve", lambda e: e.tensor_scalar(out=small[:, :, 1], in0=cs_[:, :, 0], scalar1=-1.0, scalar2=None,
                                                              op0=ALU.mult), [cs_.b], [small.b])
                        for h in range(8):
                            P.op("act", lambda e, h=h: e.activation(out=ejunk[:], in_=cs_[:, h, :], func=AF.Exp, bias=small[:, h, 1:2],
                                                                    accum_out=small[:, h, 2:3]), [cs_.b, small.b], [ejunk.b, small.b])
                        P.op("dve", lambda e: e.reciprocal(out=small[:, :, 3], in_=small[:, :, 2]), [small.b], [small.b])
                        for h in range(8):
                            P.op("dve", lambda e, h=h, tt=tt: e.tensor_copy(out=s2[:, tt, h, :], in_=sc[:, 2 * h + 1, :]), [sc.b], [s2.b])
                            P.op("act", lambda e, h=h, tt=tt: e.activation(out=w2[:, tt, h, :], in_=sc[:, 2 * h + 1, :], func=AF.Exp,
                                                                          bias=small[:, h, 1:2]), [sc.b, small.b], [w2.b])
                            P.op("act", lambda e, h=h, tt=tt: e.activation(out=scr[:, h, :], in_=sc[:, 2 * h, :], func=AF.Exp),
                                 [sc.b], [scr.b])
                            P.op("dve", lambda e, h=h, tt=tt: e.tensor_scalar(out=w1c[:, tt, h, :], in0=scr[:, h, :],
                                                                             scalar1=small[:, h, 3:4], scalar2=None, op0=ALU.mult),
                                 [scr.b, small.b], [w1c.b])
                            P.op("dve", lambda e, h=h, tt=tt: e.tensor_scalar(out=thr1[:, tt, h, :], in0=sc[:, 2 * h, :], scalar1=-1.0,
                                                                             scalar2=small[:, h, 0:1], op0=ALU.mult, op1=ALU.add),
                                 [sc.b, small.b], [thr1.b])
                    P.emit()
                outacc = sb(oes, "outacc", [128, NT, D], F32)
                with contextlib.ExitStack() as es:
                    ub = [sb(es, "ub%d" % k, [128, 32, 256], BF16) for k in range(2)]
                    vb = [sb(es, "vb%d" % k, [128, 16, 256], BF16) for k in range(2)]
                    NGH = 16
                    ghs = [sb(es, "gh%d" % k, [128, 128], BF16) for k in range(NGH)]
                    dgs = [sb(es, "dg%d" % k, [128, 128], BF16) for k in range(NGH)]
                    gls = [sb(es, "gl%d" % k, [128, TG], F32) for k in range(2)]
                    ATs = [sb(es, "AT%d" % k, [128, 16, TG], BF16) for k in range(2)]
                    puT = peer_uT.rearrange("(c p) n -> p c n", p=128)
                    pvv = peer_v.rearrange("(c p) n -> p c n", p=128)
                    cnts = {"u": 0, "v": 0, "a": 0, "g": 0}
                    ublk = {}
                    pGs = {}

                    puT_f = peer_uT.rearrange("(c p) n -> p c n", p=128)
                    pv_f = peer_v.rearrange("(c p) n -> p c n", p=128)
                    dbu, dbv = Buf("cv_u"), Buf("cv_v")

                    def load_u(blk):
                        w = ub[cnts["u"] % 2]
                        cnts["u"] += 1
                        if i == 0:
                            P.op("pool", lambda e, w=w, blk=blk: e.dma_start(out=w[:], in_=puT_f[:, :, blk * 256:(blk + 1) * 256]),
                                 [], [w.b], dma=True)
                            P.op("sp", lambda e, w=w, blk=blk: e.dma_start(out=Ubf[blk], in_=w[:].rearrange("p c n -> p (c n)")),
                                 [w.b], [dbu], dma=True, nowaw=True, semname="cvu_" + w.b.name)
                        else:
                            P.op("pool", lambda e, w=w, blk=blk: e.dma_start(
                                out=w[:].rearrange("p c n -> p (c n)"), in_=Ubf[blk]), [], [w.b], dma=True)
                        ublk[blk] = w

                    def g_stuff(idx):
                        pG = pb[idx % 2]
                        pGs[idx] = pG
                        first = True
                        for tt in range(NT):
                            for h in range(8):
                                gh, dg = ghs[cnts["g"] % NGH], dgs[cnts["g"] % NGH]
                                cnts["g"] += 1
                                P.op("dve", lambda e, h=h, tt=tt, gh=gh: e.scalar_tensor_tensor(
                                    out=gh[:], in0=s2[:, tt, h, :], scalar=thr1[:, tt, h, idx:idx + 1], in1=w2[:, tt, h, :],
                                    op0=ALU.is_ge, op1=ALU.mult), [s2.b, thr1.b, w2.b], [gh.b])
                                P.op("act", lambda e, h=h, tt=tt, dg=dg: e.activation(
                                    out=dg[:], in_=identb[:], func=AF.Copy, scale=w1c[:, tt, h, idx:idx + 1]),
                                    [identb.b, w1c.b], [dg.b])
                                P.op("pe", lambda e, h=h, tt=tt, gh=gh, dg=dg, first=first: e.matmul(
                                    pG[:, tt * 128:(tt + 1) * 128], lhsT=gh[:], rhs=dg[:], start=first, stop=(h == 7),
                                    skip_group_check=True), [gh.b, dg.b], [pG.b])
                                first = False

                    def u_mm(idx):
                        blk, sub = divmod(idx, 2)
                        if sub == 0:
                            if blk not in ublk:
                                load_u(blk)
                            if blk + 1 < 64:
                                load_u(blk + 1)
                        w = ublk[blk]
                        pH = bank()
                        for kc in range(32):
                            P.op("pe", lambda e, kc=kc: e.matmul(
                                pH[:, 0:TG], lhsT=w[:, kc, sub * 128:(sub + 1) * 128], rhs=x3T[:, kc, :],
                                start=(kc == 0), stop=(kc == 31)), [x3T.b, w.b], [pH.b])
                        return pH

                    def a_mult(idx, pH):
                        eg, eb_ = divmod(idx, 16)
                        AT = ATs[eg % 2]
                        gl = gls[cnts["a"] % 2]
                        cnts["a"] += 1
                        pG = pGs.pop(idx)
                        P.op("act", lambda e: e.activation(out=gl[:], in_=pH[:, 0:TG], func=AF.Gelu), [pH.b], [gl.b])
                        P.op("dve", lambda e: e.tensor_tensor(out=AT[:, eb_, :], in0=pG[:, 0:TG], in1=gl[:], op=ALU.mult),
                             [gl.b, pG.b], [AT.b])

                    def v_phase(eg):
                        AT = ATs[eg % 2]
                        for db in range(16):
                            w = vb[cnts["v"] % 2]
                            cnts["v"] += 1
                            if i == 0:
                                P.op("pool", lambda e, w=w, db=db: e.dma_start(
                                    out=w[:], in_=pv_f[:, eg * 16:(eg + 1) * 16, db * 256:(db + 1) * 256]), [], [w.b], dma=True)
                                P.op("sp", lambda e, w=w, pc=eg * 16 + db: e.dma_start(
                                    out=Vbf[pc], in_=w[:].rearrange("p c n -> p (c n)")), [w.b], [dbv], dma=True, nowaw=True,
                                    semname="cvv_" + w.b.name)
                            else:
                                P.op("pool", lambda e, w=w, pc=eg * 16 + db: e.dma_start(
                                    out=w[:].rearrange("p c n -> p (c n)"), in_=Vbf[pc]), [], [w.b], dma=True)
                            for tt in range(NT):
                                pO = bank()
                                for eb_ in range(16):
                                    P.op("pe", lambda e, eb_=eb_, tt=tt, w=w, pO=pO: e.matmul(
                                        pO[:, 0:256], lhsT=AT[:, eb_, tt * 128:(tt + 1) * 128], rhs=w[:, eb_, :],
                                        start=(eb_ == 0), stop=(eb_ == 15)), [AT.b, w.b], [pO.b])
                                o = outacc[:, tt, db * 256:(db + 1) * 256]
                                if eg == 0:
                                    P.op("act", lambda e, o=o, pO=pO: e.copy(out=o, in_=pO[:, 0:256]), [pO.b], [outacc.b])
                                else:
                                    P.op("dve", lambda e, o=o, pO=pO: e.tensor_tensor(out=o, in0=pO[:, 0:256], in1=o, op=ALU.add),
                                         [pO.b, outacc.b], [outacc.b])

                    bank_lo[0] = 2
                    g_stuff(0)
                    for idx in range(128):
                        pH = u_mm(idx)
                        if idx + 1 < 128:
                            g_stuff(idx + 1)
                        a_mult(idx, pH)
                        if idx % 16 == 15:
                            v_phase(idx // 16)
                    bank_lo[0] = 0
                    P.emit()
                with contextlib.ExitStack() as es:
                    gfin = sb(es, "gfin", [128, D], F32)
                    P.op("sp", lambda e: e.dma_start(out=gfin[:], in_=gfin_in.partition_broadcast(128)), [], [gfin.b], dma=True)
                    hb_ = sb(es, "hbt", [128, D], F32)
                    junk = sb(es, "junk5", [128, D], BF16)
                    ssq = sb(es, "ssq5", [128, NT], F32)
                    outb = Buf("out")
                    for tt in range(NT):
                        P.op("sp", lambda e, tt=tt: e.dma_start(out=hb_[:], in_=hB[tt * 128:(tt + 1) * 128, :]), [], [hb_.b], dma=True)
                        P.op("dve", lambda e, tt=tt: e.tensor_tensor(out=outacc[:, tt, :], in0=outacc[:, tt, :], in1=hb_[:], op=ALU.add),
                             [outacc.b, hb_.b], [outacc.b])
                        P.op("act", lambda e, tt=tt: e.activation(out=junk[:], in_=outacc[:, tt, :], func=AF.Square,
                                                                  accum_out=ssq[:, tt:tt + 1]), [outacc.b], [junk.b, ssq.b])
                    rstd = rstd_from_ssq(es, ssq, D, "fin")
                    for tt in range(NT):
                        P.op("dve", lambda e, tt=tt: e.scalar_tensor_tensor(
                            out=outacc[:, tt, :], in0=outacc[:, tt, :], scalar=rstd[:, tt:tt + 1], in1=gfin[:],
                            op0=ALU.mult, op1=ALU.mult), [outacc.b, rstd.b, gfin.b], [outacc.b])
                        P.op("sp", lambda e, tt=tt: e.dma_start(out=out[i * TG + tt * 128: i * TG + (tt + 1) * 128, :], in_=outacc[:, tt, :]),
                             [outacc.b], [outb], dma=True, nowaw=True)
                    P.emit()

        stop_after = dbg if isinstance(dbg, str) else None
        for i in range(NOWN):
            for rr in range(4):
                g = 4 * i + rr
                with contextlib.ExitStack() as gs:
                    nT = sb(gs, "nT", [128, 32, TG], BF16)
                    sweep_gla(g, nT)
            with contextlib.ExitStack() as gs:
                mixedT = sb(gs, "mixedT", [128, 32, TG], BF16)
                nT = sb(gs, "nT", [128, 32, TG], BF16)
                stage_norm(lambda tt, i=i: x_own[i * TG + tt * 128: i * TG + (tt + 1) * 128, :], NT, V_MIX, nT, "ow")
                own_mla(i, nT, mixedT)
                own_gla(i, nT, mixedT)
                own_wout(i, mixedT)
            if stop_after == "mixer":
                P.op("sp", lambda e, i=i: e.dma_start(out=out[i * TG:(i + 1) * TG, :], in_=hA), [], [Buf("out")], dma=True)
                P.emit()
                continue
            own_cross(i)
            if stop_after == "cross":
                P.op("sp", lambda e, i=i: e.dma_start(out=out[i * TG:(i + 1) * TG, :], in_=hB), [], [Buf("out")], dma=True)
                P.emit()
                continue
            own_peer(i)
    return nc


def host_consts():
    cst = np.zeros((128, 1024), np.float32)
    cst[:, 0:128] = np.eye(128)
    j = np.arange(128)[:, None]
    c = np.arange(128)[None, :]
    same = (j // 64) == (c // 64)
    cst[:, 128:256] = np.where(same & (j > c), -1.0 / 16, 0.0)
    cst[:, 256:384] = np.where(same & (j <= c), -1.0 / 16, 0.0)
    cst[:, 384:512] = np.where(same & (j <= c), 1.0, 0.0)
    cst[:, 512] = np.where(np.arange(128) < 64, -1.0 / 16, 0.0)
    cst[:, 513] = np.where(np.arange(128) >= 64, -1.0 / 16, 0.0)
    inv = 10000.0 ** (-np.arange(0, 64, 2, dtype=np.float32) / 64)
    cst[:, 514:546] = (inv / (2 * np.pi))[None, :]
    cst[:, 640:768] = np.where(j <= c, 1.0, 0.0)
    return cst


def tvec(g):
    g = np.asarray(g, np.float32).reshape(-1)
    return np.ascontiguousarray(g.reshape(-1, 128).T)


def core_inputs(b, j, S, NOWN, x, mem, positions, norm_mem, norm_mix, w_in, mla_q_norm, w_uq, mla_kv_norm, w_ukv,
                mla_out_norm, w_gate_up, b_gate, gla_out_norm, w_out, norm_cross, w_cq, w_ck, w_cv, w_co,
                norm_ffn, w_peer_q, peer_sub_keys, peer_u, peer_v, norm_final, shared):
    f = lambda a: np.ascontiguousarray(np.asarray(a, np.float32))
    xb = f(x[b])
    own_rows = np.concatenate([np.arange((4 * i + j) * TG, (4 * i + j + 1) * TG) for i in range(NOWN)])
    pos = np.asarray(positions[b], np.int32).reshape(S, 1)
    corec = np.zeros((128, 1028), np.float32)
    corec[:, j] = 1.0
    sid = np.zeros((8, 128, 128), np.float32)
    for r in range(4):
        if r > j:
            sid[r] = np.eye(128)
        if r == j:
            sid[4 + r] = np.eye(128)
    corec[:, 4:1028] = sid.transpose(1, 0, 2).reshape(128, 1024)
    d = dict(shared)
    d.update(x_all=xb, x_own=np.ascontiguousarray(xb[own_rows]), pos_all=pos, pos_own=np.ascontiguousarray(pos[own_rows]),
             mem=f(mem[b]), corec=corec)
    return d


def shared_inputs(norm_mem, norm_mix, w_in, mla_q_norm, w_uq, mla_kv_norm, w_ukv, mla_out_norm, w_gate_up, b_gate,
                  gla_out_norm, w_out, norm_cross, w_cq, w_ck, w_cv, w_co, norm_ffn, w_peer_q, peer_sub_keys,
                  peer_u, peer_v, norm_final):
    f = lambda a: np.ascontiguousarray(np.asarray(a, np.float32))
    vecs = np.zeros((128, 160), np.float32)
    vecs[:, 0:32] = tvec(norm_mix[0])
    vecs[:, 32:64] = tvec(norm_cross[0])
    vecs[:, 64:96] = tvec(norm_ffn[0])
    vecs[:, 96:128] = tvec(norm_mem)
    vecs[:, 128:136] = tvec(mla_q_norm[0])
    vecs[:, 136:140] = tvec(mla_kv_norm[0])
    vecs[:, 140:156] = tvec(mla_out_norm[0])
    vecs[:, 156:160] = tvec(gla_out_norm[0])
    wg = np.concatenate([f(w_gate_up[0]), f(b_gate[0]).reshape(1, 1024)], axis=0)
    sk = np.asarray(peer_sub_keys[0], np.float32).reshape(16, 128, 128)
    subk = np.ascontiguousarray(sk.transpose(2, 0, 1))
    return dict(w_in=f(w_in[0]), w_uq=f(w_uq[0]), w_ukv=f(w_ukv[0]), wg=wg, w_out=f(w_out[0]), w_cq=f(w_cq[0]),
                w_ck=f(w_ck[0]), w_cv=f(w_cv[0]), w_co=f(w_co[0]), w_pq=f(w_peer_q[0]), subk=subk,
                peer_uT=np.ascontiguousarray(np.asarray(peer_u[0], np.float32).T), peer_v=f(peer_v[0]),
                vecs=vecs, gfin=f(norm_final).reshape(1, D), cst=host_consts())


def kernel(x, mem, positions, norm_mem, norm_mix, w_in, mla_q_norm, w_uq, mla_kv_norm, w_ukv,
           mla_out_norm, w_gate_up, b_gate, gla_out_norm, w_out, norm_cross, w_cq, w_ck, w_cv, w_co,
           norm_ffn, w_peer_q, peer_sub_keys, peer_u, peer_v, norm_final):
    x = np.asarray(x)
    B, S, _ = x.shape
    NOWN = S // TG // 4
    shared = shared_inputs(norm_mem, norm_mix, w_in, mla_q_norm, w_uq, mla_kv_norm, w_ukv, mla_out_norm, w_gate_up,
                           b_gate, gla_out_norm, w_out, norm_cross, w_cq, w_ck, w_cv, w_co, norm_ffn, w_peer_q,
                           peer_sub_keys, peer_u, peer_v, norm_final)
    in_maps = []
    for c in range(8):
        b, j = divmod(c, 4)
        in_maps.append(core_inputs(b, j, S, NOWN, x, mem, positions, norm_mem, norm_mix, w_in, mla_q_norm, w_uq,
                                   mla_kv_norm, w_ukv, mla_out_norm, w_gate_up, b_gate, gla_out_norm, w_out, norm_cross,
                                   w_cq, w_ck, w_cv, w_co, norm_ffn, w_peer_q, peer_sub_keys, peer_u, peer_v, norm_final,
                                   shared))
    nc = build(S, NOWN)
    res = run_bass_kernel_spmd(nc, in_maps, core_ids=list(range(8)))
    outp = np.zeros((B, S, D), np.float32)
    for c in range(8):
        b, j = divmod(c, 4)
        o = np.asarray(res.results[c]["out"])
        for i in range(NOWN):
            g = 4 * i + j
            outp[b, g * TG:(g + 1) * TG] = o[i * TG:(i + 1) * TG]
    return outp
```
